# Optimizing a Trainium2 kernel written in Bass

```python
import jax, jax.numpy as jnp
from jax import lax
import numpy as np

D_MODEL = 1024
BATCH = 8
SEQ = 4096
DEPTH = 4

HEAD_DIM = 64
N_GROUP_HEADS = 4
GROUP_WIDTH = N_GROUP_HEADS * HEAD_DIM
MIX_WIDTH = 4 * GROUP_WIDTH
MLA_Q_RANK = 256
MLA_KV_RANK = 128
MLA_NOPE = 64
MLA_ROPE = 32
MLA_V = 64
SWA_KV_HEADS = 2
SWA_WINDOW = 128
IDX_HEADS = 8
IDX_DIM = 32
DSA_TOPK = 256
FOX_HEADS = 4
D_FF = 4 * D_MODEL
ROPE_THETA = 10000.0
Q_BLOCK = 128
EPS = 1e-6

IN_SPLITS = (
    MLA_Q_RANK, MLA_KV_RANK, MLA_ROPE,
    GROUP_WIDTH, SWA_KV_HEADS * HEAD_DIM, SWA_KV_HEADS * HEAD_DIM,
    GROUP_WIDTH, GROUP_WIDTH, GROUP_WIDTH, IDX_HEADS * IDX_DIM, IDX_DIM, IDX_HEADS,
    GROUP_WIDTH, GROUP_WIDTH, GROUP_WIDTH, FOX_HEADS,
)
IN_WIDTH = sum(IN_SPLITS)

kernel_name = "hybrid_parallel_mla_swa_dsa_fox"


def rms_norm(x, g):
    xf = x.astype(jnp.float32)
    y = xf * lax.rsqrt(jnp.mean(xf * xf, axis=-1, keepdims=True) + EPS)
    return (y * g.astype(jnp.float32)).astype(x.dtype)


def rope(x, pos):
    d = x.shape[-1]
    half = d // 2
    inv_freq = 1.0 / (ROPE_THETA ** (jnp.arange(0, half, dtype=jnp.float32) * 2.0 / d))
    ang = pos[:, None] * inv_freq[None, :]
    cos = jnp.cos(ang)[:, None, :]
    sin = jnp.sin(ang)[:, None, :]
    xf = x.astype(jnp.float32)
    x1, x2 = xf[..., :half], xf[..., half:]
    out = jnp.concatenate([x1 * cos - x2 * sin, x2 * cos + x1 * sin], axis=-1)
    return out.astype(x.dtype)


def split_cols(proj):
    offs, acc = [], 0
    for n in IN_SPLITS[:-1]:
        acc += n
        offs.append(acc)
    return jnp.split(proj, offs, axis=-1)


def causal_block_attention(q, k, v, scale, log_f_cum=None):
    B, S, H, dk = q.shape
    dv = v.shape[-1]
    nb = S // Q_BLOCK
    q_blocks = q.reshape(B, nb, Q_BLOCK, H, dk).swapaxes(0, 1)
    starts = jnp.arange(nb, dtype=jnp.int32) * Q_BLOCK
    key_pos = jnp.arange(S, dtype=jnp.int32)
    if log_f_cum is None:
        xs = (q_blocks, starts)
    else:
        cum_blocks = log_f_cum.reshape(B, nb, Q_BLOCK, H).swapaxes(0, 1)
        cum_keys = log_f_cum.transpose(0, 2, 1)[:, :, None, :]
        xs = (q_blocks, starts, cum_blocks)

    def block(args):
        q_blk, start = args[0], args[1]
        s = jnp.einsum('bqhd,bkhd->bhqk', q_blk, k).astype(jnp.float32) * scale
        if log_f_cum is not None:
            s = s + (args[2].transpose(0, 2, 1)[..., None] - cum_keys)
        qpos = start + jnp.arange(Q_BLOCK, dtype=jnp.int32)
        causal = key_pos[None, :] <= qpos[:, None]
        s = jnp.where(causal, s, -jnp.inf)
        p = jax.nn.softmax(s, axis=-1).astype(v.dtype)
        return jnp.einsum('bhqk,bkhd->bqhd', p, v)

    out = lax.map(block, xs)
    return out.swapaxes(0, 1).reshape(B, S, H, dv)


def sliding_window_attention(q, k, v, sinks, scale):
    B, S, H, d = q.shape
    Hk = k.shape[2]
    G = H // Hk
    W = SWA_WINDOW
    nb = S // W
    qb = q.reshape(B, nb, W, Hk, G, d)

    def with_prev(t):
        tb = t.reshape(B, nb, W, Hk, d)
        prev = jnp.concatenate([jnp.zeros_like(tb[:, :1]), tb[:, :-1]], axis=1)
        return jnp.concatenate([prev, tb], axis=2)

    kb, vb = with_prev(k), with_prev(v)
    s = jnp.einsum('bnqkgd,bnjkd->bnkgqj', qb, kb).astype(jnp.float32) * scale
    blk = jnp.arange(nb, dtype=jnp.int32)[:, None, None]
    qi = jnp.arange(W, dtype=jnp.int32)[None, :, None]
    kj = jnp.arange(2 * W, dtype=jnp.int32)[None, None, :]
    qpos = blk * W + qi
    kpos = blk * W - W + kj
    mask = (kpos >= 0) & (kpos <= qpos) & (qpos - kpos < SWA_WINDOW)
    s = jnp.where(mask[None, :, None, None], s, -jnp.inf)
    sink = jnp.broadcast_to(sinks.astype(jnp.float32).reshape(1, 1, Hk, G, 1, 1),
                            s.shape[:-1] + (1,))
    p = jax.nn.softmax(jnp.concatenate([s, sink], axis=-1), axis=-1)[..., :-1]
    o = jnp.einsum('bnkgqj,bnjkd->bnqkgd', p.astype(v.dtype), vb)
    return o.reshape(B, S, H, d)


def dsa_attention(q, k, v, q_idx, k_idx, w_idx, top_k, scale):
    B, S, H, d = q.shape
    nb = S // Q_BLOCK

    def blocks(t):
        return t.reshape((B, nb, Q_BLOCK) + t.shape[2:]).swapaxes(0, 1)

    starts = jnp.arange(nb, dtype=jnp.int32) * Q_BLOCK
    key_pos = jnp.arange(S, dtype=jnp.int32)

    def block(args):
        q_blk, qi_blk, w_blk, start = args
        qpos = start + jnp.arange(Q_BLOCK, dtype=jnp.int32)
        rel = jax.nn.relu(jnp.einsum('bqhd,bkd->bqhk', qi_blk, k_idx).astype(jnp.float32))
        score = jnp.einsum('bqhk,bqh->bqk', rel, w_blk.astype(jnp.float32))
        score = jnp.where(key_pos[None, None, :] <= qpos[None, :, None], score, -jnp.inf)
        _, idx = lax.top_k(score, top_k)
        k_sel = jax.vmap(lambda kb, ib: kb[ib])(k, idx)
        v_sel = jax.vmap(lambda vb, ib: vb[ib])(v, idx)
        s = jnp.einsum('bqhd,bqkhd->bqhk', q_blk, k_sel).astype(jnp.float32) * scale
        valid = (idx <= qpos[None, :, None])[:, :, None, :]
        s = jnp.where(valid, s, -jnp.inf)
        p = jax.nn.softmax(s, axis=-1).astype(v.dtype)
        return jnp.einsum('bqhk,bqkhd->bqhd', p, v_sel)

    out = lax.map(block, (blocks(q), blocks(q_idx), blocks(w_idx), starts))
    return out.swapaxes(0, 1).reshape(B, S, H, d)


def setup_inputs(seed: int = 0) -> dict:
    key = jax.random.key(seed)
    ks = jax.random.split(key, 16)
    H = N_GROUP_HEADS
    f32 = jnp.float32

    def nrm(k, shape, fan_in):
        return jax.random.normal(k, shape, f32) * (fan_in ** -0.5)

    def gain(k, shape):
        return 1.0 + 0.05 * jax.random.normal(k, shape, f32)

    return {
        "x": jax.random.normal(ks[0], (BATCH, SEQ, D_MODEL), f32),
        "norm1": gain(ks[1], (DEPTH, D_MODEL)),
        "w_in": nrm(ks[2], (DEPTH, D_MODEL, IN_WIDTH), D_MODEL),
        "mla_q_norm": gain(ks[3], (DEPTH, MLA_Q_RANK)),
        "mla_kv_norm": gain(ks[4], (DEPTH, MLA_KV_RANK)),
        "mla_w_uq": nrm(ks[5], (DEPTH, MLA_Q_RANK, H * (MLA_NOPE + MLA_ROPE)), MLA_Q_RANK),
        "mla_w_ukv": nrm(ks[6], (DEPTH, MLA_KV_RANK, H * (MLA_NOPE + MLA_V)), MLA_KV_RANK),
        "swa_sinks": 0.5 * jax.random.normal(ks[7], (DEPTH, H), f32),
        "fox_b_f": 0.1 * jax.random.normal(ks[8], (DEPTH, FOX_HEADS), f32),
        "w_out": nrm(ks[9], (DEPTH, MIX_WIDTH, D_MODEL), MIX_WIDTH),
        "norm2": gain(ks[10], (DEPTH, D_MODEL)),
        "w_up": nrm(ks[11], (DEPTH, D_MODEL, D_FF), D_MODEL),
        "w_down": nrm(ks[12], (DEPTH, D_FF, D_MODEL), D_FF),
        "final_norm": gain(ks[13], (D_MODEL,)),
    }


def reference(x, norm1, w_in, mla_q_norm, mla_kv_norm, mla_w_uq, mla_w_ukv, swa_sinks,
              fox_b_f, w_out, norm2, w_up, w_down, final_norm):
    B, S, _ = x.shape
    H = N_GROUP_HEADS
    Dh = HEAD_DIM
    pos = jnp.arange(S, dtype=jnp.float32)
    top_k = min(DSA_TOPK, S // 4)
    for l in range(DEPTH):
        h = rms_norm(x, norm1[l])
        proj = h @ w_in[l]
        (a_cq, a_ckv, a_kr, b_q, b_k, b_v, c_q, c_k, c_v, c_qi, c_ki, c_w,
         d_q, d_k, d_v, d_f) = split_cols(proj)

        qa = (rms_norm(a_cq, mla_q_norm[l]) @ mla_w_uq[l]).reshape(B, S, H, MLA_NOPE + MLA_ROPE)
        q_a = jnp.concatenate([qa[..., :MLA_NOPE], rope(qa[..., MLA_NOPE:], pos)], axis=-1)
        kv = (rms_norm(a_ckv, mla_kv_norm[l]) @ mla_w_ukv[l]).reshape(B, S, H, MLA_NOPE + MLA_V)
        k_rope = rope(a_kr.reshape(B, S, 1, MLA_ROPE), pos)
        k_a = jnp.concatenate([kv[..., :MLA_NOPE],
                               jnp.broadcast_to(k_rope, (B, S, H, MLA_ROPE))], axis=-1)
        o_a = causal_block_attention(q_a, k_a, kv[..., MLA_NOPE:],
                                     (MLA_NOPE + MLA_ROPE) ** -0.5)

        q_b = rope(b_q.reshape(B, S, H, Dh), pos)
        k_b = rope(b_k.reshape(B, S, SWA_KV_HEADS, Dh), pos)
        v_b = b_v.reshape(B, S, SWA_KV_HEADS, Dh)
        o_b = sliding_window_attention(q_b, k_b, v_b, swa_sinks[l], Dh ** -0.5)

        q_c = rope(c_q.reshape(B, S, H, Dh), pos)
        k_c = rope(c_k.reshape(B, S, H, Dh), pos)
        v_c = c_v.reshape(B, S, H, Dh)
        qi = rope(c_qi.reshape(B, S, IDX_HEADS, IDX_DIM), pos)
        ki = rope(c_ki.reshape(B, S, 1, IDX_DIM), pos)[:, :, 0, :]
        o_c = dsa_attention(q_c, k_c, v_c, qi, ki, c_w, top_k, Dh ** -0.5)

        log_f = jax.nn.log_sigmoid((d_f + fox_b_f[l]).astype(jnp.float32))
        cum = lax.cumsum(log_f, axis=1)
        o_d = causal_block_attention(d_q.reshape(B, S, FOX_HEADS, Dh),
                                     d_k.reshape(B, S, FOX_HEADS, Dh),
                                     d_v.reshape(B, S, FOX_HEADS, Dh),
                                     Dh ** -0.5, log_f_cum=cum)

        mixed = jnp.concatenate([o_a.reshape(B, S, GROUP_WIDTH), o_b.reshape(B, S, GROUP_WIDTH),
                                 o_c.reshape(B, S, GROUP_WIDTH), o_d.reshape(B, S, GROUP_WIDTH)],
                                axis=-1)
        x = x + mixed @ w_out[l]

        h2 = rms_norm(x, norm2[l])
        x = x + jnp.square(jax.nn.relu(h2 @ w_up[l])) @ w_down[l]
    return rms_norm(x, final_norm)
```

```python
import numpy as np
from contextlib import ExitStack
import concourse.bass as bass
import concourse.mybir as mybir
from concourse.bass_utils import run_bass_kernel_spmd

F32 = mybir.dt.float32
BF16 = mybir.dt.bfloat16
ALU = mybir.AluOpType
AF = mybir.ActivationFunctionType
AX = mybir.AxisListType

D_MODEL = 1024
DEPTH = 4
SEQ = 4096
IN_WIDTH = 2764
D_FF = 4096
EPS = 1e-6
BIGM = 262144.0
NEG_S = -1.0e30
N_BISECT = 18

_ORIG = dict(a_cq=(0, 256), a_ckv=(256, 384), a_kr=(384, 416), b_q=(416, 672), b_k=(672, 800), b_v=(800, 928),
             c_q=(928, 1184), c_k=(1184, 1440), c_v=(1440, 1696), c_qi=(1696, 1952), c_ki=(1952, 1984),
             c_w=(1984, 1992), d_q=(1992, 2248), d_k=(2248, 2504), d_v=(2504, 2760), d_f=(2760, 2764))
_ORDER = ["b_q", "b_k", "c_q", "c_k", "c_qi", "c_ki", "a_kr", "b_v", "c_v", "d_q", "d_k", "d_v",
          "a_cq", "a_ckv", "c_w", "d_f"]
COL = {}
_perm = []
_o = 0
for _n in _ORDER:
    _a, _b = _ORIG[_n]
    COL[_n] = (_o, _o + (_b - _a))
    _perm.extend(range(_a, _b))
    _o += _b - _a
PERM = np.array(_perm, dtype=np.int64)
assert _o == IN_WIDTH


class Res:
    __slots__ = ("name", "w", "r", "sem", "cnt", "psum")

    def __init__(self, name):
        self.name = name
        self.psum = False
        self.w = {}
        self.r = {}
        self.sem = None
        self.cnt = 0


class Tile:
    def __init__(self, t, res):
        self.t = t
        self.res = res

    def __getitem__(self, k):
        return self.t[k]


class Eng:
    def __init__(self, name):
        self.name = name
        self.sem = None
        self.cnt = 0
        self.seen = {}
        self.prog = []


class KB:
    def __init__(self, nc, stack):
        self.nc = nc
        self.stack = stack
        self.gstack = stack
        self.engs = {n: Eng(n) for n in ("pe", "act", "dve", "pool", "sp")}
        self.nsem = 0
        self.sems = []
        for e in self.engs.values():
            e.sem = self.new_sem(e.name)
        self.all_res = []
        self.named = {}
        self.uid = 0

    def new_sem(self, name):
        self.nsem += 1
        h = self.gstack.enter_context(self.nc.semaphore("s%d_%s" % (self.nsem, name)))
        self.sems.append(h)
        return h

    def res(self, name):
        r = Res(name)
        self.all_res.append(r)
        return r

    def sb(self, name, shape, dt):
        self.uid += 1
        t = self.stack.enter_context(self.nc.sbuf_tensor("%s_u%d" % (name, self.uid), list(shape), dt))
        return Tile(t, self.res(name))

    def ps(self, name, shape, dt):
        t = self.gstack.enter_context(self.nc.psum_tensor(name, list(shape), dt))
        r = self.res(name)
        r.psum = True
        return Tile(t, r)

    def barrier(self):
        toks = [(E.sem, E.cnt) for E in self.engs.values() if E.cnt > 0]
        toks += [(sem, cnt) for (sem, cnt) in self.named.values()]
        for E in self.engs.values():
            waits = []
            for (sem, val) in toks:
                k = id(sem)
                if sem is E.sem or E.seen.get(k, 0) >= val:
                    continue
                waits.append((sem, val))
                E.seen[k] = val

            def emit(eng, waits=waits):
                for (s_, v) in waits:
                    eng.wait_ge(s_, v)

            E.prog.append(emit)

    @staticmethod
    def _r(x):
        return x.res if isinstance(x, Tile) else x

    def _deps(self, E, reads, writes, is_dma):
        deps = {}

        def add(d, skip_dma=False):
            for k, (sem, val, isd) in d.items():
                if skip_dma and isd:
                    continue
                if k not in deps or deps[k][1] < val:
                    deps[k] = (sem, val)

        for r in reads:
            add(self._r(r).w)
            if self._r(r).psum:
                add(self._r(r).r)
        for w in writes:
            add(self._r(w).w, skip_dma=is_dma)
            add(self._r(w).r)
        waits = []
        for k, (sem, val) in deps.items():
            if E.seen.get(k, 0) >= val:
                continue
            waits.append((sem, val))
            E.seen[k] = val
        return waits

    def op(self, en, fname, reads=(), writes=(), **kw):
        E = self.engs[en]
        reads = [self._r(x) for x in reads]
        writes = [self._r(x) for x in writes]
        own = id(E.sem)
        waits = self._deps(E, reads, writes, False)
        if en == "pe":
            waits = [(s, v) for (s, v) in waits if id(s) != own]
        if E.cnt >= 60000:
            E.sem = self.new_sem(E.name)
            E.cnt = 0
        E.cnt += 1
        sem, cnt = E.sem, E.cnt
        key = id(sem)
        tok = (sem, cnt, False)

        def emit(eng, waits=waits, fname=fname, kw=kw, sem=sem):
            for (s, v) in waits:
                eng.wait_ge(s, v)
            getattr(eng, fname)(**kw).then_inc(sem, 1)

        E.prog.append(emit)
        for r in reads:
            if key not in r.r or r.r[key][1] < cnt:
                r.r[key] = tok
        for w in writes:
            w.w = {key: tok}
            w.r = {}

    def dma(self, en, out, in_, reads=(), writes=(), owner=None, **kw):
        E = self.engs[en]
        reads = [self._r(x) for x in reads]
        writes = [self._r(x) for x in writes]
        owner = self._r(owner)
        waits = self._deps(E, reads, writes, True)
        okey = owner.name + ("@sw" if en == "pool" else "")
        if okey in self.named:
            sem, cnt = self.named[okey]
        else:
            sem, cnt = self.new_sem("d_" + okey.replace("@", "_")), 0
        cnt += 16
        assert cnt < 65000, okey
        self.named[okey] = (sem, cnt)
        key = id(sem)
        tok = (sem, cnt, True)

        def emit(eng, waits=waits, sem=sem, out=out, in_=in_, kw=kw):
            for (s, v) in waits:
                eng.wait_ge(s, v)
            eng.dma_start(out=out, in_=in_, **kw).then_inc(sem, 16)

        E.prog.append(emit)
        for r in reads:
            r.r[key] = tok
        for w in writes:
            w.w[key] = tok

    def finish(self):
        self.barrier()
        sems = list(self.sems)
        done = self.new_sem("done")
        for n, E in self.engs.items():
            if n == "pool":
                continue
            E.prog.append(lambda eng, done=done: eng.sem_inc(done, 1))

        def emit(eng, sems=sems, done=done):
            eng.wait_ge(done, 4)
            for s_ in sems:
                eng.sem_clear(s_)
            eng.sem_clear(done)

        self.engs["pool"].prog.append(emit)

    def wait_all(self, en, ress):
        E = self.engs[en]
        waits = self._deps(E, [self._r(x) for x in ress], [], False)

        def emit(eng, waits=waits):
            for (s, v) in waits:
                eng.wait_ge(s, v)

        E.prog.append(emit)

    def replay(self):
        nc = self.nc
        with nc.Block() as block:
            @block.sync
            def _(e):
                for f in self.engs["sp"].prog:
                    f(e)

            @block.tensor
            def _(e):
                for f in self.engs["pe"].prog:
                    f(e)

            @block.scalar
            def _(e):
                for f in self.engs["act"].prog:
                    f(e)

            @block.vector
            def _(e):
                for f in self.engs["dve"].prog:
                    f(e)

            @block.gpsimd
            def _(e):
                for f in self.engs["pool"].prog:
                    f(e)


def build_program(S=SEQ, depth=DEPTH, topk=None, debug=False):
    NT = S // 128
    if topk is None:
        topk = min(256, S // 4)
    nc = bass.Bass("TRN2", target_bir_lowering=False)
    stack = ExitStack()
    kb = KB(nc, stack)

    def din(name, shape, dt=F32):
        return nc.dram_tensor(name, list(shape), dt, kind="ExternalInput").ap()

    def dscr(name, shape, dt):
        kind = "ExternalOutput" if debug else "Internal"
        return nc.dram_tensor(name, list(shape), dt, kind=kind).ap()

    L = depth
    x_in = din("x", [S, D_MODEL])
    w_in_d = din("w_in", [L, D_MODEL, IN_WIDTH])
    w_uq_d = din("w_uq", [L, 256, 384])
    w_ukv_d = din("w_ukv", [L, 128, 512])
    w_out_d = din("w_out", [L, 1024, 1024])
    w_up_d = din("w_up", [L, 1024, D_FF])
    w_down_d = din("w_down", [L, D_FF, 1024])
    norm1_d = din("norm1", [L, 1024])
    norm2_d = din("norm2", [L, 1024])
    gq_d = din("gq", [L, 256])
    gkv_d = din("gkv", [L, 128])
    sinks_d = din("sinks", [L, 4])
    foxb_d = din("foxb", [L, 4])
    fnorm_d = din("fnorm", [1, 1024])
    ropet_d = din("ropet", [S, 96])
    cmat_d = din("cmat", [128, 5 * 512])
    out_d = nc.dram_tensor("out", [S, D_MODEL], F32, kind="ExternalOutput").ap()

    xs = [dscr("xs0", [S, D_MODEL], F32), dscr("xs1", [S, D_MODEL], F32)]
    xm_d = dscr("xm", [S, D_MODEL], F32)
    mixed_d = dscr("mixed", [S, D_MODEL], BF16)
    DKA = dict(A=96, B=64, C=64, D=68)
    HK = dict(A=4, B=2, C=4, D=4)
    qT_d = {g: dscr("qT_" + g, [DKA[g], 4, S], BF16) for g in "ABCD"}
    kT_d = {g: dscr("kT_" + g, [DKA[g], HK[g], S], BF16) for g in "ABCD"}
    v_d = {g: dscr("v_" + g, [S, HK[g], 65], BF16) for g in "ABCD"}
    qiT_d = dscr("qiT", [32, 8, S], BF16)
    kiT_d = dscr("kiT", [32, S], BF16)
    wi_d = dscr("wi", [S, 8], F32)

    R = kb.res
    r_xin = R("x_in")
    r_xs = [R("xs0"), R("xs1")]
    r_xm = R("xm")
    r_mixed = R("mixed")
    r_qT = {g: R("qT" + g) for g in "ABCD"}
    r_kT = {g: R("kT" + g) for g in "ABCD"}
    r_v = {g: R("v" + g) for g in "ABCD"}
    r_qiT, r_kiT, r_wi = R("qiT"), R("kiT"), R("wi")
    r_out = R("out")
    r_const = R("constin")

    banks = [kb.ps("bank%d" % i, [128, 512], F32) for i in range(8)]

    cm_f = kb.sb("cm_f", [128, 5 * 512], F32)
    kb.dma("sp", cm_f[:], cmat_d[:, :], reads=[r_const], writes=[cm_f], owner=cm_f)
    ident4 = kb.sb("ident4", [128, 512], BF16)
    maskc4 = kb.sb("maskc4", [128, 512], BF16)
    maskp4 = kb.sb("maskp4", [128, 512], BF16)
    kb.op("dve", "tensor_copy", [cm_f], [ident4], out=ident4[:], in_=cm_f[:, 0:512])
    kb.op("dve", "tensor_copy", [cm_f], [maskc4], out=maskc4[:], in_=cm_f[:, 512:1024])
    kb.op("dve", "tensor_copy", [cm_f], [maskp4], out=maskp4[:], in_=cm_f[:, 1024:1536])
    ident = ident4
    negS = cm_f[:, 1536:1664]
    TRI = cm_f[:, 1664:1792]
    LAST = cm_f[:, 1792:1920]
    identf = cm_f[:, 2048:2176]

    def bcast_load(tile_, src_row_ap, n):
        kb.dma("sp", tile_[:], src_row_ap.to_broadcast([128, n]), reads=[r_const], writes=[tile_], owner=tile_)

    def load_cast_weight(dst_tile, dst_ap_fn, src_ap_fn, nchunks, ncols, stg, engs=("dve", "pool", "act")):
        for c in range(nchunks):
            st = stg[c % len(stg)]
            kb.dma("sp", st[:, 0:ncols], src_ap_fn(c), reads=[r_const], writes=[st], owner=st)
            en = engs[c % len(engs)]
            if en == "act":
                kb.op("act", "activation", [st], [dst_tile], out=dst_ap_fn(c), in_=st[:, 0:ncols], func=AF.Copy)
            else:
                kb.op(en, "tensor_copy", [st], [dst_tile], out=dst_ap_fn(c), in_=st[:, 0:ncols])

    def rmsnorm_rstd(src_tile, src_ap, n, ss, rstd, junk):
        kb.op("act", "activation", [src_tile], [junk, ss], out=junk[:, 0:n], in_=src_ap, func=AF.Square,
              accum_out=ss[:, 0:1])
        kb.op("dve", "tensor_scalar", [ss], [rstd], out=rstd[:, 0:1], in0=ss[:, 0:1], scalar1=1.0 / n,
              scalar2=EPS, op0=ALU.mult, op1=ALU.add)
        kb.op("act", "activation", [rstd], [rstd], out=rstd[:, 0:1], in_=rstd[:, 0:1], func=AF.Sqrt)
        kb.op("dve", "reciprocal", [rstd], [rstd], out=rstd[:, 0:1], in_=rstd[:, 0:1])

    def rope(src_tile, src4, dst_tile, dst4, cos_t, sin_t, nh, half, tmp, tmpb):
        (cos_t, co), (sin_t, so) = cos_t, sin_t
        cb = cos_t[:, co:co + half].unsqueeze(1).unsqueeze(1).to_broadcast([128, nh, 2, half])
        sb_ = sin_t[:, so:so + half].unsqueeze(1).unsqueeze(1).to_broadcast([128, nh, 2, half])
        n = nh * 2 * half
        tc = tmp[:, 0:n].rearrange("p (h two d) -> p h two d", h=nh, two=2)
        ts = tmpb[:, 0:n].rearrange("p (h two d) -> p h two d", h=nh, two=2)
        kb.op("dve", "tensor_tensor", [src_tile, cos_t], [tmp], out=tc, in0=src4, in1=cb, op=ALU.mult)
        kb.op("pool", "tensor_tensor", [src_tile, sin_t], [tmpb], out=ts, in0=src4, in1=sb_, op=ALU.mult)
        kb.op("dve", "tensor_tensor", [tmp, tmpb], [dst_tile], out=dst4[:, :, 0, :], in0=tc[:, :, 0, :],
              in1=ts[:, :, 1, :], op=ALU.subtract)
        kb.op("dve", "tensor_tensor", [tmp, tmpb], [dst_tile], out=dst4[:, :, 1, :], in0=tc[:, :, 1, :],
              in1=ts[:, :, 0, :], op=ALU.add)

    def phase1(l, xsrc_d, r_xsrc):
        with ExitStack() as st1:
            old = kb.stack
            kb.stack = st1
            win = kb.sb("win", [128, 8, IN_WIDTH], BF16)
            wuq = kb.sb("wuq", [128, 2, 384], BF16)
            wukv = kb.sb("wukv", [128, 512], BF16)
            stg = [kb.sb("p1stg%d" % i, [128, IN_WIDTH], F32) for i in range(2)]
            load_cast_weight(win, lambda c: win[:, c, :], lambda c: w_in_d[l, c * 128:(c + 1) * 128, :], 8,
                             IN_WIDTH, stg)
            load_cast_weight(wuq, lambda c: wuq[:, c, :], lambda c: w_uq_d[l, c * 128:(c + 1) * 128, :], 2, 384, stg)
            load_cast_weight(wukv, lambda c: wukv[:, :], lambda c: w_ukv_d[l, :, :], 1, 512, stg)
            g1 = kb.sb("g1", [128, 1024], F32)
            gq = kb.sb("gq", [128, 256], F32)
            gkv = kb.sb("gkv", [128, 128], F32)
            fb = kb.sb("fb", [128, 4], F32)
            bcast_load(g1, norm1_d[l:l + 1, :], 1024)
            bcast_load(gq, gq_d[l:l + 1, :], 256)
            bcast_load(gkv, gkv_d[l:l + 1, :], 128)
            bcast_load(fb, foxb_d[l:l + 1, :], 4)
            nfb = kb.sb("nfb", [128, 4], F32)
            kb.op("dve", "tensor_scalar", [fb], [nfb], out=nfb[:], in0=fb[:], scalar1=-1.0, scalar2=None,
                  op0=ALU.mult)

            xt = [kb.sb("xt%d" % i, [128, 1024], F32) for i in range(2)]
            rt = [kb.sb("rt%d" % i, [128, 96], F32) for i in range(2)]
            junk = kb.sb("junk", [128, 1024], F32)
            ss = kb.sb("ss", [128, 4], F32)
            rstd = kb.sb("rstd", [128, 4], F32)
            hb = kb.sb("hb", [128, 1024], BF16)
            hT = kb.sb("hT", [128, 8, 128], BF16)
            projs = [kb.sb("proj%d" % i, [128, IN_WIDTH], F32) for i in range(2)]
            junk2 = kb.sb("junk2", [128, 384], F32)
            ss2 = kb.sb("ss2", [128, 4], F32)
            rstd2 = kb.sb("rstd2", [128, 4], F32)
            r64 = kb.sb("r64", [128, 14, 64], BF16)
            r32 = kb.sb("r32", [128, 10, 32], BF16)
            tmp = kb.sb("tmp", [128, 896], F32)
            tmpb = kb.sb("tmpb", [128, 896], F32)
            tmp2 = kb.sb("tmp2", [128, 320], F32)
            tmp2b = kb.sb("tmp2b", [128, 320], F32)
            tmp3 = kb.sb("tmp3", [128, 128], F32)
            tmp3a = kb.sb("tmp3a", [128, 128], F32)
            tmp3b = kb.sb("tmp3b", [128, 128], F32)
            cqn = kb.sb("cqn", [128, 384], BF16)
            cqnT = kb.sb("cqnT", [128, 3, 128], BF16)
            qa = kb.sb("qa", [128, 4, 96], BF16)
            ka = kb.sb("ka", [128, 4, 96], BF16)
            qd = kb.sb("qd", [128, 4, 68], BF16)
            kd = kb.sb("kd", [128, 4, 68], BF16)
            vst = {g: [kb.sb("vst%s%d" % (g, i), [128, HK[g], 65], BF16) for i in range(2)] for g in "ABCD"}
            for g in "ABCD":
                for i in range(2):
                    kb.op("pool", "memset", [], [vst[g][i]], ap=vst[g][i][:], constant=1.0)
            kb.op("pool", "memset", [], [qd], ap=qd[:], constant=1.0)
            kb.op("pool", "memset", [], [kd], ap=kd[:], constant=1.0)
            logf = kb.sb("logf", [128, 4], F32)
            cum = [kb.sb("cum%d" % i, [128, 4], F32) for i in range(2)]
            kb.op("pool", "memset", [], [cum[1]], ap=cum[1][:], constant=0.0)
            c8 = kb.sb("c8", [128, 4], F32)
            cp = kb.sb("cp", [128, 3, 4], BF16)
            cr = kb.sb("cr", [128, 4], F32)
            wst = [kb.sb("wst%d" % i, [128, 8], F32) for i in range(2)]
            tst = [[kb.sb("tst%d_%d" % (j, i), [128, 4, 128], BF16) for j in range(9)] for i in range(2)]

            def stage1(tt):
                par = tt % 2
                tok = slice(tt * 128, (tt + 1) * 128)
                proj = projs[par]
                x = xt[par]
                kb.dma("pool", x[:], xsrc_d[tok, :], reads=[r_xsrc], writes=[x], owner=x)
                kb.dma("pool", rt[par][:], ropet_d[tok, :], reads=[r_const], writes=[rt[par]], owner=rt[par])
                rmsnorm_rstd(x, x[:], 1024, ss, rstd, junk)
                kb.op("dve", "scalar_tensor_tensor", [x, rstd, g1], [hb], out=hb[:], in0=x[:], scalar=rstd[:, 0:1],
                      in1=g1[:], op0=ALU.mult, op1=ALU.mult)
                bT = banks[6]
                bTv = bT[:].bitcast(BF16)
                for kc in range(8):
                    kb.op("pe", "transpose", [hb, ident], [bT], out=bTv[:, kc * 128:(kc + 1) * 128],
                          in_=hb[:, kc * 128:(kc + 1) * 128], identity=ident[:, 0:128])
                kb.op("act", "activation", [bT], [hT], out=hT[:].rearrange("p k t -> p (k t)"), in_=bTv,
                      func=AF.Copy)
                for nb in range(6):
                    c0, c1 = nb * 512, min((nb + 1) * 512, IN_WIDTH)
                    for kc in range(8):
                        kb.op("pe", "matmul", [hT, win], [banks[nb]], out=banks[nb][:, 0:c1 - c0], lhsT=hT[:, kc, :],
                              rhs=win[:, kc, c0:c1], start=(kc == 0), stop=(kc == 7))
                    if nb % 2 == 0:
                        kb.op("act", "activation", [banks[nb]], [proj], out=proj[:, c0:c1],
                              in_=banks[nb][:, 0:c1 - c0], func=AF.Copy)
                    else:
                        kb.op("dve", "tensor_copy", [banks[nb]], [proj], out=proj[:, c0:c1],
                              in_=banks[nb][:, 0:c1 - c0])
            def stage2(tt):
                par = tt % 2
                tok = slice(tt * 128, (tt + 1) * 128)
                proj = projs[par]
                c64, s64, c32, s32 = (rt[par], 0), (rt[par], 32), (rt[par], 64), (rt[par], 80)
                bT = banks[7]
                bTv = bT[:].bitcast(BF16)
                rope(proj, proj[:, 0:896].rearrange("p (h two d) -> p h two d", h=14, two=2), r64,
                     r64[:].rearrange("p h (two d) -> p h two d", two=2), c64, s64, 14, 32, tmp, tmpb)
                rope(proj, proj[:, 896:1216].rearrange("p (h two d) -> p h two d", h=10, two=2), r32,
                     r32[:].rearrange("p h (two d) -> p h two d", two=2), c32, s32, 10, 16, tmp2, tmp2b)
                vs = {g: vst[g][par] for g in "ABCD"}
                o = COL["b_v"][0]
                kb.op("pool", "tensor_copy", [proj], [vs["B"]], out=vs["B"][:, :, 0:64],
                      in_=proj[:, o:o + 128].rearrange("p (h d) -> p h d", h=2))
                o = COL["c_v"][0]
                kb.op("act", "activation", [proj], [vs["C"]], out=vs["C"][:, :, 0:64],
                      in_=proj[:, o:o + 256].rearrange("p (h d) -> p h d", h=4), func=AF.Copy)
                o = COL["d_v"][0]
                kb.op("act", "activation", [proj], [vs["D"]], out=vs["D"][:, :, 0:64],
                      in_=proj[:, o:o + 256].rearrange("p (h d) -> p h d", h=4), func=AF.Copy)
                o = COL["a_cq"][0]
                kb.op("act", "activation", [proj], [junk2, ss2], out=junk2[:, 0:256], in_=proj[:, o:o + 256],
                      func=AF.Square, accum_out=ss2[:, 1:2])
                kb.op("act", "activation", [proj], [junk2, ss2], out=junk2[:, 256:384], in_=proj[:, o + 256:o + 384],
                      func=AF.Square, accum_out=ss2[:, 2:3])
                kb.op("dve", "tensor_scalar", [ss2], [rstd2], out=rstd2[:, 1:2], in0=ss2[:, 1:2], scalar1=1.0 / 256,
                      scalar2=EPS, op0=ALU.mult, op1=ALU.add)
                kb.op("dve", "tensor_scalar", [ss2], [rstd2], out=rstd2[:, 2:3], in0=ss2[:, 2:3], scalar1=1.0 / 128,
                      scalar2=EPS, op0=ALU.mult, op1=ALU.add)
                kb.op("act", "activation", [rstd2], [rstd2], out=rstd2[:, 1:3], in_=rstd2[:, 1:3], func=AF.Sqrt)
                kb.op("dve", "reciprocal", [rstd2], [rstd2], out=rstd2[:, 1:3], in_=rstd2[:, 1:3])
                kb.op("dve", "scalar_tensor_tensor", [proj, rstd2, gq], [cqn], out=cqn[:, 0:256],
                      in0=proj[:, o:o + 256], scalar=rstd2[:, 1:2], in1=gq[:], op0=ALU.mult, op1=ALU.mult)
                kb.op("dve", "scalar_tensor_tensor", [proj, rstd2, gkv], [cqn], out=cqn[:, 256:384],
                      in0=proj[:, o + 256:o + 384], scalar=rstd2[:, 2:3], in1=gkv[:], op0=ALU.mult, op1=ALU.mult)
                for kc in range(3):
                    kb.op("pe", "transpose", [cqn, ident], [bT], out=bTv[:, kc * 128:(kc + 1) * 128],
                          in_=cqn[:, kc * 128:(kc + 1) * 128], identity=ident[:, 0:128])
                kb.op("act", "activation", [bT], [cqnT], out=cqnT[:].rearrange("p k t -> p (k t)"),
                      in_=bTv[:, 0:384], func=AF.Copy)
                bq, bkv = banks[0], banks[1]
                for kc in range(2):
                    kb.op("pe", "matmul", [cqnT, wuq], [bq], out=bq[:, 0:384], lhsT=cqnT[:, kc, :], rhs=wuq[:, kc, :],
                          start=(kc == 0), stop=(kc == 1))
                kb.op("pe", "matmul", [cqnT, wukv], [bkv], out=bkv[:, 0:512], lhsT=cqnT[:, 2, :], rhs=wukv[:, :],
                      start=True, stop=True)
                bq3 = bq[:, 0:384].rearrange("p (h d) -> p h d", h=4)
                kb.op("act", "activation", [bq], [qa], out=qa[:, :, 0:64], in_=bq3[:, :, 0:64], func=AF.Copy)
                kb.op("act", "activation", [bq], [tmp3], out=tmp3[:, 0:128].rearrange("p (h d) -> p h d", h=4),
                      in_=bq3[:, :, 64:96], func=AF.Copy)
                rope(tmp3, tmp3[:, 0:128].rearrange("p (h two d) -> p h two d", h=4, two=2), qa,
                     qa[:, :, 64:96].rearrange("p h (two d) -> p h two d", two=2), c32, s32, 4, 16, tmp3a, tmp3b)
                bkv3 = bkv[:, 0:512].rearrange("p (h d) -> p h d", h=4)
                kb.op("act", "activation", [bkv], [ka], out=ka[:, :, 0:64], in_=bkv3[:, :, 0:64], func=AF.Copy)
                kb.op("dve", "tensor_copy", [bkv], [vs["A"]], out=vs["A"][:, :, 0:64], in_=bkv3[:, :, 64:128])
                kb.op("pool", "tensor_copy", [r32], [ka], out=ka[:, :, 64:96],
                      in_=r32[:, 9:10, :].to_broadcast([128, 4, 32]))
                o = COL["d_f"][0]
                kb.op("dve", "scalar_tensor_tensor", [proj, nfb], [logf], out=logf[:], in0=proj[:, o:o + 4],
                      scalar=-1.0, in1=nfb[:], op0=ALU.mult, op1=ALU.add)
                kb.op("act", "activation", [logf], [logf], out=logf[:], in_=logf[:], func=AF.Exp)
                kb.op("act", "activation", [logf], [logf], out=logf[:], in_=logf[:], func=AF.Ln, bias=1.0)
                kb.op("dve", "tensor_scalar", [logf], [logf], out=logf[:], in0=logf[:], scalar1=-1.0, scalar2=None,
                      op0=ALU.mult)
                bc = banks[2]
                cprev, ccur = cum[(tt + 1) % 2], cum[tt % 2]
                kb.op("pe", "matmul", [cm_f, logf], [bc], out=bc[:, 0:4], lhsT=TRI, rhs=logf[:], start=True,
                      stop=False)
                kb.op("pe", "matmul", [cm_f, cprev], [bc], out=bc[:, 0:4], lhsT=LAST, rhs=cprev[:], start=False,
                      stop=True)
                kb.op("dve", "tensor_copy", [bc], [ccur], out=ccur[:], in_=bc[:, 0:4])
                kb.op("dve", "tensor_scalar", [ccur], [c8], out=c8[:], in0=ccur[:], scalar1=8.0, scalar2=None,
                      op0=ALU.mult)
                kb.op("dve", "tensor_copy", [c8], [cp], out=cp[:, 0, :], in_=c8[:])
                kb.op("dve", "tensor_tensor", [c8, cp], [cr], out=cr[:], in0=c8[:], in1=cp[:, 0, :], op=ALU.subtract)
                kb.op("dve", "tensor_copy", [cr], [cp], out=cp[:, 1, :], in_=cr[:])
                kb.op("dve", "tensor_tensor", [cr, cp], [cr], out=cr[:], in0=cr[:], in1=cp[:, 1, :], op=ALU.subtract)
                kb.op("dve", "tensor_copy", [cr], [cp], out=cp[:, 2, :], in_=cr[:])
                o = COL["d_q"][0]
                kb.op("pool", "tensor_copy", [proj], [qd], out=qd[:, :, 0:64],
                      in_=proj[:, o:o + 256].rearrange("p (h d) -> p h d", h=4))
                kb.op("pool", "tensor_copy", [cp], [qd], out=qd[:, :, 64:65], in_=cp[:, 0, :].unsqueeze(2))
                o = COL["d_k"][0]
                kb.op("pool", "tensor_copy", [proj], [kd], out=kd[:, :, 0:64],
                      in_=proj[:, o:o + 256].rearrange("p (h d) -> p h d", h=4))
                kb.op("dve", "tensor_scalar", [cp], [kd], out=kd[:, :, 65:68], in0=cp[:].rearrange("p c h -> p h c"),
                      scalar1=-1.0, scalar2=None, op0=ALU.mult)
                o = COL["c_w"][0]
                ws = wst[par]
                kb.op("pool", "tensor_copy", [proj], [ws], out=ws[:], in_=proj[:, o:o + 8])
                kb.dma("sp", wi_d[tok, :], ws[:], reads=[ws], writes=[r_wi], owner=ws)
                for g in "ABCD":
                    kb.dma("sp", v_d[g][tok, :, :], vs[g][:], reads=[vs[g]], writes=[r_v[g]], owner=vs[g])
                items = [
                    (qa, [qa[:, h, :] for h in range(4)], 96, qT_d["A"], r_qT["A"]),
                    (ka, [ka[:, h, :] for h in range(4)], 96, kT_d["A"], r_kT["A"]),
                    (r64, [r64[:, h, :] for h in range(0, 4)], 64, qT_d["B"], r_qT["B"]),
                    (r64, [r64[:, h, :] for h in range(4, 6)], 64, kT_d["B"], r_kT["B"]),
                    (r64, [r64[:, h, :] for h in range(6, 10)], 64, qT_d["C"], r_qT["C"]),
                    (r64, [r64[:, h, :] for h in range(10, 14)], 64, kT_d["C"], r_kT["C"]),
                    (qd, [qd[:, h, :] for h in range(4)], 68, qT_d["D"], r_qT["D"]),
                    (kd, [kd[:, h, :] for h in range(4)], 68, kT_d["D"], r_kT["D"]),
                    (r32, [r32[:, h, :] for h in range(0, 4)], 32, qiT_d[:, 0:4, :], r_qiT),
                    (r32, [r32[:, h, :] for h in range(4, 8)], 32, qiT_d[:, 4:8, :], r_qiT),
                    (r32, [r32[:, 8, :]], 32, None, r_kiT),
                ]
                for ii, (src, aps, n, dst, rdst) in enumerate(items):
                    bk = banks[3 + (ii % 3)]
                    bkv_ = bk[:].bitcast(BF16)
                    nh = len(aps)
                    for h, ap in enumerate(aps):
                        kb.op("pe", "transpose", [src, ident], [bk], out=bkv_[0:n, h * 128:(h + 1) * 128], in_=ap,
                              identity=ident[:, 0:128])
                    sg = tst[par][ii % 9] if ii < 9 else tst[par][ii - 9 + 0]
                    en = "act" if ii % 2 == 0 else "dve"
                    if en == "act":
                        kb.op("act", "activation", [bk], [sg], out=sg[0:n, 0:nh, :].rearrange("p h t -> p (h t)"),
                              in_=bkv_[0:n, 0:nh * 128], func=AF.Copy)
                    else:
                        kb.op("dve", "tensor_copy", [bk], [sg], out=sg[0:n, 0:nh, :].rearrange("p h t -> p (h t)"),
                              in_=bkv_[0:n, 0:nh * 128])
                    if dst is None:
                        kb.dma("sp", kiT_d[:, tok], sg[0:32, 0, :], reads=[sg], writes=[rdst], owner=sg)
                    else:
                        kb.dma("sp", dst[:, :, tok], sg[0:n, 0:nh, :], reads=[sg], writes=[rdst], owner=sg)
            stage1(0)
            for tt in range(NT):
                if tt + 1 < NT:
                    stage1(tt + 1)
                stage2(tt)
            kb.barrier()
            kb.stack = old

    def attention(l, g):
        dk = DKA[g]
        hk = HK[g]
        gi = "ABCD".index(g)
        scale = {"A": 96 ** -0.5, "B": 0.125, "C": 0.125, "D": 0.125}[g]
        with ExitStack() as st2:
            old = kb.stack
            kb.stack = st2
            qT = kb.sb("aqT", [dk, 4, S], BF16)
            kT = kb.sb("akT", [dk, hk, S], BF16)
            vv = kb.sb("avv", [128, NT, hk * 65], BF16)
            for h in range(4):
                kb.dma("sp", qT[:, h, :], qT_d[g][:, h, :], reads=[r_qT[g]], writes=[qT], owner=qT)
            for h in range(hk):
                kb.dma("sp", kT[:, h, :], kT_d[g][:, h, :], reads=[r_kT[g]], writes=[kT], owner=kT)
            vsrc = v_d[g].rearrange("(j p) h d -> p j (h d)", p=128)
            for j0 in range(0, NT, 4):
                kb.dma("sp", vv[:, j0:j0 + 4, :], vsrc[:, j0:j0 + 4, :], reads=[r_v[g]], writes=[vv], owner=vv)
            pT = [kb.sb("apT%d" % i, [128, 512], BF16) for i in range(2)]
            osb = [kb.sb("aosb%d" % i, [128, 4, 64], BF16) for i in range(2)]
            den = kb.sb("aden", [128, 4], F32)
            if g == "B":
                esink = kb.sb("esink", [128, 4], F32)
                bcast_load(esink, sinks_d[l:l + 1, :], 4)
                kb.op("act", "activation", [esink], [esink], out=esink[:], in_=esink[:], func=AF.Exp)
            if g == "C":
                qiTs = [kb.sb("cqiT%d" % i, [32, 8, 128], BF16) for i in range(2)]
                kiT = kb.sb("ckiT", [32, S], BF16)
                kb.dma("sp", kiT[:], kiT_d[:, :], reads=[r_kiT], writes=[kiT], owner=kiT)
                wsb = kb.sb("cwsb", [128, NT, 8], F32)
                wsrc = wi_d.rearrange("(j p) h -> p j h", p=128)
                for j0 in range(0, NT, 4):
                    kb.dma("sp", wsb[:, j0:j0 + 4, :], wsrc[:, j0:j0 + 4, :], reads=[r_wi], writes=[wsb], owner=wsb)
                Isbs = [kb.sb("cI%d" % i, [128, S], F32) for i in range(2)]
                Madd = [kb.sb("cMadd%d" % i, [128, S], BF16) for i in range(2)]
                rl = [kb.sb("crl%d" % i, [128, 512], BF16) for i in range(3)]
                dgs = [kb.sb("cdg%d" % i, [128, 8, 128], BF16) for i in range(2)]
                cjunk = kb.sb("cjunk", [128, S], BF16)
                lo = kb.sb("clo", [128, 1], F32)
                stp = kb.sb("cstp", [128, 1], F32)
                cand = kb.sb("ccand", [128, 1], F32)
                cnt = kb.sb("ccnt", [128, 1], F32)
                mm = kb.sb("cmm", [128, 1], F32)
                hi = kb.sb("chi", [128, 1], F32)

            def kts_of(qt):
                if g == "B":
                    return [kt for kt in (qt - 1, qt) if kt >= 0]
                return list(range(qt + 1))

            def c_scores(qt):
                qs = slice(qt * 128, (qt + 1) * 128)
                Lk = 128 * (qt + 1)
                nblk = (Lk + 511) // 512
                Isb = Isbs[qt % 2]
                qiT = qiTs[qt % 2]
                dg = dgs[qt % 2]
                kb.dma("pool", qiT[:], qiT_d[:, :, qs], reads=[r_qiT], writes=[qiT], owner=qiT)
                kb.op("pool", "tensor_tensor", [ident4, wsb], [dg], out=dg[:],
                      in0=ident4[:, 0:128].unsqueeze(1).to_broadcast([128, 8, 128]),
                      in1=wsb[:, qt, :].unsqueeze(2).to_broadcast([128, 8, 128]), op=ALU.mult)
                hc = 0
                for kbk in range(nblk):
                    k0, k1 = kbk * 512, min((kbk + 1) * 512, Lk)
                    n = k1 - k0
                    bacc = banks[6 + (kbk % 2)]
                    pend = None
                    for hh in range(8):
                        bi = banks[4 + (hc % 2)]
                        r_ = rl[hc % 3]
                        kb.op("pe", "matmul", [qiT, kiT], [bi], out=bi[:, 0:n], lhsT=qiT[:, hh, :],
                              rhs=kiT[:, k0:k1], start=True, stop=True)
                        kb.op("act", "activation", [bi], [r_], out=r_[:, 0:n], in_=bi[:, 0:n], func=AF.Relu)
                        if pend is not None:
                            ph, pr = pend
                            kb.op("pe", "matmul", [dg, pr], [bacc], out=bacc[:, 0:n], lhsT=dg[:, ph, :], rhs=pr[:, 0:n],
                                  start=(ph == 0), stop=False)
                        pend = (hh, r_)
                        hc += 1
                    ph, pr = pend
                    kb.op("pe", "matmul", [dg, pr], [bacc], out=bacc[:, 0:n], lhsT=dg[:, ph, :], rhs=pr[:, 0:n],
                          start=False, stop=True)
                    kb.op("act", "activation", [bacc], [Isb], out=Isb[:, k0:k1], in_=bacc[:, 0:n], func=AF.Copy)

            def c_select(qt):
                qs = slice(qt * 128, (qt + 1) * 128)
                Lk = 128 * (qt + 1)
                Isb = Isbs[qt % 2]
                madd = Madd[qt % 2]
                kb.op("dve", "tensor_tensor", [Isb, cm_f], [Isb], out=Isb[:, qs], in0=Isb[:, qs], in1=negS, op=ALU.add)
                if Lk <= topk:
                    kb.op("dve", "tensor_scalar", [Isb], [madd], out=madd[:, 0:Lk], in0=Isb[:, 0:Lk],
                          scalar1=-1.0e29, scalar2=-BIGM, op0=ALU.is_lt, op1=ALU.mult)
                    return
                assert Lk - 128 >= topk
                kb.op("dve", "tensor_reduce", [Isb], [hi], out=hi[:], in_=Isb[:, 0:Lk], axis=AX.X, op=ALU.max)
                kb.op("dve", "tensor_reduce", [Isb], [lo], out=lo[:], in_=Isb[:, 0:Lk - 128], axis=AX.X, op=ALU.min)
                kb.op("dve", "tensor_tensor", [hi, lo], [stp], out=stp[:], in0=hi[:], in1=lo[:], op=ALU.subtract)
                for it in range(N_BISECT):
                    f = 0.5 ** (it + 1)
                    kb.op("dve", "scalar_tensor_tensor", [stp, lo], [cand], out=cand[:], in0=stp[:], scalar=f,
                          in1=lo[:], op0=ALU.mult, op1=ALU.add)
                    kb.op("dve", "tensor_scalar", [Isb, cand], [cjunk, cnt], out=cjunk[:, 0:Lk], in0=Isb[:, 0:Lk],
                          scalar1=cand[:, 0:1], scalar2=None, op0=ALU.is_ge, op1=ALU.add, accum_out=cnt[:, 0:1])
                    kb.op("dve", "scalar_tensor_tensor", [cnt, stp], [mm], out=mm[:], in0=cnt[:],
                          scalar=float(topk) - 0.5, in1=stp[:], op0=ALU.is_ge, op1=ALU.mult)
                    kb.op("dve", "scalar_tensor_tensor", [mm, lo], [lo], out=lo[:], in0=mm[:], scalar=f, in1=lo[:],
                          op0=ALU.mult, op1=ALU.add)
                kb.op("dve", "tensor_scalar", [Isb, lo], [madd], out=madd[:, 0:Lk], in0=Isb[:, 0:Lk],
                      scalar1=lo[:, 0:1], scalar2=-BIGM, op0=ALU.is_lt, op1=ALU.mult)

            def emit_scores(qt, ki, kt):
                qs = slice(qt * 128, (qt + 1) * 128)
                ks = slice(kt * 128, (kt + 1) * 128)
                bs = banks[ki % 2]
                have_mask = False
                if g == "C":
                    madd = Madd[qt % 2]
                    kb.op("pe", "matmul", [madd, ident4], [bs], out=bs[:, 0:512], lhsT=madd[:, ks],
                          rhs=ident4[:, 0:512], start=True, stop=False, skip_group_check=True)
                    have_mask = True
                elif kt == qt:
                    kb.op("pe", "matmul", [ident4, maskc4], [bs], out=bs[:, 0:512], lhsT=ident4[:, 0:128],
                          rhs=maskc4[:, 0:512], start=True, stop=False, skip_group_check=True)
                    have_mask = True
                elif g == "B":
                    kb.op("pe", "matmul", [ident4, maskp4], [bs], out=bs[:, 0:512], lhsT=ident4[:, 0:128],
                          rhs=maskp4[:, 0:512], start=True, stop=False, skip_group_check=True)
                    have_mask = True
                for h in range(4):
                    kvh = h if hk == 4 else h // 2
                    kb.op("pe", "matmul", [kT, qT], [bs], out=bs[:, h * 128:(h + 1) * 128], lhsT=kT[:, kvh, ks],
                          rhs=qT[:, h, qs], start=((not have_mask) and h == 0), stop=True, skip_group_check=True)
                p = pT[ki % 2]
                kb.op("act", "activation", [bs], [p], out=p[:], in_=bs[:, 0:512], func=AF.Exp, scale=scale)

            def emit_pv(qt, ki, kt, nk):
                bo = banks[2 + (qt % 2)]
                p = pT[ki % 2]
                for h in range(4):
                    kvh = h if hk == 4 else h // 2
                    kb.op("pe", "matmul", [p, vv], [bo], out=bo[:, h * 65:(h + 1) * 65],
                          lhsT=p[:, h * 128:(h + 1) * 128], rhs=vv[:, kt, kvh * 65:(kvh + 1) * 65],
                          start=(ki == 0 and h == 0), stop=(ki == nk - 1), skip_group_check=True)

            def emit_attn(qt):
                kts = kts_of(qt)
                prev = None
                for ki, kt in enumerate(kts):
                    emit_scores(qt, ki, kt)
                    if prev is not None:
                        emit_pv(qt, prev[0], prev[1], len(kts))
                    prev = (ki, kt)
                emit_pv(qt, prev[0], prev[1], len(kts))

            def emit_norm(qt):
                qs = slice(qt * 128, (qt + 1) * 128)
                bo = banks[2 + (qt % 2)]
                bo3 = bo[:, 0:260].rearrange("p (h d) -> p h d", h=4)
                if g == "B":
                    kb.op("dve", "tensor_tensor", [bo, esink], [den], out=den[:].unsqueeze(2), in0=bo3[:, :, 64:65],
                          in1=esink[:].unsqueeze(2), op=ALU.add)
                else:
                    kb.op("dve", "tensor_copy", [bo], [den], out=den[:].unsqueeze(2), in_=bo3[:, :, 64:65])
                kb.op("dve", "reciprocal", [den], [den], out=den[:], in_=den[:])
                ob = osb[qt % 2]
                kb.op("dve", "tensor_tensor", [bo, den], [ob], out=ob[:], in0=bo3[:, :, 0:64],
                      in1=den[:].unsqueeze(2).to_broadcast([128, 4, 64]), op=ALU.mult)
                kb.dma("sp", mixed_d[qs, gi * 256:(gi + 1) * 256], ob[:].rearrange("p h d -> p (h d)"), reads=[ob],
                       writes=[r_mixed], owner=ob)

            if g == "C":
                c_scores(0)
                for qt in range(NT):
                    if qt + 1 < NT:
                        c_scores(qt + 1)
                    c_select(qt)
                    if qt > 0:
                        emit_norm(qt - 1)
                    emit_attn(qt)
                emit_norm(NT - 1)
            else:
                for qt in range(NT):
                    emit_attn(qt)
                    emit_norm(qt)
            kb.barrier()
            kb.stack = old

    def phase3a(l, xsrc_d, r_xsrc):
        with ExitStack() as st3:
            old = kb.stack
            kb.stack = st3
            wo = kb.sb("wo", [128, 8, 1024], BF16)
            stg = [kb.sb("p3stg%d" % i, [128, 1024], F32) for i in range(2)]
            load_cast_weight(wo, lambda c: wo[:, c, :], lambda c: w_out_d[l, c * 128:(c + 1) * 128, :], 8, 1024, stg)
            mx = [kb.sb("mx%d" % i, [128, 1024], BF16) for i in range(2)]
            mT = [kb.sb("mT%d" % i, [128, 8, 128], BF16) for i in range(2)]
            xt = [kb.sb("x3t%d" % i, [128, 1024], F32) for i in range(2)]
            xo = [kb.sb("x3o%d" % i, [128, 1024], F32) for i in range(2)]
            def sA(tt):
                par = tt % 2
                tok = slice(tt * 128, (tt + 1) * 128)
                kb.dma("pool", mx[par][:], mixed_d[tok, :], reads=[r_mixed], writes=[mx[par]], owner=mx[par])
                kb.dma("pool", xt[par][:], xsrc_d[tok, :], reads=[r_xsrc], writes=[xt[par]], owner=xt[par])
                bT = banks[4 + par]
                bTv = bT[:].bitcast(BF16)
                for kc in range(8):
                    kb.op("pe", "transpose", [mx[par], ident], [bT], out=bTv[:, kc * 128:(kc + 1) * 128],
                          in_=mx[par][:, kc * 128:(kc + 1) * 128], identity=ident[:, 0:128])
                kb.op("act", "activation", [bT], [mT[par]], out=mT[par][:].rearrange("p k t -> p (k t)"), in_=bTv,
                      func=AF.Copy)

            def sB(tt):
                par = tt % 2
                tok = slice(tt * 128, (tt + 1) * 128)
                for nb in range(2):
                    bk = banks[2 * par + nb]
                    for kc in range(8):
                        kb.op("pe", "matmul", [mT[par], wo], [bk], out=bk[:, 0:512], lhsT=mT[par][:, kc, :],
                              rhs=wo[:, kc, nb * 512:(nb + 1) * 512], start=(kc == 0), stop=(kc == 7))
                    kb.op("dve", "tensor_tensor", [bk, xt[par]], [xo[par]], out=xo[par][:, nb * 512:(nb + 1) * 512],
                          in0=bk[:, 0:512], in1=xt[par][:, nb * 512:(nb + 1) * 512], op=ALU.add)
                kb.dma("sp", xm_d[tok, :], xo[par][:], reads=[xo[par]], writes=[r_xm], owner=xo[par])

            sA(0)
            for tt in range(NT):
                if tt + 1 < NT:
                    sA(tt + 1)
                sB(tt)
            kb.barrier()
            kb.stack = old

    def phase3b(l, xdst_d, r_xdst, final):
        with ExitStack() as st4:
            old = kb.stack
            kb.stack = st4
            wu = kb.sb("wu", [128, 8, D_FF], BF16)
            wd = kb.sb("wd", [128, 32, 1024], BF16)
            stg = [kb.sb("p4stg%d" % i, [128, 2048], F32) for i in range(2)]
            load_cast_weight(wu, lambda c: wu[:, c // 2, (c % 2) * 2048:(c % 2 + 1) * 2048],
                             lambda c: w_up_d[l, (c // 2) * 128:(c // 2 + 1) * 128, (c % 2) * 2048:(c % 2 + 1) * 2048],
                             16, 2048, stg)
            load_cast_weight(wd, lambda c: wd[:, c, :], lambda c: w_down_d[l, c * 128:(c + 1) * 128, :], 32, 1024,
                             stg)
            g2 = kb.sb("g2", [128, 1024], F32)
            bcast_load(g2, norm2_d[l:l + 1, :], 1024)
            if final:
                gf = kb.sb("gf", [128, 1024], F32)
                bcast_load(gf, fnorm_d[0:1, :], 1024)
            TB = 2
            xt = [[kb.sb("x4t%d_%d" % (i, j), [128, 1024], F32) for j in range(TB)] for i in range(2)]
            hb = kb.sb("h4b", [128, 1024], BF16)
            hT = [kb.sb("h4T%d" % i, [128, 8, TB * 128], BF16) for i in range(2)]
            uT = [kb.sb("u4T%d" % i, [128, TB * 128], BF16) for i in range(3)]
            rT = [kb.sb("r4T%d" % i, [128, TB * 128], F32) for i in range(2)]
            junk = kb.sb("junk4", [128, 1024], F32)
            ss = kb.sb("ss4", [128, 2], F32)
            rstd = kb.sb("rstd4", [128, 2], F32)
            nblk = NT // TB
            def pre(b):
                par = b % 2
                for j in range(TB):
                    tok = slice((b * TB + j) * 128, (b * TB + j + 1) * 128)
                    x = xt[par][j]
                    kb.dma("pool", x[:], xm_d[tok, :], reads=[r_xm], writes=[x], owner=x)
                    rmsnorm_rstd(x, x[:], 1024, ss, rstd, junk)
                    kb.op("dve", "scalar_tensor_tensor", [x, rstd, g2], [hb], out=hb[:], in0=x[:],
                          scalar=rstd[:, 0:1], in1=g2[:], op0=ALU.mult, op1=ALU.mult)
                    bT = banks[6 + j % 2]
                    bTv = bT[:].bitcast(BF16)
                    for kc in range(8):
                        kb.op("pe", "transpose", [hb, ident], [bT], out=bTv[:, kc * 128:(kc + 1) * 128],
                              in_=hb[:, kc * 128:(kc + 1) * 128], identity=ident[:, 0:128])
                    kb.op("act", "activation", [bT], [hT[par]], out=hT[par][:, :, j * 128:(j + 1) * 128],
                          in_=bTv.rearrange("p (k t) -> p k t", k=8), func=AF.Copy)
            def main(b):
                par = b % 2
                accs = [banks[0], banks[1], banks[2], banks[3]]
                def emit_up(fc):
                    bu = banks[4 + fc % 2]
                    for kc in range(8):
                        kb.op("pe", "matmul", [wu, hT[par]], [bu], out=bu[:, 0:TB * 128],
                              lhsT=wu[:, kc, fc * 128:(fc + 1) * 128], rhs=hT[par][:, kc, :], start=(kc == 0),
                              stop=(kc == 7))
                    u = uT[fc % 3]
                    rr = rT[fc % 2]
                    kb.op("act", "activation", [bu], [rr], out=rr[:], in_=bu[:, 0:TB * 128], func=AF.Relu)
                    kb.op("dve" if fc % 2 == 0 else "pool", "tensor_tensor", [rr], [u], out=u[:], in0=rr[:],
                          in1=rr[:], op=ALU.mult)

                def emit_down(fc):
                    u = uT[fc % 3]
                    for j in range(TB):
                        for nb in range(2):
                            acc = accs[j * 2 + nb]
                            kb.op("pe", "matmul", [u, wd], [acc], out=acc[:, 0:512], lhsT=u[:, j * 128:(j + 1) * 128],
                                  rhs=wd[:, fc, nb * 512:(nb + 1) * 512], start=(fc == 0), stop=(fc == 31))

                emit_up(0)
                for fc in range(32):
                    if fc + 1 < 32:
                        emit_up(fc + 1)
                    emit_down(fc)
                for j in range(TB):
                    tok = slice((b * TB + j) * 128, (b * TB + j + 1) * 128)
                    o = xt[par][j]
                    for nb in range(2):
                        acc = accs[j * 2 + nb]
                        kb.op("dve", "tensor_tensor", [acc, xt[par][j]], [o], out=o[:, nb * 512:(nb + 1) * 512],
                              in0=acc[:, 0:512], in1=xt[par][j][:, nb * 512:(nb + 1) * 512], op=ALU.add)
                    if final:
                        kb.op("act", "activation", [o], [junk, ss], out=junk[:], in_=o[:], func=AF.Square,
                              accum_out=ss[:, 1:2])
                        kb.op("dve", "tensor_scalar", [ss], [rstd], out=rstd[:, 1:2], in0=ss[:, 1:2],
                              scalar1=1.0 / 1024, scalar2=EPS, op0=ALU.mult, op1=ALU.add)
                        kb.op("act", "activation", [rstd], [rstd], out=rstd[:, 1:2], in_=rstd[:, 1:2], func=AF.Sqrt)
                        kb.op("dve", "reciprocal", [rstd], [rstd], out=rstd[:, 1:2], in_=rstd[:, 1:2])
                        kb.op("dve", "scalar_tensor_tensor", [o, rstd, gf], [o], out=o[:], in0=o[:],
                              scalar=rstd[:, 1:2], in1=gf[:], op0=ALU.mult, op1=ALU.mult)
                    kb.dma("sp", xdst_d[tok, :], o[:], reads=[o], writes=[r_xdst], owner=o)

            pre(0)
            for b in range(nblk):
                if b + 1 < nblk:
                    pre(b + 1)
                main(b)
            kb.barrier()
            kb.stack = old

    cur_d, cur_r = x_in, r_xin
    for l in range(depth):
        phase1(l, cur_d, cur_r)
        for g in "ABCD":
            attention(l, g)
        phase3a(l, cur_d, cur_r)
        last = (l == depth - 1)
        if last:
            phase3b(l, out_d, r_out, True)
        else:
            phase3b(l, xs[l % 2], r_xs[l % 2], False)
            cur_d, cur_r = xs[l % 2], r_xs[l % 2]
    kb.wait_all("sp", [r_out])
    kb.wait_all("pool", [r_out])
    kb.barrier()
    print("KB: nsem=%d instr=%s" % (kb.nsem, {n: len(E.prog) for n, E in kb.engs.items()}), flush=True)
    kb.replay()
    stack.close()
    return nc


def host_consts(S):
    pos = np.arange(S, dtype=np.float32)

    def tables(d):
        half = d // 2
        inv = (1.0 / (10000.0 ** (np.arange(0, half, dtype=np.float32) * 2.0 / d))).astype(np.float32)
        ang = pos[:, None] * inv[None, :]
        return np.cos(ang).astype(np.float32), np.sin(ang).astype(np.float32)

    c64, s64 = tables(64)
    c32, s32 = tables(32)
    cm = np.zeros((128, 5 * 512), np.float32)
    eye = np.eye(128, dtype=np.float32)
    kk = np.arange(128)[:, None]
    qq = np.arange(128)[None, :]
    mc = np.where(kk > qq, -BIGM, 0.0).astype(np.float32)
    mp = np.where(kk <= qq, -BIGM, 0.0).astype(np.float32)
    for h in range(4):
        cm[:, h * 128:(h + 1) * 128] = eye
        cm[:, 512 + h * 128:512 + (h + 1) * 128] = mc
        cm[:, 1024 + h * 128:1024 + (h + 1) * 128] = mp
    cm[:, 1536:1664] = np.where(qq > kk, NEG_S, 0.0)
    cm[:, 1664:1792] = (kk <= qq).astype(np.float32)
    cm[:, 1792:1920] = (kk == 127).astype(np.float32) * np.ones((1, 128), np.float32)
    cm[:, 2048:2176] = eye
    return c64, s64, c32, s32, cm


_CACHE = {}


def kernel(x, norm1, w_in, mla_q_norm, mla_kv_norm, mla_w_uq, mla_w_ukv, swa_sinks, fox_b_f, w_out, norm2, w_up,
           w_down, final_norm, _depth=None, _ncores=None, _debug=False):
    x = np.asarray(x, dtype=np.float32)
    B, S, _ = x.shape
    depth = int(_depth) if _depth is not None else int(np.asarray(w_in).shape[0])
    ncores = int(_ncores) if _ncores is not None else B
    f = lambda a: np.ascontiguousarray(np.asarray(a, dtype=np.float32))
    key = (S, depth, _debug)
    if key not in _CACHE:
        _CACHE[key] = build_program(S=S, depth=depth, debug=_debug)
    nc = _CACHE[key]
    c64, s64, c32, s32, cm = host_consts(S)
    shared = {
        "w_in": f(np.asarray(w_in)[:depth][:, :, PERM]),
        "w_uq": f(np.asarray(mla_w_uq)[:depth]),
        "w_ukv": f(np.asarray(mla_w_ukv)[:depth]),
        "w_out": f(np.asarray(w_out)[:depth]),
        "w_up": f(np.asarray(w_up)[:depth]),
        "w_down": f(np.asarray(w_down)[:depth]),
        "norm1": f(np.asarray(norm1)[:depth]),
        "norm2": f(np.asarray(norm2)[:depth]),
        "gq": f(np.asarray(mla_q_norm)[:depth]),
        "gkv": f(np.asarray(mla_kv_norm)[:depth]),
        "sinks": f(np.asarray(swa_sinks)[:depth]),
        "foxb": f(np.asarray(fox_b_f)[:depth]),
        "fnorm": f(np.asarray(final_norm).reshape(1, -1)),
        "ropet": np.ascontiguousarray(np.concatenate([c64, s64, c32, s32], axis=1)), "cmat": cm,
    }
    in_maps = []
    for b in range(ncores):
        m = dict(shared)
        m["x"] = f(x[b])
        in_maps.append(m)
    res = run_bass_kernel_spmd(nc, in_maps, core_ids=list(range(ncores)))
    out = np.stack([np.asarray(r["out"], dtype=np.float32) for r in res.results], axis=0)
    if _debug:
        return out, res.results
    return out
```

```python
import numpy as np
from contextlib import ExitStack
import concourse.bass as bass
import concourse.mybir as mybir
from concourse.bass_utils import run_bass_kernel_spmd

F32 = mybir.dt.float32
BF16 = mybir.dt.bfloat16
ALU = mybir.AluOpType
AF = mybir.ActivationFunctionType
AX = mybir.AxisListType

D_MODEL = 1024
DEPTH = 4
SEQ = 4096
IN_WIDTH = 2764
D_FF = 4096
EPS = 1e-6
BIGM = 262144.0
NEG_S = -1.0e30
N_BISECT = 15

_ORIG = dict(a_cq=(0, 256), a_ckv=(256, 384), a_kr=(384, 416), b_q=(416, 672), b_k=(672, 800), b_v=(800, 928),
             c_q=(928, 1184), c_k=(1184, 1440), c_v=(1440, 1696), c_qi=(1696, 1952), c_ki=(1952, 1984),
             c_w=(1984, 1992), d_q=(1992, 2248), d_k=(2248, 2504), d_v=(2504, 2760), d_f=(2760, 2764))
_ORDER = ["b_q", "b_k", "c_q", "c_k", "c_qi", "c_ki", "a_kr", "b_v", "c_v", "d_q", "d_k", "d_v",
          "a_cq", "a_ckv", "c_w", "d_f"]
COL = {}
_perm = []
_o = 0
for _n in _ORDER:
    _a, _b = _ORIG[_n]
    COL[_n] = (_o, _o + (_b - _a))
    _perm.extend(range(_a, _b))
    _o += _b - _a
PERM = np.array(_perm, dtype=np.int64)
assert _o == IN_WIDTH


class Res:
    __slots__ = ("name", "w", "r", "sem", "cnt", "psum")

    def __init__(self, name):
        self.name = name
        self.psum = False
        self.w = {}
        self.r = {}
        self.sem = None
        self.cnt = 0


class Tile:
    def __init__(self, t, res):
        self.t = t
        self.res = res

    def __getitem__(self, k):
        return self.t[k]


class Eng:
    def __init__(self, name):
        self.name = name
        self.sem = None
        self.cnt = 0
        self.seen = {}
        self.prog = []


class KB:
    def __init__(self, nc, stack):
        self.nc = nc
        self.stack = stack
        self.gstack = stack
        self.engs = {n: Eng(n) for n in ("pe", "act", "dve", "pool", "sp")}
        self.nsem = 0
        self.sems = []
        for e in self.engs.values():
            e.sem = self.new_sem(e.name)
        self.all_res = []
        self.named = {}
        self.uid = 0

    def new_sem(self, name):
        self.nsem += 1
        h = self.gstack.enter_context(self.nc.semaphore("s%d_%s" % (self.nsem, name)))
        self.sems.append(h)
        return h

    def res(self, name):
        r = Res(name)
        self.all_res.append(r)
        return r

    def sb(self, name, shape, dt):
        self.uid += 1
        t = self.stack.enter_context(self.nc.sbuf_tensor("%s_u%d" % (name, self.uid), list(shape), dt))
        return Tile(t, self.res(name))

    def ps(self, name, shape, dt):
        t = self.gstack.enter_context(self.nc.psum_tensor(name, list(shape), dt))
        r = self.res(name)
        r.psum = True
        return Tile(t, r)

    def barrier(self):
        toks = [(E.sem, E.cnt) for E in self.engs.values() if E.cnt > 0]
        toks += [(sem, cnt) for (sem, cnt) in self.named.values()]
        for E in self.engs.values():
            waits = []
            for (sem, val) in toks:
                k = id(sem)
                if sem is E.sem or E.seen.get(k, 0) >= val:
                    continue
                waits.append((sem, val))
                E.seen[k] = val

            def emit(eng, waits=waits):
                for (s_, v) in waits:
                    eng.wait_ge(s_, v)

            E.prog.append(emit)

    @staticmethod
    def _r(x):
        return x.res if isinstance(x, Tile) else x

    def _deps(self, E, reads, writes, is_dma):
        deps = {}

        def add(d, skip_dma=False):
            for k, (sem, val, isd) in d.items():
                if skip_dma and isd:
                    continue
                if k not in deps or deps[k][1] < val:
                    deps[k] = (sem, val)

        for r in reads:
            add(self._r(r).w)
            if self._r(r).psum:
                add(self._r(r).r)
        for w in writes:
            add(self._r(w).w, skip_dma=is_dma)
            add(self._r(w).r)
        waits = []
        for k, (sem, val) in deps.items():
            if E.seen.get(k, 0) >= val:
                continue
            waits.append((sem, val))
            E.seen[k] = val
        return waits

    def op(self, en, fname, reads=(), writes=(), **kw):
        E = self.engs[en]
        reads = [self._r(x) for x in reads]
        writes = [self._r(x) for x in writes]
        own = id(E.sem)
        waits = self._deps(E, reads, writes, False)
        if en == "pe":
            waits = [(s, v) for (s, v) in waits if id(s) != own]
        if E.cnt >= 60000:
            E.sem = self.new_sem(E.name)
            E.cnt = 0
        E.cnt += 1
        sem, cnt = E.sem, E.cnt
        key = id(sem)
        tok = (sem, cnt, False)

        def emit(eng, waits=waits, fname=fname, kw=kw, sem=sem):
            for (s, v) in waits:
                eng.wait_ge(s, v)
            getattr(eng, fname)(**kw).then_inc(sem, 1)

        E.prog.append(emit)
        for r in reads:
            if key not in r.r or r.r[key][1] < cnt:
                r.r[key] = tok
        for w in writes:
            w.w = {key: tok}
            w.r = {}

    def dma(self, en, out, in_, reads=(), writes=(), owner=None, **kw):
        E = self.engs[en]
        reads = [self._r(x) for x in reads]
        writes = [self._r(x) for x in writes]
        owner = self._r(owner)
        waits = self._deps(E, reads, writes, True)
        okey = owner.name + ("@sw" if en == "pool" else "")
        if okey in self.named:
            sem, cnt = self.named[okey]
        else:
            sem, cnt = self.new_sem("d_" + okey.replace("@", "_")), 0
        cnt += 16
        assert cnt < 65000, okey
        self.named[okey] = (sem, cnt)
        key = id(sem)
        tok = (sem, cnt, True)

        def emit(eng, waits=waits, sem=sem, out=out, in_=in_, kw=kw):
            for (s, v) in waits:
                eng.wait_ge(s, v)
            eng.dma_start(out=out, in_=in_, **kw).then_inc(sem, 16)

        E.prog.append(emit)
        for r in reads:
            r.r[key] = tok
        for w in writes:
            w.w[key] = tok

    def finish(self):
        self.barrier()
        sems = list(self.sems)
        done = self.new_sem("done")
        for n, E in self.engs.items():
            if n == "pool":
                continue
            E.prog.append(lambda eng, done=done: eng.sem_inc(done, 1))

        def emit(eng, sems=sems, done=done):
            eng.wait_ge(done, 4)
            for s_ in sems:
                eng.sem_clear(s_)
            eng.sem_clear(done)

        self.engs["pool"].prog.append(emit)

    def wait_all(self, en, ress):
        E = self.engs[en]
        waits = self._deps(E, [self._r(x) for x in ress], [], False)

        def emit(eng, waits=waits):
            for (s, v) in waits:
                eng.wait_ge(s, v)

        E.prog.append(emit)

    def replay(self):
        nc = self.nc
        with nc.Block() as block:
            @block.sync
            def _(e):
                for f in self.engs["sp"].prog:
                    f(e)

            @block.tensor
            def _(e):
                for f in self.engs["pe"].prog:
                    f(e)

            @block.scalar
            def _(e):
                for f in self.engs["act"].prog:
                    f(e)

            @block.vector
            def _(e):
                for f in self.engs["dve"].prog:
                    f(e)

            @block.gpsimd
            def _(e):
                for f in self.engs["pool"].prog:
                    f(e)


def build_program(S=SEQ, depth=DEPTH, topk=None, debug=False):
    NT = S // 128
    if topk is None:
        topk = min(256, S // 4)
    nc = bass.Bass("TRN2", target_bir_lowering=False)
    stack = ExitStack()
    kb = KB(nc, stack)

    def din(name, shape, dt=F32):
        return nc.dram_tensor(name, list(shape), dt, kind="ExternalInput").ap()

    def dscr(name, shape, dt):
        kind = "ExternalOutput" if debug else "Internal"
        return nc.dram_tensor(name, list(shape), dt, kind=kind).ap()

    L = depth
    x_in = din("x", [S, D_MODEL])
    w_in_d = din("w_in", [L, D_MODEL, IN_WIDTH])
    w_uq_d = din("w_uq", [L, 256, 384])
    w_ukv_d = din("w_ukv", [L, 128, 512])
    w_out_d = din("w_out", [L, 1024, 1024])
    w_up_d = din("w_up", [L, 1024, D_FF])
    w_down_d = din("w_down", [L, D_FF, 1024])
    norm1_d = din("norm1", [L, 1024])
    norm2_d = din("norm2", [L, 1024])
    gq_d = din("gq", [L, 256])
    gkv_d = din("gkv", [L, 128])
    sinks_d = din("sinks", [L, 4])
    foxb_d = din("foxb", [L, 4])
    fnorm_d = din("fnorm", [1, 1024])
    ropet_d = din("ropet", [S, 96])
    cmat_d = din("cmat", [128, 5 * 512])
    out_d = nc.dram_tensor("out", [S, D_MODEL], F32, kind="ExternalOutput").ap()

    xs = [dscr("xs0", [S, D_MODEL], F32), dscr("xs1", [S, D_MODEL], F32)]
    xm_d = dscr("xm", [S, D_MODEL], F32)
    mixed_d = dscr("mixed", [S, D_MODEL], BF16)
    DKA = dict(A=96, B=64, C=64, D=68)
    HK = dict(A=4, B=2, C=4, D=4)
    qT_d = {g: dscr("qT_" + g, [DKA[g], 4, S], BF16) for g in "ABCD"}
    kT_d = {g: dscr("kT_" + g, [DKA[g], HK[g], S], BF16) for g in "ABCD"}
    v_d = {g: dscr("v_" + g, [S, HK[g], 65], BF16) for g in "ABCD"}
    qiT_d = dscr("qiT", [32, 8, S], BF16)
    kiT_d = dscr("kiT", [32, S], BF16)
    wi_d = dscr("wi", [S, 8], F32)

    R = kb.res
    r_xin = R("x_in")
    r_xs = [R("xs0"), R("xs1")]
    r_xm = R("xm")
    r_mixed = R("mixed")
    r_qT = {g: R("qT" + g) for g in "ABCD"}
    r_kT = {g: R("kT" + g) for g in "ABCD"}
    r_v = {g: R("v" + g) for g in "ABCD"}
    r_qiT, r_kiT, r_wi = R("qiT"), R("kiT"), R("wi")
    r_out = R("out")
    r_const = R("constin")

    banks = [kb.ps("bank%d" % i, [128, 512], F32) for i in range(8)]

    cm_f = kb.sb("cm_f", [128, 5 * 512], F32)
    kb.dma("sp", cm_f[:], cmat_d[:, :], reads=[r_const], writes=[cm_f], owner=cm_f)
    ident4 = kb.sb("ident4", [128, 512], BF16)
    maskc4 = kb.sb("maskc4", [128, 512], BF16)
    maskp4 = kb.sb("maskp4", [128, 512], BF16)
    kb.op("dve", "tensor_copy", [cm_f], [ident4], out=ident4[:], in_=cm_f[:, 0:512])
    kb.op("dve", "tensor_copy", [cm_f], [maskc4], out=maskc4[:], in_=cm_f[:, 512:1024])
    kb.op("dve", "tensor_copy", [cm_f], [maskp4], out=maskp4[:], in_=cm_f[:, 1024:1536])
    ident = ident4
    negS = cm_f[:, 1536:1664]
    TRI = cm_f[:, 1664:1792]
    LAST = cm_f[:, 1792:1920]
    identf = cm_f[:, 2048:2176]

    def bcast_load(tile_, src_row_ap, n):
        kb.dma("sp", tile_[:], src_row_ap.to_broadcast([128, n]), reads=[r_const], writes=[tile_], owner=tile_)

    def load_cast_weight(dst_tile, dst_ap_fn, src_ap_fn, nchunks, ncols, stg, engs=("dve", "pool", "act")):
        for c in range(nchunks):
            st = stg[c % len(stg)]
            kb.dma("sp", st[:, 0:ncols], src_ap_fn(c), reads=[r_const], writes=[st], owner=st)
            en = engs[c % len(engs)]
            if en == "act":
                kb.op("act", "activation", [st], [dst_tile], out=dst_ap_fn(c), in_=st[:, 0:ncols], func=AF.Copy)
            else:
                kb.op(en, "tensor_copy", [st], [dst_tile], out=dst_ap_fn(c), in_=st[:, 0:ncols])

    def rmsnorm_rstd(src_tile, src_ap, n, ss, rstd, junk, lnexp=False):
        kb.op("act", "activation", [src_tile], [junk, ss], out=junk[:, 0:n], in_=src_ap, func=AF.Square,
              accum_out=ss[:, 0:1])
        kb.op("dve", "tensor_scalar", [ss], [rstd], out=rstd[:, 0:1], in0=ss[:, 0:1], scalar1=1.0 / n,
              scalar2=EPS, op0=ALU.mult, op1=ALU.add)
        if lnexp:
            kb.op("act", "activation", [rstd], [rstd], out=rstd[:, 0:1], in_=rstd[:, 0:1], func=AF.Ln)
            kb.op("act", "activation", [rstd], [rstd], out=rstd[:, 0:1], in_=rstd[:, 0:1], func=AF.Exp, scale=-0.5)
            return
        kb.op("act", "activation", [rstd], [rstd], out=rstd[:, 0:1], in_=rstd[:, 0:1], func=AF.Sqrt)
        kb.op("dve", "reciprocal", [rstd], [rstd], out=rstd[:, 0:1], in_=rstd[:, 0:1])

    def rope(src_tile, src4, dst_tile, dst4, cos_t, sin_t, nh, half, tmp, tmpb):
        (cos_t, co), (sin_t, so) = cos_t, sin_t
        cb = cos_t[:, co:co + half].unsqueeze(1).unsqueeze(1).to_broadcast([128, nh, 2, half])
        sb_ = sin_t[:, so:so + half].unsqueeze(1).unsqueeze(1).to_broadcast([128, nh, 2, half])
        n = nh * 2 * half
        tc = tmp[:, 0:n].rearrange("p (h two d) -> p h two d", h=nh, two=2)
        ts = tmpb[:, 0:n].rearrange("p (h two d) -> p h two d", h=nh, two=2)
        kb.op("dve", "tensor_tensor", [src_tile, cos_t], [tmp], out=tc, in0=src4, in1=cb, op=ALU.mult)
        kb.op("pool", "tensor_tensor", [src_tile, sin_t], [tmpb], out=ts, in0=src4, in1=sb_, op=ALU.mult)
        kb.op("dve", "tensor_tensor", [tmp, tmpb], [dst_tile], out=dst4[:, :, 0, :], in0=tc[:, :, 0, :],
              in1=ts[:, :, 1, :], op=ALU.subtract)
        kb.op("dve", "tensor_tensor", [tmp, tmpb], [dst_tile], out=dst4[:, :, 1, :], in0=tc[:, :, 1, :],
              in1=ts[:, :, 0, :], op=ALU.add)

    def phase1(l, xsrc_d, r_xsrc):
        with ExitStack() as st1:
            old = kb.stack
            kb.stack = st1
            win = kb.sb("win", [128, 8, IN_WIDTH], BF16)
            wuq = kb.sb("wuq", [128, 2, 384], BF16)
            wukv = kb.sb("wukv", [128, 512], BF16)
            stg = [kb.sb("p1stg%d" % i, [128, IN_WIDTH], F32) for i in range(2)]
            load_cast_weight(win, lambda c: win[:, c, :], lambda c: w_in_d[l, c * 128:(c + 1) * 128, :], 8,
                             IN_WIDTH, stg)
            load_cast_weight(wuq, lambda c: wuq[:, c, :], lambda c: w_uq_d[l, c * 128:(c + 1) * 128, :], 2, 384, stg)
            load_cast_weight(wukv, lambda c: wukv[:, :], lambda c: w_ukv_d[l, :, :], 1, 512, stg)
            g1 = kb.sb("g1", [128, 1024], F32)
            gq = kb.sb("gq", [128, 256], F32)
            gkv = kb.sb("gkv", [128, 128], F32)
            fb = kb.sb("fb", [128, 4], F32)
            bcast_load(g1, norm1_d[l:l + 1, :], 1024)
            bcast_load(gq, gq_d[l:l + 1, :], 256)
            bcast_load(gkv, gkv_d[l:l + 1, :], 128)
            bcast_load(fb, foxb_d[l:l + 1, :], 4)
            nfb = kb.sb("nfb", [128, 4], F32)
            kb.op("dve", "tensor_scalar", [fb], [nfb], out=nfb[:], in0=fb[:], scalar1=-1.0, scalar2=None,
                  op0=ALU.mult)

            xt = [kb.sb("xt%d" % i, [128, 1024], F32) for i in range(2)]
            rt = [kb.sb("rt%d" % i, [128, 96], F32) for i in range(2)]
            junk = kb.sb("junk", [128, 1024], F32)
            ss = kb.sb("ss", [128, 4], F32)
            rstd = kb.sb("rstd", [128, 4], F32)
            hb = kb.sb("hb", [128, 1024], BF16)
            hT = kb.sb("hT", [128, 8, 128], BF16)
            projs = [kb.sb("proj%d" % i, [128, IN_WIDTH], F32) for i in range(2)]
            junk2 = kb.sb("junk2", [128, 384], F32)
            ss2 = kb.sb("ss2", [128, 4], F32)
            rstd2 = kb.sb("rstd2", [128, 4], F32)
            r64 = kb.sb("r64", [128, 14, 64], BF16)
            r32 = kb.sb("r32", [128, 10, 32], BF16)
            tmp = kb.sb("tmp", [128, 896], F32)
            tmpb = kb.sb("tmpb", [128, 896], F32)
            tmp2 = kb.sb("tmp2", [128, 320], F32)
            tmp2b = kb.sb("tmp2b", [128, 320], F32)
            tmp3 = kb.sb("tmp3", [128, 128], F32)
            tmp3a = kb.sb("tmp3a", [128, 128], F32)
            tmp3b = kb.sb("tmp3b", [128, 128], F32)
            cqn = kb.sb("cqn", [128, 384], BF16)
            cqnT = kb.sb("cqnT", [128, 3, 128], BF16)
            qa = kb.sb("qa", [128, 4, 96], BF16)
            ka = kb.sb("ka", [128, 4, 96], BF16)
            qd = kb.sb("qd", [128, 4, 68], BF16)
            kd = kb.sb("kd", [128, 4, 68], BF16)
            vst = {g: [kb.sb("vst%s%d" % (g, i), [128, HK[g], 65], BF16) for i in range(2)] for g in "ABCD"}
            for g in "ABCD":
                for i in range(2):
                    kb.op("pool", "memset", [], [vst[g][i]], ap=vst[g][i][:], constant=1.0)
            kb.op("pool", "memset", [], [qd], ap=qd[:], constant=1.0)
            kb.op("pool", "memset", [], [kd], ap=kd[:], constant=1.0)
            logf = kb.sb("logf", [128, 4], F32)
            cum = [kb.sb("cum%d" % i, [128, 4], F32) for i in range(2)]
            kb.op("pool", "memset", [], [cum[1]], ap=cum[1][:], constant=0.0)
            c8 = kb.sb("c8", [128, 4], F32)
            cp = kb.sb("cp", [128, 3, 4], BF16)
            cr = kb.sb("cr", [128, 4], F32)
            wst = [kb.sb("wst%d" % i, [128, 8], F32) for i in range(2)]
            tst = [[kb.sb("tst%d_%d" % (j, i), [128, 4, 128], BF16) for j in range(9)] for i in range(2)]

            def stage1(tt):
                par = tt % 2
                tok = slice(tt * 128, (tt + 1) * 128)
                proj = projs[par]
                x = xt[par]
                kb.dma("pool", x[:], xsrc_d[tok, :], reads=[r_xsrc], writes=[x], owner=x)
                kb.dma("pool", rt[par][:], ropet_d[tok, :], reads=[r_const], writes=[rt[par]], owner=rt[par])
                rmsnorm_rstd(x, x[:], 1024, ss, rstd, junk, lnexp=True)
                kb.op("dve", "scalar_tensor_tensor", [x, rstd, g1], [hb], out=hb[:], in0=x[:], scalar=rstd[:, 0:1],
                      in1=g1[:], op0=ALU.mult, op1=ALU.mult)
                bT = banks[6]
                bTv = bT[:].bitcast(BF16)
                for kc in range(8):
                    kb.op("pe", "transpose", [hb, ident], [bT], out=bTv[:, kc * 128:(kc + 1) * 128],
                          in_=hb[:, kc * 128:(kc + 1) * 128], identity=ident[:, 0:128])
                kb.op("act", "activation", [bT], [hT], out=hT[:].rearrange("p k t -> p (k t)"), in_=bTv,
                      func=AF.Copy)
                for nb in range(6):
                    c0, c1 = nb * 512, min((nb + 1) * 512, IN_WIDTH)
                    for kc in range(8):
                        kb.op("pe", "matmul", [hT, win], [banks[nb]], out=banks[nb][:, 0:c1 - c0], lhsT=hT[:, kc, :],
                              rhs=win[:, kc, c0:c1], start=(kc == 0), stop=(kc == 7))
                    if nb % 2 == 0:
                        kb.op("act", "activation", [banks[nb]], [proj], out=proj[:, c0:c1],
                              in_=banks[nb][:, 0:c1 - c0], func=AF.Copy)
                    else:
                        kb.op("dve", "tensor_copy", [banks[nb]], [proj], out=proj[:, c0:c1],
                              in_=banks[nb][:, 0:c1 - c0])
            def stage2(tt):
                par = tt % 2
                tok = slice(tt * 128, (tt + 1) * 128)
                proj = projs[par]
                c64, s64, c32, s32 = (rt[par], 0), (rt[par], 32), (rt[par], 64), (rt[par], 80)
                bT = banks[7]
                bTv = bT[:].bitcast(BF16)
                rope(proj, proj[:, 0:896].rearrange("p (h two d) -> p h two d", h=14, two=2), r64,
                     r64[:].rearrange("p h (two d) -> p h two d", two=2), c64, s64, 14, 32, tmp, tmpb)
                rope(proj, proj[:, 896:1216].rearrange("p (h two d) -> p h two d", h=10, two=2), r32,
                     r32[:].rearrange("p h (two d) -> p h two d", two=2), c32, s32, 10, 16, tmp2, tmp2b)
                vs = {g: vst[g][par] for g in "ABCD"}
                o = COL["b_v"][0]
                kb.op("pool", "tensor_copy", [proj], [vs["B"]], out=vs["B"][:, :, 0:64],
                      in_=proj[:, o:o + 128].rearrange("p (h d) -> p h d", h=2))
                o = COL["c_v"][0]
                kb.op("act", "activation", [proj], [vs["C"]], out=vs["C"][:, :, 0:64],
                      in_=proj[:, o:o + 256].rearrange("p (h d) -> p h d", h=4), func=AF.Copy)
                o = COL["d_v"][0]
                kb.op("act", "activation", [proj], [vs["D"]], out=vs["D"][:, :, 0:64],
                      in_=proj[:, o:o + 256].rearrange("p (h d) -> p h d", h=4), func=AF.Copy)
                o = COL["a_cq"][0]
                kb.op("act", "activation", [proj], [junk2, ss2], out=junk2[:, 0:256], in_=proj[:, o:o + 256],
                      func=AF.Square, accum_out=ss2[:, 1:2])
                kb.op("act", "activation", [proj], [junk2, ss2], out=junk2[:, 256:384], in_=proj[:, o + 256:o + 384],
                      func=AF.Square, accum_out=ss2[:, 2:3])
                kb.op("dve", "tensor_scalar", [ss2], [rstd2], out=rstd2[:, 1:2], in0=ss2[:, 1:2], scalar1=1.0 / 256,
                      scalar2=EPS, op0=ALU.mult, op1=ALU.add)
                kb.op("dve", "tensor_scalar", [ss2], [rstd2], out=rstd2[:, 2:3], in0=ss2[:, 2:3], scalar1=1.0 / 128,
                      scalar2=EPS, op0=ALU.mult, op1=ALU.add)
                kb.op("act", "activation", [rstd2], [rstd2], out=rstd2[:, 1:3], in_=rstd2[:, 1:3], func=AF.Ln)
                kb.op("act", "activation", [rstd2], [rstd2], out=rstd2[:, 1:3], in_=rstd2[:, 1:3], func=AF.Exp,
                      scale=-0.5)
                kb.op("dve", "scalar_tensor_tensor", [proj, rstd2, gq], [cqn], out=cqn[:, 0:256],
                      in0=proj[:, o:o + 256], scalar=rstd2[:, 1:2], in1=gq[:], op0=ALU.mult, op1=ALU.mult)
                kb.op("dve", "scalar_tensor_tensor", [proj, rstd2, gkv], [cqn], out=cqn[:, 256:384],
                      in0=proj[:, o + 256:o + 384], scalar=rstd2[:, 2:3], in1=gkv[:], op0=ALU.mult, op1=ALU.mult)
                for kc in range(3):
                    kb.op("pe", "transpose", [cqn, ident], [bT], out=bTv[:, kc * 128:(kc + 1) * 128],
                          in_=cqn[:, kc * 128:(kc + 1) * 128], identity=ident[:, 0:128])
                kb.op("act", "activation", [bT], [cqnT], out=cqnT[:].rearrange("p k t -> p (k t)"),
                      in_=bTv[:, 0:384], func=AF.Copy)
                bq, bkv = banks[0], banks[1]
                for kc in range(2):
                    kb.op("pe", "matmul", [cqnT, wuq], [bq], out=bq[:, 0:384], lhsT=cqnT[:, kc, :], rhs=wuq[:, kc, :],
                          start=(kc == 0), stop=(kc == 1))
                kb.op("pe", "matmul", [cqnT, wukv], [bkv], out=bkv[:, 0:512], lhsT=cqnT[:, 2, :], rhs=wukv[:, :],
                      start=True, stop=True)
                bq3 = bq[:, 0:384].rearrange("p (h d) -> p h d", h=4)
                kb.op("act", "activation", [bq], [qa], out=qa[:, :, 0:64], in_=bq3[:, :, 0:64], func=AF.Copy)
                kb.op("act", "activation", [bq], [tmp3], out=tmp3[:, 0:128].rearrange("p (h d) -> p h d", h=4),
                      in_=bq3[:, :, 64:96], func=AF.Copy)
                rope(tmp3, tmp3[:, 0:128].rearrange("p (h two d) -> p h two d", h=4, two=2), qa,
                     qa[:, :, 64:96].rearrange("p h (two d) -> p h two d", two=2), c32, s32, 4, 16, tmp3a, tmp3b)
                bkv3 = bkv[:, 0:512].rearrange("p (h d) -> p h d", h=4)
                kb.op("act", "activation", [bkv], [ka], out=ka[:, :, 0:64], in_=bkv3[:, :, 0:64], func=AF.Copy)
                kb.op("dve", "tensor_copy", [bkv], [vs["A"]], out=vs["A"][:, :, 0:64], in_=bkv3[:, :, 64:128])
                kb.op("pool", "tensor_copy", [r32], [ka], out=ka[:, :, 64:96],
                      in_=r32[:, 9:10, :].to_broadcast([128, 4, 32]))
                o = COL["d_f"][0]
                kb.op("dve", "scalar_tensor_tensor", [proj, nfb], [logf], out=logf[:], in0=proj[:, o:o + 4],
                      scalar=-1.0, in1=nfb[:], op0=ALU.mult, op1=ALU.add)
                kb.op("act", "activation", [logf], [logf], out=logf[:], in_=logf[:], func=AF.Exp)
                kb.op("act", "activation", [logf], [logf], out=logf[:], in_=logf[:], func=AF.Ln, bias=1.0)
                kb.op("dve", "tensor_scalar", [logf], [logf], out=logf[:], in0=logf[:], scalar1=-1.0, scalar2=None,
                      op0=ALU.mult)
                bc = banks[2]
                cprev, ccur = cum[(tt + 1) % 2], cum[tt % 2]
                kb.op("pe", "matmul", [cm_f, logf], [bc], out=bc[:, 0:4], lhsT=TRI, rhs=logf[:], start=True,
                      stop=False)
                kb.op("pe", "matmul", [cm_f, cprev], [bc], out=bc[:, 0:4], lhsT=LAST, rhs=cprev[:], start=False,
                      stop=True)
                kb.op("dve", "tensor_copy", [bc], [ccur], out=ccur[:], in_=bc[:, 0:4])
                kb.op("dve", "tensor_scalar", [ccur], [c8], out=c8[:], in0=ccur[:], scalar1=8.0, scalar2=None,
                      op0=ALU.mult)
                kb.op("dve", "tensor_copy", [c8], [cp], out=cp[:, 0, :], in_=c8[:])
                kb.op("dve", "tensor_tensor", [c8, cp], [cr], out=cr[:], in0=c8[:], in1=cp[:, 0, :], op=ALU.subtract)
                kb.op("dve", "tensor_copy", [cr], [cp], out=cp[:, 1, :], in_=cr[:])
                kb.op("dve", "tensor_tensor", [cr, cp], [cr], out=cr[:], in0=cr[:], in1=cp[:, 1, :], op=ALU.subtract)
                kb.op("dve", "tensor_copy", [cr], [cp], out=cp[:, 2, :], in_=cr[:])
                o = COL["d_q"][0]
                kb.op("pool", "tensor_copy", [proj], [qd], out=qd[:, :, 0:64],
                      in_=proj[:, o:o + 256].rearrange("p (h d) -> p h d", h=4))
                kb.op("pool", "tensor_copy", [cp], [qd], out=qd[:, :, 64:65], in_=cp[:, 0, :].unsqueeze(2))
                o = COL["d_k"][0]
                kb.op("pool", "tensor_copy", [proj], [kd], out=kd[:, :, 0:64],
                      in_=proj[:, o:o + 256].rearrange("p (h d) -> p h d", h=4))
                kb.op("dve", "tensor_scalar", [cp], [kd], out=kd[:, :, 65:68], in0=cp[:].rearrange("p c h -> p h c"),
                      scalar1=-1.0, scalar2=None, op0=ALU.mult)
                o = COL["c_w"][0]
                ws = wst[par]
                kb.op("pool", "tensor_copy", [proj], [ws], out=ws[:], in_=proj[:, o:o + 8])
                kb.dma("sp", wi_d[tok, :], ws[:], reads=[ws], writes=[r_wi], owner=ws)
                for g in "ABCD":
                    kb.dma("sp", v_d[g][tok, :, :], vs[g][:], reads=[vs[g]], writes=[r_v[g]], owner=vs[g])
                items = [
                    (qa, [qa[:, h, :] for h in range(4)], 96, qT_d["A"], r_qT["A"]),
                    (ka, [ka[:, h, :] for h in range(4)], 96, kT_d["A"], r_kT["A"]),
                    (r64, [r64[:, h, :] for h in range(0, 4)], 64, qT_d["B"], r_qT["B"]),
                    (r64, [r64[:, h, :] for h in range(4, 6)], 64, kT_d["B"], r_kT["B"]),
                    (r64, [r64[:, h, :] for h in range(6, 10)], 64, qT_d["C"], r_qT["C"]),
                    (r64, [r64[:, h, :] for h in range(10, 14)], 64, kT_d["C"], r_kT["C"]),
                    (qd, [qd[:, h, :] for h in range(4)], 68, qT_d["D"], r_qT["D"]),
                    (kd, [kd[:, h, :] for h in range(4)], 68, kT_d["D"], r_kT["D"]),
                    (r32, [r32[:, h, :] for h in range(0, 4)], 32, qiT_d[:, 0:4, :], r_qiT),
                    (r32, [r32[:, h, :] for h in range(4, 8)], 32, qiT_d[:, 4:8, :], r_qiT),
                    (r32, [r32[:, 8, :]], 32, None, r_kiT),
                ]
                for ii, (src, aps, n, dst, rdst) in enumerate(items):
                    bk = banks[3 + (ii % 3)]
                    bkv_ = bk[:].bitcast(BF16)
                    nh = len(aps)
                    for h, ap in enumerate(aps):
                        kb.op("pe", "transpose", [src, ident], [bk], out=bkv_[0:n, h * 128:(h + 1) * 128], in_=ap,
                              identity=ident[:, 0:128])
                    sg = tst[par][ii % 9] if ii < 9 else tst[par][ii - 9 + 0]
                    en = "act" if ii % 2 == 0 else "dve"
                    if en == "act":
                        kb.op("act", "activation", [bk], [sg], out=sg[0:n, 0:nh, :].rearrange("p h t -> p (h t)"),
                              in_=bkv_[0:n, 0:nh * 128], func=AF.Copy)
                    else:
                        kb.op("dve", "tensor_copy", [bk], [sg], out=sg[0:n, 0:nh, :].rearrange("p h t -> p (h t)"),
                              in_=bkv_[0:n, 0:nh * 128])
                    if dst is None:
                        kb.dma("sp", kiT_d[:, tok], sg[0:32, 0, :], reads=[sg], writes=[rdst], owner=sg)
                    else:
                        kb.dma("sp", dst[:, :, tok], sg[0:n, 0:nh, :], reads=[sg], writes=[rdst], owner=sg)
            stage1(0)
            for tt in range(NT):
                if tt + 1 < NT:
                    stage1(tt + 1)
                stage2(tt)
            kb.barrier()
            kb.stack = old

    def attention(l, g):
        dk = DKA[g]
        hk = HK[g]
        gi = "ABCD".index(g)
        scale = {"A": 96 ** -0.5, "B": 0.125, "C": 0.125, "D": 0.125}[g]
        with ExitStack() as st2:
            old = kb.stack
            kb.stack = st2
            qT = kb.sb("aqT", [dk, 4, S], BF16)
            kT = kb.sb("akT", [dk, hk, S], BF16)
            vv = kb.sb("avv", [128, NT, hk * 65], BF16)
            for h in range(4):
                kb.dma("sp", qT[:, h, :], qT_d[g][:, h, :], reads=[r_qT[g]], writes=[qT], owner=qT)
            for h in range(hk):
                kb.dma("sp", kT[:, h, :], kT_d[g][:, h, :], reads=[r_kT[g]], writes=[kT], owner=kT)
            vsrc = v_d[g].rearrange("(j p) h d -> p j (h d)", p=128)
            for j0 in range(0, NT, 4):
                kb.dma("sp", vv[:, j0:j0 + 4, :], vsrc[:, j0:j0 + 4, :], reads=[r_v[g]], writes=[vv], owner=vv)
            pT = [kb.sb("apT%d" % i, [128, 512], BF16) for i in range(2)]
            osb = [kb.sb("aosb%d" % i, [128, 4, 64], BF16) for i in range(2)]
            den = kb.sb("aden", [128, 4], F32)
            if g == "B":
                esink = kb.sb("esink", [128, 4], F32)
                bcast_load(esink, sinks_d[l:l + 1, :], 4)
                kb.op("act", "activation", [esink], [esink], out=esink[:], in_=esink[:], func=AF.Exp)
            if g == "C":
                qiTs = [kb.sb("cqiT%d" % i, [32, 8, 128], BF16) for i in range(2)]
                kiT = kb.sb("ckiT", [32, S], BF16)
                kb.dma("sp", kiT[:], kiT_d[:, :], reads=[r_kiT], writes=[kiT], owner=kiT)
                wsb = kb.sb("cwsb", [128, NT, 8], F32)
                wsrc = wi_d.rearrange("(j p) h -> p j h", p=128)
                for j0 in range(0, NT, 4):
                    kb.dma("sp", wsb[:, j0:j0 + 4, :], wsrc[:, j0:j0 + 4, :], reads=[r_wi], writes=[wsb], owner=wsb)
                Isbs = [kb.sb("cI%d" % i, [128, S], F32) for i in range(2)]
                Madd = [kb.sb("cMadd%d" % i, [128, S], BF16) for i in range(2)]
                rl = [kb.sb("crl%d" % i, [128, 512], BF16) for i in range(3)]
                dgs = [kb.sb("cdg%d" % i, [128, 8, 128], BF16) for i in range(2)]
                cjunk = kb.sb("cjunk", [128, S], BF16)
                lo = kb.sb("clo", [128, 1], F32)
                stp = kb.sb("cstp", [128, 1], F32)
                cand = kb.sb("ccand", [128, 1], F32)
                cnt = kb.sb("ccnt", [128, 1], F32)
                mm = kb.sb("cmm", [128, 1], F32)
                hi = kb.sb("chi", [128, 1], F32)

            def kts_of(qt):
                if g == "B":
                    return [kt for kt in (qt - 1, qt) if kt >= 0]
                return list(range(qt + 1))

            def c_scores(qt):
                qs = slice(qt * 128, (qt + 1) * 128)
                Lk = 128 * (qt + 1)
                nblk = (Lk + 511) // 512
                Isb = Isbs[qt % 2]
                qiT = qiTs[qt % 2]
                dg = dgs[qt % 2]
                kb.dma("pool", qiT[:], qiT_d[:, :, qs], reads=[r_qiT], writes=[qiT], owner=qiT)
                kb.op("pool", "tensor_tensor", [ident4, wsb], [dg], out=dg[:],
                      in0=ident4[:, 0:128].unsqueeze(1).to_broadcast([128, 8, 128]),
                      in1=wsb[:, qt, :].unsqueeze(2).to_broadcast([128, 8, 128]), op=ALU.mult)
                hc = 0
                for kbk in range(nblk):
                    k0, k1 = kbk * 512, min((kbk + 1) * 512, Lk)
                    n = k1 - k0
                    bacc = banks[6 + (kbk % 2)]
                    pend = None
                    for hh in range(8):
                        bi = banks[4 + (hc % 2)]
                        r_ = rl[hc % 3]
                        kb.op("pe", "matmul", [qiT, kiT], [bi], out=bi[:, 0:n], lhsT=qiT[:, hh, :],
                              rhs=kiT[:, k0:k1], start=True, stop=True)
                        kb.op("act", "activation", [bi], [r_], out=r_[:, 0:n], in_=bi[:, 0:n], func=AF.Relu)
                        if pend is not None:
                            ph, pr = pend
                            kb.op("pe", "matmul", [dg, pr], [bacc], out=bacc[:, 0:n], lhsT=dg[:, ph, :], rhs=pr[:, 0:n],
                                  start=(ph == 0), stop=False)
                        pend = (hh, r_)
                        hc += 1
                    ph, pr = pend
                    kb.op("pe", "matmul", [dg, pr], [bacc], out=bacc[:, 0:n], lhsT=dg[:, ph, :], rhs=pr[:, 0:n],
                          start=False, stop=True)
                    kb.op("act", "activation", [bacc], [Isb], out=Isb[:, k0:k1], in_=bacc[:, 0:n], func=AF.Copy)

            def c_select(qt):
                qs = slice(qt * 128, (qt + 1) * 128)
                Lk = 128 * (qt + 1)
                Isb = Isbs[qt % 2]
                madd = Madd[qt % 2]
                kb.op("dve", "tensor_tensor", [Isb, cm_f], [Isb], out=Isb[:, qs], in0=Isb[:, qs], in1=negS, op=ALU.add)
                if Lk <= topk:
                    kb.op("dve", "tensor_scalar", [Isb], [madd], out=madd[:, 0:Lk], in0=Isb[:, 0:Lk],
                          scalar1=-1.0e29, scalar2=-BIGM, op0=ALU.is_lt, op1=ALU.mult)
                    return
                assert Lk - 128 >= topk
                kb.op("dve", "tensor_reduce", [Isb], [hi], out=hi[:], in_=Isb[:, 0:Lk], axis=AX.X, op=ALU.max)
                kb.op("dve", "tensor_reduce", [Isb], [lo], out=lo[:], in_=Isb[:, 0:Lk - 128], axis=AX.X, op=ALU.min)
                kb.op("dve", "tensor_tensor", [hi, lo], [stp], out=stp[:], in0=hi[:], in1=lo[:], op=ALU.subtract)
                for it in range(N_BISECT):
                    f = 0.5 ** (it + 1)
                    kb.op("dve", "scalar_tensor_tensor", [stp, lo], [cand], out=cand[:], in0=stp[:], scalar=f,
                          in1=lo[:], op0=ALU.mult, op1=ALU.add)
                    kb.op("dve", "tensor_scalar", [Isb, cand], [cjunk, cnt], out=cjunk[:, 0:Lk], in0=Isb[:, 0:Lk],
                          scalar1=cand[:, 0:1], scalar2=None, op0=ALU.is_ge, op1=ALU.add, accum_out=cnt[:, 0:1])
                    kb.op("dve", "scalar_tensor_tensor", [cnt, stp], [mm], out=mm[:], in0=cnt[:],
                          scalar=float(topk) - 0.5, in1=stp[:], op0=ALU.is_ge, op1=ALU.mult)
                    kb.op("dve", "scalar_tensor_tensor", [mm, lo], [lo], out=lo[:], in0=mm[:], scalar=f, in1=lo[:],
                          op0=ALU.mult, op1=ALU.add)
                kb.op("dve", "tensor_scalar", [Isb, lo], [madd], out=madd[:, 0:Lk], in0=Isb[:, 0:Lk],
                      scalar1=lo[:, 0:1], scalar2=-BIGM, op0=ALU.is_lt, op1=ALU.mult)

            def emit_scores(qt, ki, kt):
                qs = slice(qt * 128, (qt + 1) * 128)
                ks = slice(kt * 128, (kt + 1) * 128)
                bs = banks[ki % 2]
                have_mask = False
                if g == "C":
                    madd = Madd[qt % 2]
                    kb.op("pe", "matmul", [madd, ident4], [bs], out=bs[:, 0:512], lhsT=madd[:, ks],
                          rhs=ident4[:, 0:512], start=True, stop=False, skip_group_check=True)
                    have_mask = True
                elif kt == qt:
                    kb.op("pe", "matmul", [ident4, maskc4], [bs], out=bs[:, 0:512], lhsT=ident4[:, 0:128],
                          rhs=maskc4[:, 0:512], start=True, stop=False, skip_group_check=True)
                    have_mask = True
                elif g == "B":
                    kb.op("pe", "matmul", [ident4, maskp4], [bs], out=bs[:, 0:512], lhsT=ident4[:, 0:128],
                          rhs=maskp4[:, 0:512], start=True, stop=False, skip_group_check=True)
                    have_mask = True
                for h in range(4):
                    kvh = h if hk == 4 else h // 2
                    kb.op("pe", "matmul", [kT, qT], [bs], out=bs[:, h * 128:(h + 1) * 128], lhsT=kT[:, kvh, ks],
                          rhs=qT[:, h, qs], start=((not have_mask) and h == 0), stop=True, skip_group_check=True)
                p = pT[ki % 2]
                kb.op("act", "activation", [bs], [p], out=p[:], in_=bs[:, 0:512], func=AF.Exp, scale=scale)

            def emit_pv(qt, ki, kt, nk):
                bo = banks[2 + (qt % 2)]
                p = pT[ki % 2]
                for h in range(4):
                    kvh = h if hk == 4 else h // 2
                    kb.op("pe", "matmul", [p, vv], [bo], out=bo[:, h * 65:(h + 1) * 65],
                          lhsT=p[:, h * 128:(h + 1) * 128], rhs=vv[:, kt, kvh * 65:(kvh + 1) * 65],
                          start=(ki == 0 and h == 0), stop=(ki == nk - 1), skip_group_check=True)

            def emit_attn(qt):
                kts = kts_of(qt)
                prev = None
                for ki, kt in enumerate(kts):
                    emit_scores(qt, ki, kt)
                    if prev is not None:
                        emit_pv(qt, prev[0], prev[1], len(kts))
                    prev = (ki, kt)
                emit_pv(qt, prev[0], prev[1], len(kts))

            def emit_norm(qt):
                qs = slice(qt * 128, (qt + 1) * 128)
                bo = banks[2 + (qt % 2)]
                bo3 = bo[:, 0:260].rearrange("p (h d) -> p h d", h=4)
                if g == "B":
                    kb.op("dve", "tensor_tensor", [bo, esink], [den], out=den[:].unsqueeze(2), in0=bo3[:, :, 64:65],
                          in1=esink[:].unsqueeze(2), op=ALU.add)
                else:
                    kb.op("dve", "tensor_copy", [bo], [den], out=den[:].unsqueeze(2), in_=bo3[:, :, 64:65])
                kb.op("dve", "reciprocal", [den], [den], out=den[:], in_=den[:])
                ob = osb[qt % 2]
                kb.op("dve", "tensor_tensor", [bo, den], [ob], out=ob[:], in0=bo3[:, :, 0:64],
                      in1=den[:].unsqueeze(2).to_broadcast([128, 4, 64]), op=ALU.mult)
                kb.dma("sp", mixed_d[qs, gi * 256:(gi + 1) * 256], ob[:].rearrange("p h d -> p (h d)"), reads=[ob],
                       writes=[r_mixed], owner=ob)

            if g == "C":
                c_scores(0)
                for qt in range(NT):
                    if qt + 1 < NT:
                        c_scores(qt + 1)
                    c_select(qt)
                    if qt > 0:
                        emit_norm(qt - 1)
                    emit_attn(qt)
                emit_norm(NT - 1)
            else:
                for qt in range(NT):
                    emit_attn(qt)
                    emit_norm(qt)
            kb.barrier()
            kb.stack = old

    def mlp_weight_chunks(l, wu, wd, stg):
        ems = []
        engs = ("pool", "act", "dve")

        def mk(c, dst_ap, src_ap, ncols):
            def em():
                st = stg[c % len(stg)]
                kb.dma("sp", st[:, 0:ncols], src_ap, reads=[r_const], writes=[st], owner=st)
                en = engs[c % 3]
                dst_tile = wu if c < 16 else wd
                if en == "act":
                    kb.op("act", "activation", [st], [dst_tile], out=dst_ap, in_=st[:, 0:ncols], func=AF.Copy)
                else:
                    kb.op(en, "tensor_copy", [st], [dst_tile], out=dst_ap, in_=st[:, 0:ncols])
            return em

        for c in range(16):
            ems.append(mk(c, wu[:, c // 2, (c % 2) * 2048:(c % 2 + 1) * 2048],
                          w_up_d[l, (c // 2) * 128:(c // 2 + 1) * 128, (c % 2) * 2048:(c % 2 + 1) * 2048], 2048))
        for c in range(32):
            ems.append(mk(16 + c, wd[:, c, :], w_down_d[l, c * 128:(c + 1) * 128, :], 1024))
        return ems

    def phase3a(l, xsrc_d, r_xsrc, wchunks):
        with ExitStack() as st3:
            old = kb.stack
            kb.stack = st3
            wo = kb.sb("wo", [128, 8, 1024], BF16)
            stg = [kb.sb("p3stg%d" % i, [128, 1024], F32) for i in range(2)]
            load_cast_weight(wo, lambda c: wo[:, c, :], lambda c: w_out_d[l, c * 128:(c + 1) * 128, :], 8, 1024, stg)
            mx = [kb.sb("mx%d" % i, [128, 1024], BF16) for i in range(2)]
            mT = [kb.sb("mT%d" % i, [128, 8, 128], BF16) for i in range(2)]
            xt = [kb.sb("x3t%d" % i, [128, 1024], F32) for i in range(2)]
            xo = [kb.sb("x3o%d" % i, [128, 1024], F32) for i in range(2)]
            def sA(tt):
                par = tt % 2
                tok = slice(tt * 128, (tt + 1) * 128)
                kb.dma("pool", mx[par][:], mixed_d[tok, :], reads=[r_mixed], writes=[mx[par]], owner=mx[par])
                kb.dma("pool", xt[par][:], xsrc_d[tok, :], reads=[r_xsrc], writes=[xt[par]], owner=xt[par])
                bT = banks[4 + par]
                bTv = bT[:].bitcast(BF16)
                for kc in range(8):
                    kb.op("pe", "transpose", [mx[par], ident], [bT], out=bTv[:, kc * 128:(kc + 1) * 128],
                          in_=mx[par][:, kc * 128:(kc + 1) * 128], identity=ident[:, 0:128])
                kb.op("act", "activation", [bT], [mT[par]], out=mT[par][:].rearrange("p k t -> p (k t)"), in_=bTv,
                      func=AF.Copy)

            def sB(tt):
                par = tt % 2
                tok = slice(tt * 128, (tt + 1) * 128)
                for nb in range(2):
                    bk = banks[2 * par + nb]
                    for kc in range(8):
                        kb.op("pe", "matmul", [mT[par], wo], [bk], out=bk[:, 0:512], lhsT=mT[par][:, kc, :],
                              rhs=wo[:, kc, nb * 512:(nb + 1) * 512], start=(kc == 0), stop=(kc == 7))
                    kb.op("dve", "tensor_tensor", [bk, xt[par]], [xo[par]], out=xo[par][:, nb * 512:(nb + 1) * 512],
                          in0=bk[:, 0:512], in1=xt[par][:, nb * 512:(nb + 1) * 512], op=ALU.add)
                kb.dma("sp", xm_d[tok, :], xo[par][:], reads=[xo[par]], writes=[r_xm], owner=xo[par])

            sA(0)
            wq = list(wchunks)
            per = (len(wq) + NT - 1) // NT
            for tt in range(NT):
                if tt + 1 < NT:
                    sA(tt + 1)
                sB(tt)
                for _ in range(per):
                    if wq:
                        wq.pop(0)()
            while wq:
                wq.pop(0)()
            kb.barrier()
            kb.stack = old

    def phase3b(l, xdst_d, r_xdst, final, wu, wd):
        with ExitStack() as st4:
            old = kb.stack
            kb.stack = st4
            g2 = kb.sb("g2", [128, 1024], F32)
            bcast_load(g2, norm2_d[l:l + 1, :], 1024)
            if final:
                gf = kb.sb("gf", [128, 1024], F32)
                bcast_load(gf, fnorm_d[0:1, :], 1024)
            TB = 2
            xt = [[kb.sb("x4t%d_%d" % (i, j), [128, 1024], F32) for j in range(TB)] for i in range(2)]
            hb = kb.sb("h4b", [128, 1024], BF16)
            hT = [kb.sb("h4T%d" % i, [128, 8, TB * 128], BF16) for i in range(2)]
            uT = [kb.sb("u4T%d" % i, [128, TB * 128], BF16) for i in range(3)]
            rT = [kb.sb("r4T%d" % i, [128, TB * 128], F32) for i in range(2)]
            junk = kb.sb("junk4", [128, 1024], F32)
            ss = kb.sb("ss4", [128, 2], F32)
            rstd = kb.sb("rstd4", [128, 2], F32)
            nblk = NT // TB
            def pre(b):
                par = b % 2
                for j in range(TB):
                    tok = slice((b * TB + j) * 128, (b * TB + j + 1) * 128)
                    x = xt[par][j]
                    kb.dma("pool", x[:], xm_d[tok, :], reads=[r_xm], writes=[x], owner=x)
                    rmsnorm_rstd(x, x[:], 1024, ss, rstd, junk)
                    kb.op("dve", "scalar_tensor_tensor", [x, rstd, g2], [hb], out=hb[:], in0=x[:],
                          scalar=rstd[:, 0:1], in1=g2[:], op0=ALU.mult, op1=ALU.mult)
                    bT = banks[6 + j % 2]
                    bTv = bT[:].bitcast(BF16)
                    for kc in range(8):
                        kb.op("pe", "transpose", [hb, ident], [bT], out=bTv[:, kc * 128:(kc + 1) * 128],
                              in_=hb[:, kc * 128:(kc + 1) * 128], identity=ident[:, 0:128])
                    kb.op("act", "activation", [bT], [hT[par]], out=hT[par][:, :, j * 128:(j + 1) * 128],
                          in_=bTv.rearrange("p (k t) -> p k t", k=8), func=AF.Copy)
            def main(b):
                par = b % 2
                accs = [banks[0], banks[1], banks[2], banks[3]]
                def emit_up(fc):
                    bu = banks[4 + fc % 2]
                    for kc in range(8):
                        kb.op("pe", "matmul", [wu, hT[par]], [bu], out=bu[:, 0:TB * 128],
                              lhsT=wu[:, kc, fc * 128:(fc + 1) * 128], rhs=hT[par][:, kc, :], start=(kc == 0),
                              stop=(kc == 7))
                    u = uT[fc % 3]
                    rr = rT[fc % 2]
                    kb.op("act", "activation", [bu], [rr], out=rr[:], in_=bu[:, 0:TB * 128], func=AF.Relu)
                    kb.op("dve" if fc % 2 == 0 else "pool", "tensor_tensor", [rr], [u], out=u[:], in0=rr[:],
                          in1=rr[:], op=ALU.mult)

                def emit_down(fc):
                    u = uT[fc % 3]
                    for j in range(TB):
                        for nb in range(2):
                            acc = accs[j * 2 + nb]
                            kb.op("pe", "matmul", [u, wd], [acc], out=acc[:, 0:512], lhsT=u[:, j * 128:(j + 1) * 128],
                                  rhs=wd[:, fc, nb * 512:(nb + 1) * 512], start=(fc == 0), stop=(fc == 31))

                emit_up(0)
                for fc in range(32):
                    if fc + 1 < 32:
                        emit_up(fc + 1)
                    emit_down(fc)
                for j in range(TB):
                    tok = slice((b * TB + j) * 128, (b * TB + j + 1) * 128)
                    o = xt[par][j]
                    for nb in range(2):
                        acc = accs[j * 2 + nb]
                        kb.op("dve", "tensor_tensor", [acc, xt[par][j]], [o], out=o[:, nb * 512:(nb + 1) * 512],
                              in0=acc[:, 0:512], in1=xt[par][j][:, nb * 512:(nb + 1) * 512], op=ALU.add)
                    if final:
                        kb.op("act", "activation", [o], [junk, ss], out=junk[:], in_=o[:], func=AF.Square,
                              accum_out=ss[:, 1:2])
                        kb.op("dve", "tensor_scalar", [ss], [rstd], out=rstd[:, 1:2], in0=ss[:, 1:2],
                              scalar1=1.0 / 1024, scalar2=EPS, op0=ALU.mult, op1=ALU.add)
                        kb.op("act", "activation", [rstd], [rstd], out=rstd[:, 1:2], in_=rstd[:, 1:2], func=AF.Sqrt)
                        kb.op("dve", "reciprocal", [rstd], [rstd], out=rstd[:, 1:2], in_=rstd[:, 1:2])
                        kb.op("dve", "scalar_tensor_tensor", [o, rstd, gf], [o], out=o[:], in0=o[:],
                              scalar=rstd[:, 1:2], in1=gf[:], op0=ALU.mult, op1=ALU.mult)
                    kb.dma("sp", xdst_d[tok, :], o[:], reads=[o], writes=[r_xdst], owner=o)

            pre(0)
            for b in range(nblk):
                if b + 1 < nblk:
                    pre(b + 1)
                main(b)
            kb.barrier()
            kb.stack = old

    cur_d, cur_r = x_in, r_xin
    for l in range(depth):
        phase1(l, cur_d, cur_r)
        for g in "ABCD":
            attention(l, g)
        with ExitStack() as stw:
            oldw = kb.stack
            kb.stack = stw
            wu = kb.sb("wu", [128, 8, D_FF], BF16)
            wd = kb.sb("wd", [128, 32, 1024], BF16)
            wstg = [kb.sb("p4stg%d" % i, [128, 2048], F32) for i in range(2)]
            phase3a(l, cur_d, cur_r, mlp_weight_chunks(l, wu, wd, wstg))
            last = (l == depth - 1)
            if last:
                phase3b(l, out_d, r_out, True, wu, wd)
            else:
                phase3b(l, xs[l % 2], r_xs[l % 2], False, wu, wd)
                cur_d, cur_r = xs[l % 2], r_xs[l % 2]
            kb.stack = oldw
    kb.wait_all("sp", [r_out])
    kb.wait_all("pool", [r_out])
    kb.barrier()
    print("KB: nsem=%d instr=%s" % (kb.nsem, {n: len(E.prog) for n, E in kb.engs.items()}), flush=True)
    kb.replay()
    stack.close()
    return nc


def host_consts(S):
    pos = np.arange(S, dtype=np.float32)

    def tables(d):
        half = d // 2
        inv = (1.0 / (10000.0 ** (np.arange(0, half, dtype=np.float32) * 2.0 / d))).astype(np.float32)
        ang = pos[:, None] * inv[None, :]
        return np.cos(ang).astype(np.float32), np.sin(ang).astype(np.float32)

    c64, s64 = tables(64)
    c32, s32 = tables(32)
    cm = np.zeros((128, 5 * 512), np.float32)
    eye = np.eye(128, dtype=np.float32)
    kk = np.arange(128)[:, None]
    qq = np.arange(128)[None, :]
    mc = np.where(kk > qq, -BIGM, 0.0).astype(np.float32)
    mp = np.where(kk <= qq, -BIGM, 0.0).astype(np.float32)
    for h in range(4):
        cm[:, h * 128:(h + 1) * 128] = eye
        cm[:, 512 + h * 128:512 + (h + 1) * 128] = mc
        cm[:, 1024 + h * 128:1024 + (h + 1) * 128] = mp
    cm[:, 1536:1664] = np.where(qq > kk, NEG_S, 0.0)
    cm[:, 1664:1792] = (kk <= qq).astype(np.float32)
    cm[:, 1792:1920] = (kk == 127).astype(np.float32) * np.ones((1, 128), np.float32)
    cm[:, 2048:2176] = eye
    return c64, s64, c32, s32, cm


_CACHE = {}


def kernel(x, norm1, w_in, mla_q_norm, mla_kv_norm, mla_w_uq, mla_w_ukv, swa_sinks, fox_b_f, w_out, norm2, w_up,
           w_down, final_norm, _depth=None, _ncores=None, _debug=False):
    x = np.asarray(x, dtype=np.float32)
    B, S, _ = x.shape
    depth = int(_depth) if _depth is not None else int(np.asarray(w_in).shape[0])
    ncores = int(_ncores) if _ncores is not None else B
    f = lambda a: np.ascontiguousarray(np.asarray(a, dtype=np.float32))
    key = (S, depth, _debug)
    if key not in _CACHE:
        _CACHE[key] = build_program(S=S, depth=depth, debug=_debug)
    nc = _CACHE[key]
    c64, s64, c32, s32, cm = host_consts(S)
    shared = {
        "w_in": f(np.asarray(w_in)[:depth][:, :, PERM]),
        "w_uq": f(np.asarray(mla_w_uq)[:depth]),
        "w_ukv": f(np.asarray(mla_w_ukv)[:depth]),
        "w_out": f(np.asarray(w_out)[:depth]),
        "w_up": f(np.asarray(w_up)[:depth]),
        "w_down": f(np.asarray(w_down)[:depth]),
        "norm1": f(np.asarray(norm1)[:depth]),
        "norm2": f(np.asarray(norm2)[:depth]),
        "gq": f(np.asarray(mla_q_norm)[:depth]),
        "gkv": f(np.asarray(mla_kv_norm)[:depth]),
        "sinks": f(np.asarray(swa_sinks)[:depth]),
        "foxb": f(np.asarray(fox_b_f)[:depth]),
        "fnorm": f(np.asarray(final_norm).reshape(1, -1)),
        "ropet": np.ascontiguousarray(np.concatenate([c64, s64, c32, s32], axis=1)), "cmat": cm,
    }
    in_maps = []
    for b in range(ncores):
        m = dict(shared)
        m["x"] = f(x[b])
        in_maps.append(m)
    res = run_bass_kernel_spmd(nc, in_maps, core_ids=list(range(ncores)))
    out = np.stack([np.asarray(r["out"], dtype=np.float32) for r in res.results], axis=0)
    if _debug:
        return out, res.results
    return out
```

```python
import numpy as np
from contextlib import ExitStack
import concourse.bass as bass
import concourse.mybir as mybir
from concourse.bass_utils import run_bass_kernel_spmd

F32 = mybir.dt.float32
BF16 = mybir.dt.bfloat16
ALU = mybir.AluOpType
AF = mybir.ActivationFunctionType
AX = mybir.AxisListType

D_MODEL = 1024
DEPTH = 4
SEQ = 4096
IN_WIDTH = 2764
D_FF = 4096
EPS = 1e-6
BIGM = 262144.0
NEG_S = -1.0e30
N_BISECT = 15

_ORIG = dict(a_cq=(0, 256), a_ckv=(256, 384), a_kr=(384, 416), b_q=(416, 672), b_k=(672, 800), b_v=(800, 928),
             c_q=(928, 1184), c_k=(1184, 1440), c_v=(1440, 1696), c_qi=(1696, 1952), c_ki=(1952, 1984),
             c_w=(1984, 1992), d_q=(1992, 2248), d_k=(2248, 2504), d_v=(2504, 2760), d_f=(2760, 2764))
_ORDER = ["b_q", "b_k", "c_q", "c_k", "c_qi", "c_ki", "a_kr", "b_v", "c_v", "d_q", "d_k", "d_v",
          "a_cq", "a_ckv", "c_w", "d_f"]
COL = {}
_perm = []
_o = 0
for _n in _ORDER:
    _a, _b = _ORIG[_n]
    COL[_n] = (_o, _o + (_b - _a))
    _perm.extend(range(_a, _b))
    _o += _b - _a
PERM = np.array(_perm, dtype=np.int64)
assert _o == IN_WIDTH


class Res:
    __slots__ = ("name", "w", "r", "sem", "cnt", "psum")

    def __init__(self, name):
        self.name = name
        self.psum = False
        self.w = {}
        self.r = {}
        self.sem = None
        self.cnt = 0


class Tile:
    def __init__(self, t, res):
        self.t = t
        self.res = res

    def __getitem__(self, k):
        return self.t[k]


class Eng:
    def __init__(self, name):
        self.name = name
        self.sem = None
        self.cnt = 0
        self.seen = {}
        self.prog = []


class KB:
    def __init__(self, nc, stack):
        self.nc = nc
        self.stack = stack
        self.gstack = stack
        self.engs = {n: Eng(n) for n in ("pe", "act", "dve", "pool", "sp")}
        self.nsem = 0
        self.sems = []
        for e in self.engs.values():
            e.sem = self.new_sem(e.name)
        self.all_res = []
        self.named = {}
        self.uid = 0

    def new_sem(self, name):
        self.nsem += 1
        h = self.gstack.enter_context(self.nc.semaphore("s%d_%s" % (self.nsem, name)))
        self.sems.append(h)
        return h

    def res(self, name):
        r = Res(name)
        self.all_res.append(r)
        return r

    def sb(self, name, shape, dt):
        self.uid += 1
        t = self.stack.enter_context(self.nc.sbuf_tensor("%s_u%d" % (name, self.uid), list(shape), dt))
        return Tile(t, self.res(name))

    def ps(self, name, shape, dt):
        t = self.gstack.enter_context(self.nc.psum_tensor(name, list(shape), dt))
        r = self.res(name)
        r.psum = True
        return Tile(t, r)

    def barrier(self):
        toks = [(E.sem, E.cnt) for E in self.engs.values() if E.cnt > 0]
        toks += [(sem, cnt) for (sem, cnt) in self.named.values()]
        for E in self.engs.values():
            waits = []
            for (sem, val) in toks:
                k = id(sem)
                if sem is E.sem or E.seen.get(k, 0) >= val:
                    continue
                waits.append((sem, val))
                E.seen[k] = val

            def emit(eng, waits=waits):
                for (s_, v) in waits:
                    eng.wait_ge(s_, v)

            E.prog.append(emit)

    @staticmethod
    def _r(x):
        return x.res if isinstance(x, Tile) else x

    def _deps(self, E, reads, writes, is_dma):
        deps = {}

        def add(d, skip_dma=False):
            for k, (sem, val, isd) in d.items():
                if skip_dma and isd:
                    continue
                if k not in deps or deps[k][1] < val:
                    deps[k] = (sem, val)

        for r in reads:
            add(self._r(r).w)
            if self._r(r).psum:
                add(self._r(r).r)
        for w in writes:
            add(self._r(w).w, skip_dma=is_dma)
            add(self._r(w).r)
        waits = []
        for k, (sem, val) in deps.items():
            if E.seen.get(k, 0) >= val:
                continue
            waits.append((sem, val))
            E.seen[k] = val
        return waits

    def op(self, en, fname, reads=(), writes=(), **kw):
        E = self.engs[en]
        reads = [self._r(x) for x in reads]
        writes = [self._r(x) for x in writes]
        own = id(E.sem)
        waits = self._deps(E, reads, writes, False)
        if en == "pe":
            waits = [(s, v) for (s, v) in waits if id(s) != own]
        if E.cnt >= 60000:
            E.sem = self.new_sem(E.name)
            E.cnt = 0
        E.cnt += 1
        sem, cnt = E.sem, E.cnt
        key = id(sem)
        tok = (sem, cnt, False)

        def emit(eng, waits=waits, fname=fname, kw=kw, sem=sem):
            for (s, v) in waits:
                eng.wait_ge(s, v)
            getattr(eng, fname)(**kw).then_inc(sem, 1)

        E.prog.append(emit)
        for r in reads:
            if key not in r.r or r.r[key][1] < cnt:
                r.r[key] = tok
        for w in writes:
            w.w = {key: tok}
            w.r = {}

    def dma(self, en, out, in_, reads=(), writes=(), owner=None, **kw):
        E = self.engs[en]
        reads = [self._r(x) for x in reads]
        writes = [self._r(x) for x in writes]
        owner = self._r(owner)
        waits = self._deps(E, reads, writes, True)
        okey = owner.name + ("@sw" if en == "pool" else "")
        if okey in self.named:
            sem, cnt = self.named[okey]
        else:
            sem, cnt = self.new_sem("d_" + okey.replace("@", "_")), 0
        cnt += 16
        assert cnt < 65000, okey
        self.named[okey] = (sem, cnt)
        key = id(sem)
        tok = (sem, cnt, True)

        def emit(eng, waits=waits, sem=sem, out=out, in_=in_, kw=kw):
            for (s, v) in waits:
                eng.wait_ge(s, v)
            eng.dma_start(out=out, in_=in_, **kw).then_inc(sem, 16)

        E.prog.append(emit)
        for r in reads:
            r.r[key] = tok
        for w in writes:
            w.w[key] = tok

    def finish(self):
        self.barrier()
        sems = list(self.sems)
        done = self.new_sem("done")
        for n, E in self.engs.items():
            if n == "pool":
                continue
            E.prog.append(lambda eng, done=done: eng.sem_inc(done, 1))

        def emit(eng, sems=sems, done=done):
            eng.wait_ge(done, 4)
            for s_ in sems:
                eng.sem_clear(s_)
            eng.sem_clear(done)

        self.engs["pool"].prog.append(emit)

    def wait_all(self, en, ress):
        E = self.engs[en]
        waits = self._deps(E, [self._r(x) for x in ress], [], False)

        def emit(eng, waits=waits):
            for (s, v) in waits:
                eng.wait_ge(s, v)

        E.prog.append(emit)

    def replay(self):
        nc = self.nc
        with nc.Block() as block:
            @block.sync
            def _(e):
                for f in self.engs["sp"].prog:
                    f(e)

            @block.tensor
            def _(e):
                for f in self.engs["pe"].prog:
                    f(e)

            @block.scalar
            def _(e):
                for f in self.engs["act"].prog:
                    f(e)

            @block.vector
            def _(e):
                for f in self.engs["dve"].prog:
                    f(e)

            @block.gpsimd
            def _(e):
                for f in self.engs["pool"].prog:
                    f(e)


def build_program(S=SEQ, depth=DEPTH, topk=None, debug=False):
    NT = S // 128
    if topk is None:
        topk = min(256, S // 4)
    nc = bass.Bass("TRN2", target_bir_lowering=False)
    stack = ExitStack()
    kb = KB(nc, stack)

    def din(name, shape, dt=F32):
        return nc.dram_tensor(name, list(shape), dt, kind="ExternalInput").ap()

    def dscr(name, shape, dt):
        kind = "ExternalOutput" if debug else "Internal"
        return nc.dram_tensor(name, list(shape), dt, kind=kind).ap()

    L = depth
    x_in = din("x", [S, D_MODEL])
    w_in_d = din("w_in", [L, D_MODEL, IN_WIDTH])
    w_uq_d = din("w_uq", [L, 256, 384])
    w_ukv_d = din("w_ukv", [L, 128, 512])
    w_out_d = din("w_out", [L, 1024, 1024])
    w_up_d = din("w_up", [L, 1024, D_FF])
    w_down_d = din("w_down", [L, D_FF, 1024])
    norm1_d = din("norm1", [L, 1024])
    norm2_d = din("norm2", [L, 1024])
    gq_d = din("gq", [L, 256])
    gkv_d = din("gkv", [L, 128])
    sinks_d = din("sinks", [L, 4])
    foxb_d = din("foxb", [L, 4])
    fnorm_d = din("fnorm", [1, 1024])
    ropet_d = din("ropet", [S, 96])
    cmat_d = din("cmat", [128, 5 * 512])
    out_d = nc.dram_tensor("out", [S, D_MODEL], F32, kind="ExternalOutput").ap()

    xs = [dscr("xs0", [S, D_MODEL], F32), dscr("xs1", [S, D_MODEL], F32)]
    xm_d = dscr("xm", [S, D_MODEL], F32)
    mixed_d = dscr("mixed", [S, D_MODEL], BF16)
    DKA = dict(A=96, B=64, C=64, D=68)
    HK = dict(A=4, B=2, C=4, D=4)
    qT_d = {g: dscr("qT_" + g, [DKA[g], 4, S], BF16) for g in "ABCD"}
    kT_d = {g: dscr("kT_" + g, [DKA[g], HK[g], S], BF16) for g in "ABCD"}
    v_d = {g: dscr("v_" + g, [S, HK[g], 65], BF16) for g in "ABCD"}
    qiT_d = dscr("qiT", [32, 8, S], BF16)
    kiT_d = dscr("kiT", [32, S], BF16)
    wi_d = dscr("wi", [S, 8], F32)

    R = kb.res
    r_xin = R("x_in")
    r_xs = [R("xs0"), R("xs1")]
    r_xm = R("xm")
    r_mixed = R("mixed")
    r_qT = {g: R("qT" + g) for g in "ABCD"}
    r_kT = {g: R("kT" + g) for g in "ABCD"}
    r_v = {g: R("v" + g) for g in "ABCD"}
    r_qiT, r_kiT, r_wi = R("qiT"), R("kiT"), R("wi")
    r_out = R("out")
    r_const = R("constin")

    banks = [kb.ps("bank%d" % i, [128, 512], F32) for i in range(8)]

    cm_f = kb.sb("cm_f", [128, 5 * 512], F32)
    kb.dma("sp", cm_f[:], cmat_d[:, :], reads=[r_const], writes=[cm_f], owner=cm_f)
    ident4 = kb.sb("ident4", [128, 512], BF16)
    maskc4 = kb.sb("maskc4", [128, 512], BF16)
    maskp4 = kb.sb("maskp4", [128, 512], BF16)
    kb.op("dve", "tensor_copy", [cm_f], [ident4], out=ident4[:], in_=cm_f[:, 0:512])
    kb.op("dve", "tensor_copy", [cm_f], [maskc4], out=maskc4[:], in_=cm_f[:, 512:1024])
    kb.op("dve", "tensor_copy", [cm_f], [maskp4], out=maskp4[:], in_=cm_f[:, 1024:1536])
    ident = ident4
    negS = cm_f[:, 1536:1664]
    TRI = cm_f[:, 1664:1792]
    LAST = cm_f[:, 1792:1920]
    identf = cm_f[:, 2048:2176]

    def bcast_load(tile_, src_row_ap, n):
        kb.dma("sp", tile_[:], src_row_ap.to_broadcast([128, n]), reads=[r_const], writes=[tile_], owner=tile_)

    def load_cast_weight(dst_tile, dst_ap_fn, src_ap_fn, nchunks, ncols, stg, engs=("dve", "act")):
        for c in range(nchunks):
            st = stg[c % len(stg)]
            kb.dma("sp", st[:, 0:ncols], src_ap_fn(c), reads=[r_const], writes=[st], owner=st)
            en = engs[c % len(engs)]
            if en == "act":
                kb.op("act", "activation", [st], [dst_tile], out=dst_ap_fn(c), in_=st[:, 0:ncols], func=AF.Copy)
            else:
                kb.op(en, "tensor_copy", [st], [dst_tile], out=dst_ap_fn(c), in_=st[:, 0:ncols])

    def rmsnorm_rstd(src_tile, src_ap, n, ss, rstd, junk, lnexp=False):
        kb.op("act", "activation", [src_tile], [junk, ss], out=junk[:, 0:n], in_=src_ap, func=AF.Square,
              accum_out=ss[:, 0:1])
        kb.op("dve", "tensor_scalar", [ss], [rstd], out=rstd[:, 0:1], in0=ss[:, 0:1], scalar1=1.0 / n,
              scalar2=EPS, op0=ALU.mult, op1=ALU.add)
        if lnexp:
            kb.op("act", "activation", [rstd], [rstd], out=rstd[:, 0:1], in_=rstd[:, 0:1], func=AF.Ln)
            kb.op("act", "activation", [rstd], [rstd], out=rstd[:, 0:1], in_=rstd[:, 0:1], func=AF.Exp, scale=-0.5)
            return
        kb.op("act", "activation", [rstd], [rstd], out=rstd[:, 0:1], in_=rstd[:, 0:1], func=AF.Sqrt)
        kb.op("dve", "reciprocal", [rstd], [rstd], out=rstd[:, 0:1], in_=rstd[:, 0:1])

    def rope(src_tile, src4, dst_tile, dst4, cos_t, sin_t, nh, half, tmp, tmpb):
        (cos_t, co), (sin_t, so) = cos_t, sin_t
        cb = cos_t[:, co:co + half].unsqueeze(1).unsqueeze(1).to_broadcast([128, nh, 2, half])
        sb_ = sin_t[:, so:so + half].unsqueeze(1).unsqueeze(1).to_broadcast([128, nh, 2, half])
        n = nh * 2 * half
        tc = tmp[:, 0:n].rearrange("p (h two d) -> p h two d", h=nh, two=2)
        ts = tmpb[:, 0:n].rearrange("p (h two d) -> p h two d", h=nh, two=2)
        kb.op("dve", "tensor_tensor", [src_tile, cos_t], [tmp], out=tc, in0=src4, in1=cb, op=ALU.mult)
        kb.op("pool", "tensor_tensor", [src_tile, sin_t], [tmpb], out=ts, in0=src4, in1=sb_, op=ALU.mult)
        kb.op("dve", "tensor_tensor", [tmp, tmpb], [dst_tile], out=dst4[:, :, 0, :], in0=tc[:, :, 0, :],
              in1=ts[:, :, 1, :], op=ALU.subtract)
        kb.op("dve", "tensor_tensor", [tmp, tmpb], [dst_tile], out=dst4[:, :, 1, :], in0=tc[:, :, 1, :],
              in1=ts[:, :, 0, :], op=ALU.add)

    def phase1(l, xsrc_d, r_xsrc):
        with ExitStack() as st1:
            old = kb.stack
            kb.stack = st1
            win = kb.sb("win", [128, 8, IN_WIDTH], BF16)
            wuq = kb.sb("wuq", [128, 2, 384], BF16)
            wukv = kb.sb("wukv", [128, 512], BF16)
            stg = [kb.sb("p1stg%d" % i, [128, IN_WIDTH], F32) for i in range(2)]
            load_cast_weight(win, lambda c: win[:, c, :], lambda c: w_in_d[l, c * 128:(c + 1) * 128, :], 8,
                             IN_WIDTH, stg)
            load_cast_weight(wuq, lambda c: wuq[:, c, :], lambda c: w_uq_d[l, c * 128:(c + 1) * 128, :], 2, 384, stg)
            load_cast_weight(wukv, lambda c: wukv[:, :], lambda c: w_ukv_d[l, :, :], 1, 512, stg)
            g1 = kb.sb("g1", [128, 1024], F32)
            gq = kb.sb("gq", [128, 256], F32)
            gkv = kb.sb("gkv", [128, 128], F32)
            fb = kb.sb("fb", [128, 4], F32)
            bcast_load(g1, norm1_d[l:l + 1, :], 1024)
            bcast_load(gq, gq_d[l:l + 1, :], 256)
            bcast_load(gkv, gkv_d[l:l + 1, :], 128)
            bcast_load(fb, foxb_d[l:l + 1, :], 4)
            nfb = kb.sb("nfb", [128, 4], F32)
            kb.op("dve", "tensor_scalar", [fb], [nfb], out=nfb[:], in0=fb[:], scalar1=-1.0, scalar2=None,
                  op0=ALU.mult)

            xt = [kb.sb("xt%d" % i, [128, 1024], F32) for i in range(2)]
            rt = [kb.sb("rt%d" % i, [128, 96], F32) for i in range(2)]
            junk = kb.sb("junk", [128, 1024], F32)
            ss = kb.sb("ss", [128, 4], F32)
            rstd = kb.sb("rstd", [128, 4], F32)
            hb = kb.sb("hb", [128, 1024], BF16)
            hT = kb.sb("hT", [128, 8, 128], BF16)
            projs = [kb.sb("proj%d" % i, [128, IN_WIDTH], F32) for i in range(2)]
            junk2 = kb.sb("junk2", [128, 384], F32)
            ss2 = kb.sb("ss2", [128, 4], F32)
            rstd2 = kb.sb("rstd2", [128, 4], F32)
            r64 = kb.sb("r64", [128, 14, 64], BF16)
            r32 = kb.sb("r32", [128, 10, 32], BF16)
            tmp = kb.sb("tmp", [128, 896], F32)
            tmpb = kb.sb("tmpb", [128, 896], F32)
            tmp2 = kb.sb("tmp2", [128, 320], F32)
            tmp2b = kb.sb("tmp2b", [128, 320], F32)
            tmp3 = kb.sb("tmp3", [128, 128], F32)
            tmp3a = kb.sb("tmp3a", [128, 128], F32)
            tmp3b = kb.sb("tmp3b", [128, 128], F32)
            cqn = kb.sb("cqn", [128, 384], BF16)
            cqnT = kb.sb("cqnT", [128, 3, 128], BF16)
            qa = kb.sb("qa", [128, 4, 96], BF16)
            ka = kb.sb("ka", [128, 4, 96], BF16)
            qd = kb.sb("qd", [128, 4, 68], BF16)
            kd = kb.sb("kd", [128, 4, 68], BF16)
            vst = {g: [kb.sb("vst%s%d" % (g, i), [128, HK[g], 65], BF16) for i in range(2)] for g in "ABCD"}
            for g in "ABCD":
                for i in range(2):
                    kb.op("pool", "memset", [], [vst[g][i]], ap=vst[g][i][:], constant=1.0)
            kb.op("pool", "memset", [], [qd], ap=qd[:], constant=1.0)
            kb.op("pool", "memset", [], [kd], ap=kd[:], constant=1.0)
            logf = kb.sb("logf", [128, 4], F32)
            cum = [kb.sb("cum%d" % i, [128, 4], F32) for i in range(2)]
            kb.op("pool", "memset", [], [cum[1]], ap=cum[1][:], constant=0.0)
            c8 = kb.sb("c8", [128, 4], F32)
            cp = kb.sb("cp", [128, 3, 4], BF16)
            cr = kb.sb("cr", [128, 4], F32)
            wst = [kb.sb("wst%d" % i, [128, 8], F32) for i in range(2)]
            tst = [[kb.sb("tst%d_%d" % (j, i), [128, 4, 128], BF16) for j in range(9)] for i in range(2)]

            def stage1(tt):
                par = tt % 2
                tok = slice(tt * 128, (tt + 1) * 128)
                proj = projs[par]
                x = xt[par]
                kb.dma("pool", x[:], xsrc_d[tok, :], reads=[r_xsrc], writes=[x], owner=x)
                kb.dma("pool", rt[par][:], ropet_d[tok, :], reads=[r_const], writes=[rt[par]], owner=rt[par])
                rmsnorm_rstd(x, x[:], 1024, ss, rstd, junk, lnexp=True)
                kb.op("dve", "scalar_tensor_tensor", [x, rstd, g1], [hb], out=hb[:], in0=x[:], scalar=rstd[:, 0:1],
                      in1=g1[:], op0=ALU.mult, op1=ALU.mult)
                bT = banks[6]
                bTv = bT[:].bitcast(BF16)
                for kc in range(8):
                    kb.op("pe", "transpose", [hb, ident], [bT], out=bTv[:, kc * 128:(kc + 1) * 128],
                          in_=hb[:, kc * 128:(kc + 1) * 128], identity=ident[:, 0:128])
                kb.op("act", "activation", [bT], [hT], out=hT[:].rearrange("p k t -> p (k t)"), in_=bTv,
                      func=AF.Copy)
                for nb in range(6):
                    c0, c1 = nb * 512, min((nb + 1) * 512, IN_WIDTH)
                    for kc in range(8):
                        kb.op("pe", "matmul", [hT, win], [banks[nb]], out=banks[nb][:, 0:c1 - c0], lhsT=hT[:, kc, :],
                              rhs=win[:, kc, c0:c1], start=(kc == 0), stop=(kc == 7))
                    if nb % 2 == 0:
                        kb.op("act", "activation", [banks[nb]], [proj], out=proj[:, c0:c1],
                              in_=banks[nb][:, 0:c1 - c0], func=AF.Copy)
                    else:
                        kb.op("dve", "tensor_copy", [banks[nb]], [proj], out=proj[:, c0:c1],
                              in_=banks[nb][:, 0:c1 - c0])
            def stage2(tt):
                par = tt % 2
                tok = slice(tt * 128, (tt + 1) * 128)
                proj = projs[par]
                c64, s64, c32, s32 = (rt[par], 0), (rt[par], 32), (rt[par], 64), (rt[par], 80)
                bT = banks[7]
                bTv = bT[:].bitcast(BF16)
                rope(proj, proj[:, 0:896].rearrange("p (h two d) -> p h two d", h=14, two=2), r64,
                     r64[:].rearrange("p h (two d) -> p h two d", two=2), c64, s64, 14, 32, tmp, tmpb)
                rope(proj, proj[:, 896:1216].rearrange("p (h two d) -> p h two d", h=10, two=2), r32,
                     r32[:].rearrange("p h (two d) -> p h two d", two=2), c32, s32, 10, 16, tmp2, tmp2b)
                vs = {g: vst[g][par] for g in "ABCD"}
                o = COL["b_v"][0]
                kb.op("pool", "tensor_copy", [proj], [vs["B"]], out=vs["B"][:, :, 0:64],
                      in_=proj[:, o:o + 128].rearrange("p (h d) -> p h d", h=2))
                o = COL["c_v"][0]
                kb.op("act", "activation", [proj], [vs["C"]], out=vs["C"][:, :, 0:64],
                      in_=proj[:, o:o + 256].rearrange("p (h d) -> p h d", h=4), func=AF.Copy)
                o = COL["d_v"][0]
                kb.op("act", "activation", [proj], [vs["D"]], out=vs["D"][:, :, 0:64],
                      in_=proj[:, o:o + 256].rearrange("p (h d) -> p h d", h=4), func=AF.Copy)
                o = COL["a_cq"][0]
                kb.op("act", "activation", [proj], [junk2, ss2], out=junk2[:, 0:256], in_=proj[:, o:o + 256],
                      func=AF.Square, accum_out=ss2[:, 1:2])
                kb.op("act", "activation", [proj], [junk2, ss2], out=junk2[:, 256:384], in_=proj[:, o + 256:o + 384],
                      func=AF.Square, accum_out=ss2[:, 2:3])
                kb.op("dve", "tensor_scalar", [ss2], [rstd2], out=rstd2[:, 1:2], in0=ss2[:, 1:2], scalar1=1.0 / 256,
                      scalar2=EPS, op0=ALU.mult, op1=ALU.add)
                kb.op("dve", "tensor_scalar", [ss2], [rstd2], out=rstd2[:, 2:3], in0=ss2[:, 2:3], scalar1=1.0 / 128,
                      scalar2=EPS, op0=ALU.mult, op1=ALU.add)
                kb.op("act", "activation", [rstd2], [rstd2], out=rstd2[:, 1:3], in_=rstd2[:, 1:3], func=AF.Ln)
                kb.op("act", "activation", [rstd2], [rstd2], out=rstd2[:, 1:3], in_=rstd2[:, 1:3], func=AF.Exp,
                      scale=-0.5)
                kb.op("dve", "scalar_tensor_tensor", [proj, rstd2, gq], [cqn], out=cqn[:, 0:256],
                      in0=proj[:, o:o + 256], scalar=rstd2[:, 1:2], in1=gq[:], op0=ALU.mult, op1=ALU.mult)
                kb.op("dve", "scalar_tensor_tensor", [proj, rstd2, gkv], [cqn], out=cqn[:, 256:384],
                      in0=proj[:, o + 256:o + 384], scalar=rstd2[:, 2:3], in1=gkv[:], op0=ALU.mult, op1=ALU.mult)
                for kc in range(3):
                    kb.op("pe", "transpose", [cqn, ident], [bT], out=bTv[:, kc * 128:(kc + 1) * 128],
                          in_=cqn[:, kc * 128:(kc + 1) * 128], identity=ident[:, 0:128])
                kb.op("act", "activation", [bT], [cqnT], out=cqnT[:].rearrange("p k t -> p (k t)"),
                      in_=bTv[:, 0:384], func=AF.Copy)
                bq, bkv = banks[0], banks[1]
                for kc in range(2):
                    kb.op("pe", "matmul", [cqnT, wuq], [bq], out=bq[:, 0:384], lhsT=cqnT[:, kc, :], rhs=wuq[:, kc, :],
                          start=(kc == 0), stop=(kc == 1))
                kb.op("pe", "matmul", [cqnT, wukv], [bkv], out=bkv[:, 0:512], lhsT=cqnT[:, 2, :], rhs=wukv[:, :],
                      start=True, stop=True)
                bq3 = bq[:, 0:384].rearrange("p (h d) -> p h d", h=4)
                kb.op("act", "activation", [bq], [qa], out=qa[:, :, 0:64], in_=bq3[:, :, 0:64], func=AF.Copy)
                kb.op("act", "activation", [bq], [tmp3], out=tmp3[:, 0:128].rearrange("p (h d) -> p h d", h=4),
                      in_=bq3[:, :, 64:96], func=AF.Copy)
                rope(tmp3, tmp3[:, 0:128].rearrange("p (h two d) -> p h two d", h=4, two=2), qa,
                     qa[:, :, 64:96].rearrange("p h (two d) -> p h two d", two=2), c32, s32, 4, 16, tmp3a, tmp3b)
                bkv3 = bkv[:, 0:512].rearrange("p (h d) -> p h d", h=4)
                kb.op("act", "activation", [bkv], [ka], out=ka[:, :, 0:64], in_=bkv3[:, :, 0:64], func=AF.Copy)
                kb.op("dve", "tensor_copy", [bkv], [vs["A"]], out=vs["A"][:, :, 0:64], in_=bkv3[:, :, 64:128])
                kb.op("pool", "tensor_copy", [r32], [ka], out=ka[:, :, 64:96],
                      in_=r32[:, 9:10, :].to_broadcast([128, 4, 32]))
                o = COL["d_f"][0]
                kb.op("dve", "scalar_tensor_tensor", [proj, nfb], [logf], out=logf[:], in0=proj[:, o:o + 4],
                      scalar=-1.0, in1=nfb[:], op0=ALU.mult, op1=ALU.add)
                kb.op("act", "activation", [logf], [logf], out=logf[:], in_=logf[:], func=AF.Exp)
                kb.op("act", "activation", [logf], [logf], out=logf[:], in_=logf[:], func=AF.Ln, bias=1.0)
                kb.op("dve", "tensor_scalar", [logf], [logf], out=logf[:], in0=logf[:], scalar1=-1.0, scalar2=None,
                      op0=ALU.mult)
                bc = banks[2]
                cprev, ccur = cum[(tt + 1) % 2], cum[tt % 2]
                kb.op("pe", "matmul", [cm_f, logf], [bc], out=bc[:, 0:4], lhsT=TRI, rhs=logf[:], start=True,
                      stop=False)
                kb.op("pe", "matmul", [cm_f, cprev], [bc], out=bc[:, 0:4], lhsT=LAST, rhs=cprev[:], start=False,
                      stop=True)
                kb.op("dve", "tensor_copy", [bc], [ccur], out=ccur[:], in_=bc[:, 0:4])
                kb.op("dve", "tensor_scalar", [ccur], [c8], out=c8[:], in0=ccur[:], scalar1=8.0, scalar2=None,
                      op0=ALU.mult)
                kb.op("dve", "tensor_copy", [c8], [cp], out=cp[:, 0, :], in_=c8[:])
                kb.op("dve", "tensor_tensor", [c8, cp], [cr], out=cr[:], in0=c8[:], in1=cp[:, 0, :], op=ALU.subtract)
                kb.op("dve", "tensor_copy", [cr], [cp], out=cp[:, 1, :], in_=cr[:])
                kb.op("dve", "tensor_tensor", [cr, cp], [cr], out=cr[:], in0=cr[:], in1=cp[:, 1, :], op=ALU.subtract)
                kb.op("dve", "tensor_copy", [cr], [cp], out=cp[:, 2, :], in_=cr[:])
                o = COL["d_q"][0]
                kb.op("pool", "tensor_copy", [proj], [qd], out=qd[:, :, 0:64],
                      in_=proj[:, o:o + 256].rearrange("p (h d) -> p h d", h=4))
                kb.op("pool", "tensor_copy", [cp], [qd], out=qd[:, :, 64:65], in_=cp[:, 0, :].unsqueeze(2))
                o = COL["d_k"][0]
                kb.op("pool", "tensor_copy", [proj], [kd], out=kd[:, :, 0:64],
                      in_=proj[:, o:o + 256].rearrange("p (h d) -> p h d", h=4))
                kb.op("dve", "tensor_scalar", [cp], [kd], out=kd[:, :, 65:68], in0=cp[:].rearrange("p c h -> p h c"),
                      scalar1=-1.0, scalar2=None, op0=ALU.mult)
                o = COL["c_w"][0]
                ws = wst[par]
                kb.op("pool", "tensor_copy", [proj], [ws], out=ws[:], in_=proj[:, o:o + 8])
                kb.dma("sp", wi_d[tok, :], ws[:], reads=[ws], writes=[r_wi], owner=ws)
                for g in "ABCD":
                    kb.dma("sp", v_d[g][tok, :, :], vs[g][:], reads=[vs[g]], writes=[r_v[g]], owner=vs[g])
                items = [
                    (qa, [qa[:, h, :] for h in range(4)], 96, qT_d["A"], r_qT["A"]),
                    (ka, [ka[:, h, :] for h in range(4)], 96, kT_d["A"], r_kT["A"]),
                    (r64, [r64[:, h, :] for h in range(0, 4)], 64, qT_d["B"], r_qT["B"]),
                    (r64, [r64[:, h, :] for h in range(4, 6)], 64, kT_d["B"], r_kT["B"]),
                    (r64, [r64[:, h, :] for h in range(6, 10)], 64, qT_d["C"], r_qT["C"]),
                    (r64, [r64[:, h, :] for h in range(10, 14)], 64, kT_d["C"], r_kT["C"]),
                    (qd, [qd[:, h, :] for h in range(4)], 68, qT_d["D"], r_qT["D"]),
                    (kd, [kd[:, h, :] for h in range(4)], 68, kT_d["D"], r_kT["D"]),
                    (r32, [r32[:, h, :] for h in range(0, 4)], 32, qiT_d[:, 0:4, :], r_qiT),
                    (r32, [r32[:, h, :] for h in range(4, 8)], 32, qiT_d[:, 4:8, :], r_qiT),
                    (r32, [r32[:, 8, :]], 32, None, r_kiT),
                ]
                for ii, (src, aps, n, dst, rdst) in enumerate(items):
                    bk = banks[3 + (ii % 3)]
                    bkv_ = bk[:].bitcast(BF16)
                    nh = len(aps)
                    for h, ap in enumerate(aps):
                        kb.op("pe", "transpose", [src, ident], [bk], out=bkv_[0:n, h * 128:(h + 1) * 128], in_=ap,
                              identity=ident[:, 0:128])
                    sg = tst[par][ii % 9] if ii < 9 else tst[par][ii - 9 + 0]
                    en = "act" if ii % 2 == 0 else "dve"
                    if en == "act":
                        kb.op("act", "activation", [bk], [sg], out=sg[0:n, 0:nh, :].rearrange("p h t -> p (h t)"),
                              in_=bkv_[0:n, 0:nh * 128], func=AF.Copy)
                    else:
                        kb.op("dve", "tensor_copy", [bk], [sg], out=sg[0:n, 0:nh, :].rearrange("p h t -> p (h t)"),
                              in_=bkv_[0:n, 0:nh * 128])
                    if dst is None:
                        kb.dma("sp", kiT_d[:, tok], sg[0:32, 0, :], reads=[sg], writes=[rdst], owner=sg)
                    else:
                        kb.dma("sp", dst[:, :, tok], sg[0:n, 0:nh, :], reads=[sg], writes=[rdst], owner=sg)
            stage1(0)
            for tt in range(NT):
                if tt + 1 < NT:
                    stage1(tt + 1)
                stage2(tt)
            kb.barrier()
            kb.stack = old

    def attention(l, g):
        dk = DKA[g]
        hk = HK[g]
        gi = "ABCD".index(g)
        scale = {"A": 96 ** -0.5, "B": 0.125, "C": 0.125, "D": 0.125}[g]
        with ExitStack() as st2:
            old = kb.stack
            kb.stack = st2
            qT = kb.sb("aqT", [dk, 4, S], BF16)
            kT = kb.sb("akT", [dk, hk, S], BF16)
            vv = kb.sb("avv", [128, NT, hk * 65], BF16)
            for h in range(4):
                kb.dma("sp", qT[:, h, :], qT_d[g][:, h, :], reads=[r_qT[g]], writes=[qT], owner=qT)
            for h in range(hk):
                kb.dma("sp", kT[:, h, :], kT_d[g][:, h, :], reads=[r_kT[g]], writes=[kT], owner=kT)
            vsrc = v_d[g].rearrange("(j p) h d -> p j (h d)", p=128)
            for j0 in range(0, NT, 4):
                kb.dma("sp", vv[:, j0:j0 + 4, :], vsrc[:, j0:j0 + 4, :], reads=[r_v[g]], writes=[vv], owner=vv)
            pT = [kb.sb("apT%d" % i, [128, 512], BF16) for i in range(2)]
            osb = [kb.sb("aosb%d" % i, [128, 4, 64], BF16) for i in range(2)]
            den = kb.sb("aden", [128, 4], F32)
            if g == "B":
                esink = kb.sb("esink", [128, 4], F32)
                bcast_load(esink, sinks_d[l:l + 1, :], 4)
                kb.op("act", "activation", [esink], [esink], out=esink[:], in_=esink[:], func=AF.Exp)
            if g == "C":
                qiTs = [kb.sb("cqiT%d" % i, [32, 8, 128], BF16) for i in range(2)]
                kiT = kb.sb("ckiT", [32, S], BF16)
                kb.dma("sp", kiT[:], kiT_d[:, :], reads=[r_kiT], writes=[kiT], owner=kiT)
                wsb = kb.sb("cwsb", [128, NT, 8], F32)
                wsrc = wi_d.rearrange("(j p) h -> p j h", p=128)
                for j0 in range(0, NT, 4):
                    kb.dma("sp", wsb[:, j0:j0 + 4, :], wsrc[:, j0:j0 + 4, :], reads=[r_wi], writes=[wsb], owner=wsb)
                Isbs = [kb.sb("cI%d" % i, [128, S], F32) for i in range(2)]
                Madd = [kb.sb("cMadd%d" % i, [128, S], BF16) for i in range(2)]
                rl = [kb.sb("crl%d" % i, [128, 512], BF16) for i in range(3)]
                dgs = [kb.sb("cdg%d" % i, [128, 8, 128], BF16) for i in range(2)]
                cjunk = kb.sb("cjunk", [128, S], BF16)
                lo = kb.sb("clo", [128, 1], F32)
                stp = kb.sb("cstp", [128, 1], F32)
                cand = kb.sb("ccand", [128, 1], F32)
                cnt = kb.sb("ccnt", [128, 1], F32)
                mm = kb.sb("cmm", [128, 1], F32)
                hi = kb.sb("chi", [128, 1], F32)

            def kts_of(qt):
                if g == "B":
                    return [kt for kt in (qt - 1, qt) if kt >= 0]
                return list(range(qt + 1))

            def c_scores(qt):
                qs = slice(qt * 128, (qt + 1) * 128)
                Lk = 128 * (qt + 1)
                nblk = (Lk + 511) // 512
                Isb = Isbs[qt % 2]
                qiT = qiTs[qt % 2]
                dg = dgs[qt % 2]
                kb.dma("pool", qiT[:], qiT_d[:, :, qs], reads=[r_qiT], writes=[qiT], owner=qiT)
                kb.op("pool", "tensor_tensor", [ident4, wsb], [dg], out=dg[:],
                      in0=ident4[:, 0:128].unsqueeze(1).to_broadcast([128, 8, 128]),
                      in1=wsb[:, qt, :].unsqueeze(2).to_broadcast([128, 8, 128]), op=ALU.mult)
                hc = 0
                for kbk in range(nblk):
                    k0, k1 = kbk * 512, min((kbk + 1) * 512, Lk)
                    n = k1 - k0
                    bacc = banks[6 + (kbk % 2)]
                    pend = None
                    for hh in range(8):
                        bi = banks[4 + (hc % 2)]
                        r_ = rl[hc % 3]
                        kb.op("pe", "matmul", [qiT, kiT], [bi], out=bi[:, 0:n], lhsT=qiT[:, hh, :],
                              rhs=kiT[:, k0:k1], start=True, stop=True)
                        kb.op("act", "activation", [bi], [r_], out=r_[:, 0:n], in_=bi[:, 0:n], func=AF.Relu)
                        if pend is not None:
                            ph, pr = pend
                            kb.op("pe", "matmul", [dg, pr], [bacc], out=bacc[:, 0:n], lhsT=dg[:, ph, :], rhs=pr[:, 0:n],
                                  start=(ph == 0), stop=False)
                        pend = (hh, r_)
                        hc += 1
                    ph, pr = pend
                    kb.op("pe", "matmul", [dg, pr], [bacc], out=bacc[:, 0:n], lhsT=dg[:, ph, :], rhs=pr[:, 0:n],
                          start=False, stop=True)
                    kb.op("act", "activation", [bacc], [Isb], out=Isb[:, k0:k1], in_=bacc[:, 0:n], func=AF.Copy)

            def c_select(qt):
                qs = slice(qt * 128, (qt + 1) * 128)
                Lk = 128 * (qt + 1)
                Isb = Isbs[qt % 2]
                madd = Madd[qt % 2]
                kb.op("dve", "tensor_tensor", [Isb, cm_f], [Isb], out=Isb[:, qs], in0=Isb[:, qs], in1=negS, op=ALU.add)
                if Lk <= topk:
                    kb.op("dve", "tensor_scalar", [Isb], [madd], out=madd[:, 0:Lk], in0=Isb[:, 0:Lk],
                          scalar1=-1.0e29, scalar2=-BIGM, op0=ALU.is_lt, op1=ALU.mult)
                    return
                assert Lk - 128 >= topk
                kb.op("dve", "tensor_reduce", [Isb], [hi], out=hi[:], in_=Isb[:, 0:Lk], axis=AX.X, op=ALU.max)
                kb.op("dve", "tensor_reduce", [Isb], [lo], out=lo[:], in_=Isb[:, 0:Lk - 128], axis=AX.X, op=ALU.min)
                kb.op("dve", "tensor_tensor", [hi, lo], [stp], out=stp[:], in0=hi[:], in1=lo[:], op=ALU.subtract)
                for it in range(N_BISECT):
                    f = 0.5 ** (it + 1)
                    kb.op("dve", "scalar_tensor_tensor", [stp, lo], [cand], out=cand[:], in0=stp[:], scalar=f,
                          in1=lo[:], op0=ALU.mult, op1=ALU.add)
                    kb.op("dve", "tensor_scalar", [Isb, cand], [cjunk, cnt], out=cjunk[:, 0:Lk], in0=Isb[:, 0:Lk],
                          scalar1=cand[:, 0:1], scalar2=None, op0=ALU.is_ge, op1=ALU.add, accum_out=cnt[:, 0:1])
                    kb.op("dve", "scalar_tensor_tensor", [cnt, stp], [mm], out=mm[:], in0=cnt[:],
                          scalar=float(topk) - 0.5, in1=stp[:], op0=ALU.is_ge, op1=ALU.mult)
                    kb.op("dve", "scalar_tensor_tensor", [mm, lo], [lo], out=lo[:], in0=mm[:], scalar=f, in1=lo[:],
                          op0=ALU.mult, op1=ALU.add)
                kb.op("dve", "tensor_scalar", [Isb, lo], [madd], out=madd[:, 0:Lk], in0=Isb[:, 0:Lk],
                      scalar1=lo[:, 0:1], scalar2=-BIGM, op0=ALU.is_lt, op1=ALU.mult)

            def emit_scores(qt, ki, kt):
                qs = slice(qt * 128, (qt + 1) * 128)
                ks = slice(kt * 128, (kt + 1) * 128)
                bs = banks[ki % 2]
                have_mask = False
                if g == "C":
                    madd = Madd[qt % 2]
                    kb.op("pe", "matmul", [madd, ident4], [bs], out=bs[:, 0:512], lhsT=madd[:, ks],
                          rhs=ident4[:, 0:512], start=True, stop=False, skip_group_check=True)
                    have_mask = True
                elif kt == qt:
                    kb.op("pe", "matmul", [ident4, maskc4], [bs], out=bs[:, 0:512], lhsT=ident4[:, 0:128],
                          rhs=maskc4[:, 0:512], start=True, stop=False, skip_group_check=True)
                    have_mask = True
                elif g == "B":
                    kb.op("pe", "matmul", [ident4, maskp4], [bs], out=bs[:, 0:512], lhsT=ident4[:, 0:128],
                          rhs=maskp4[:, 0:512], start=True, stop=False, skip_group_check=True)
                    have_mask = True
                for h in range(4):
                    kvh = h if hk == 4 else h // 2
                    kb.op("pe", "matmul", [kT, qT], [bs], out=bs[:, h * 128:(h + 1) * 128], lhsT=kT[:, kvh, ks],
                          rhs=qT[:, h, qs], start=((not have_mask) and h == 0), stop=True, skip_group_check=True)
                p = pT[ki % 2]
                kb.op("act", "activation", [bs], [p], out=p[:], in_=bs[:, 0:512], func=AF.Exp, scale=scale)

            def emit_pv(qt, ki, kt, nk):
                bo = banks[2 + (qt % 2)]
                p = pT[ki % 2]
                for h in range(4):
                    kvh = h if hk == 4 else h // 2
                    kb.op("pe", "matmul", [p, vv], [bo], out=bo[:, h * 65:(h + 1) * 65],
                          lhsT=p[:, h * 128:(h + 1) * 128], rhs=vv[:, kt, kvh * 65:(kvh + 1) * 65],
                          start=(ki == 0 and h == 0), stop=(ki == nk - 1), skip_group_check=True)

            def emit_attn(qt):
                kts = kts_of(qt)
                prev = None
                for ki, kt in enumerate(kts):
                    emit_scores(qt, ki, kt)
                    if prev is not None:
                        emit_pv(qt, prev[0], prev[1], len(kts))
                    prev = (ki, kt)
                emit_pv(qt, prev[0], prev[1], len(kts))

            def emit_norm(qt):
                qs = slice(qt * 128, (qt + 1) * 128)
                bo = banks[2 + (qt % 2)]
                bo3 = bo[:, 0:260].rearrange("p (h d) -> p h d", h=4)
                if g == "B":
                    kb.op("dve", "tensor_tensor", [bo, esink], [den], out=den[:].unsqueeze(2), in0=bo3[:, :, 64:65],
                          in1=esink[:].unsqueeze(2), op=ALU.add)
                else:
                    kb.op("dve", "tensor_copy", [bo], [den], out=den[:].unsqueeze(2), in_=bo3[:, :, 64:65])
                kb.op("dve", "reciprocal", [den], [den], out=den[:], in_=den[:])
                ob = osb[qt % 2]
                kb.op("dve", "tensor_tensor", [bo, den], [ob], out=ob[:], in0=bo3[:, :, 0:64],
                      in1=den[:].unsqueeze(2).to_broadcast([128, 4, 64]), op=ALU.mult)
                kb.dma("sp", mixed_d[qs, gi * 256:(gi + 1) * 256], ob[:].rearrange("p h d -> p (h d)"), reads=[ob],
                       writes=[r_mixed], owner=ob)

            if g == "C":
                c_scores(0)
                for qt in range(NT):
                    if qt + 1 < NT:
                        c_scores(qt + 1)
                    c_select(qt)
                    if qt > 0:
                        emit_norm(qt - 1)
                    emit_attn(qt)
                emit_norm(NT - 1)
            else:
                for qt in range(NT):
                    emit_attn(qt)
                    emit_norm(qt)
            kb.barrier()
            kb.stack = old

    def mlp_weight_chunks(l, wu, wd, stg):
        ems = []
        engs = ("pool", "act", "dve")

        def mk(c, dst_ap, src_ap, ncols):
            def em():
                st = stg[c % len(stg)]
                kb.dma("sp", st[:, 0:ncols], src_ap, reads=[r_const], writes=[st], owner=st)
                en = engs[c % 3]
                dst_tile = wu if c < 16 else wd
                if en == "act":
                    kb.op("act", "activation", [st], [dst_tile], out=dst_ap, in_=st[:, 0:ncols], func=AF.Copy)
                else:
                    kb.op(en, "tensor_copy", [st], [dst_tile], out=dst_ap, in_=st[:, 0:ncols])
            return em

        for c in range(16):
            ems.append(mk(c, wu[:, c // 2, (c % 2) * 2048:(c % 2 + 1) * 2048],
                          w_up_d[l, (c // 2) * 128:(c // 2 + 1) * 128, (c % 2) * 2048:(c % 2 + 1) * 2048], 2048))
        for c in range(32):
            ems.append(mk(16 + c, wd[:, c, :], w_down_d[l, c * 128:(c + 1) * 128, :], 1024))
        return ems

    def phase3a(l, xsrc_d, r_xsrc, wchunks):
        with ExitStack() as st3:
            old = kb.stack
            kb.stack = st3
            wo = kb.sb("wo", [128, 8, 1024], BF16)
            stg = [kb.sb("p3stg%d" % i, [128, 1024], F32) for i in range(2)]
            load_cast_weight(wo, lambda c: wo[:, c, :], lambda c: w_out_d[l, c * 128:(c + 1) * 128, :], 8, 1024, stg)
            mx = [kb.sb("mx%d" % i, [128, 1024], BF16) for i in range(2)]
            mT = [kb.sb("mT%d" % i, [128, 8, 128], BF16) for i in range(2)]
            xt = [kb.sb("x3t%d" % i, [128, 1024], F32) for i in range(2)]
            xo = [kb.sb("x3o%d" % i, [128, 1024], F32) for i in range(2)]
            def sA(tt):
                par = tt % 2
                tok = slice(tt * 128, (tt + 1) * 128)
                kb.dma("pool", mx[par][:], mixed_d[tok, :], reads=[r_mixed], writes=[mx[par]], owner=mx[par])
                kb.dma("pool", xt[par][:], xsrc_d[tok, :], reads=[r_xsrc], writes=[xt[par]], owner=xt[par])
                bT = banks[4 + par]
                bTv = bT[:].bitcast(BF16)
                for kc in range(8):
                    kb.op("pe", "transpose", [mx[par], ident], [bT], out=bTv[:, kc * 128:(kc + 1) * 128],
                          in_=mx[par][:, kc * 128:(kc + 1) * 128], identity=ident[:, 0:128])
                kb.op("act", "activation", [bT], [mT[par]], out=mT[par][:].rearrange("p k t -> p (k t)"), in_=bTv,
                      func=AF.Copy)

            def sB(tt):
                par = tt % 2
                tok = slice(tt * 128, (tt + 1) * 128)
                for nb in range(2):
                    bk = banks[2 * par + nb]
                    for kc in range(8):
                        kb.op("pe", "matmul", [mT[par], wo], [bk], out=bk[:, 0:512], lhsT=mT[par][:, kc, :],
                              rhs=wo[:, kc, nb * 512:(nb + 1) * 512], start=(kc == 0), stop=(kc == 7))
                    kb.op("dve", "tensor_tensor", [bk, xt[par]], [xo[par]], out=xo[par][:, nb * 512:(nb + 1) * 512],
                          in0=bk[:, 0:512], in1=xt[par][:, nb * 512:(nb + 1) * 512], op=ALU.add)
                kb.dma("sp", xm_d[tok, :], xo[par][:], reads=[xo[par]], writes=[r_xm], owner=xo[par])

            sA(0)
            wq = list(wchunks)
            per = (len(wq) + NT - 1) // NT
            for tt in range(NT):
                if tt + 1 < NT:
                    sA(tt + 1)
                sB(tt)
                for _ in range(per):
                    if wq:
                        wq.pop(0)()
            while wq:
                wq.pop(0)()
            kb.barrier()
            kb.stack = old

    def phase3b(l, xdst_d, r_xdst, final, wu, wd):
        with ExitStack() as st4:
            old = kb.stack
            kb.stack = st4
            g2 = kb.sb("g2", [128, 1024], F32)
            bcast_load(g2, norm2_d[l:l + 1, :], 1024)
            if final:
                gf = kb.sb("gf", [128, 1024], F32)
                bcast_load(gf, fnorm_d[0:1, :], 1024)
            TB = 2
            xt = [[kb.sb("x4t%d_%d" % (i, j), [128, 1024], F32) for j in range(TB)] for i in range(2)]
            hb = kb.sb("h4b", [128, 1024], BF16)
            hT = [kb.sb("h4T%d" % i, [128, 8, TB * 128], BF16) for i in range(2)]
            uT = [kb.sb("u4T%d" % i, [128, TB * 128], BF16) for i in range(4)]
            rT = [kb.sb("r4T%d" % i, [128, TB * 128], F32) for i in range(3)]
            junk = kb.sb("junk4", [128, 1024], F32)
            ss = kb.sb("ss4", [128, 2], F32)
            rstd = kb.sb("rstd4", [128, 2], F32)
            nblk = NT // TB
            def pre(b):
                par = b % 2
                for j in range(TB):
                    tok = slice((b * TB + j) * 128, (b * TB + j + 1) * 128)
                    x = xt[par][j]
                    kb.dma("pool", x[:], xm_d[tok, :], reads=[r_xm], writes=[x], owner=x)
                    rmsnorm_rstd(x, x[:], 1024, ss, rstd, junk)
                    kb.op("dve", "scalar_tensor_tensor", [x, rstd, g2], [hb], out=hb[:], in0=x[:],
                          scalar=rstd[:, 0:1], in1=g2[:], op0=ALU.mult, op1=ALU.mult)
                    bT = banks[7]
                    bTv = bT[:].bitcast(BF16)
                    for kc in range(8):
                        kb.op("pe", "transpose", [hb, ident], [bT], out=bTv[:, kc * 128:(kc + 1) * 128],
                              in_=hb[:, kc * 128:(kc + 1) * 128], identity=ident[:, 0:128])
                    kb.op("act", "activation", [bT], [hT[par]], out=hT[par][:, :, j * 128:(j + 1) * 128],
                          in_=bTv.rearrange("p (k t) -> p k t", k=8), func=AF.Copy)
            def main(b):
                par = b % 2
                accs = [banks[0], banks[1], banks[2], banks[3]]
                def emit_up(fc):
                    bu = banks[4 + fc % 3]
                    for kc in range(8):
                        kb.op("pe", "matmul", [wu, hT[par]], [bu], out=bu[:, 0:TB * 128],
                              lhsT=wu[:, kc, fc * 128:(fc + 1) * 128], rhs=hT[par][:, kc, :], start=(kc == 0),
                              stop=(kc == 7))
                    u = uT[fc % 4]
                    rr = rT[fc % 3]
                    kb.op("act", "activation", [bu], [rr], out=rr[:], in_=bu[:, 0:TB * 128], func=AF.Relu)
                    kb.op("dve" if fc % 2 == 0 else "pool", "tensor_tensor", [rr], [u], out=u[:], in0=rr[:],
                          in1=rr[:], op=ALU.mult)

                def emit_down(fc):
                    u = uT[fc % 4]
                    for j in range(TB):
                        for nb in range(2):
                            acc = accs[j * 2 + nb]
                            kb.op("pe", "matmul", [u, wd], [acc], out=acc[:, 0:512], lhsT=u[:, j * 128:(j + 1) * 128],
                                  rhs=wd[:, fc, nb * 512:(nb + 1) * 512], start=(fc == 0), stop=(fc == 31))

                emit_up(0)
                emit_up(1)
                for fc in range(32):
                    if fc + 2 < 32:
                        emit_up(fc + 2)
                    emit_down(fc)
                for j in range(TB):
                    tok = slice((b * TB + j) * 128, (b * TB + j + 1) * 128)
                    o = xt[par][j]
                    for nb in range(2):
                        acc = accs[j * 2 + nb]
                        kb.op("dve", "tensor_tensor", [acc, xt[par][j]], [o], out=o[:, nb * 512:(nb + 1) * 512],
                              in0=acc[:, 0:512], in1=xt[par][j][:, nb * 512:(nb + 1) * 512], op=ALU.add)
                    if final:
                        kb.op("act", "activation", [o], [junk, ss], out=junk[:], in_=o[:], func=AF.Square,
                              accum_out=ss[:, 1:2])
                        kb.op("dve", "tensor_scalar", [ss], [rstd], out=rstd[:, 1:2], in0=ss[:, 1:2],
                              scalar1=1.0 / 1024, scalar2=EPS, op0=ALU.mult, op1=ALU.add)
                        kb.op("act", "activation", [rstd], [rstd], out=rstd[:, 1:2], in_=rstd[:, 1:2], func=AF.Sqrt)
                        kb.op("dve", "reciprocal", [rstd], [rstd], out=rstd[:, 1:2], in_=rstd[:, 1:2])
                        kb.op("dve", "scalar_tensor_tensor", [o, rstd, gf], [o], out=o[:], in0=o[:],
                              scalar=rstd[:, 1:2], in1=gf[:], op0=ALU.mult, op1=ALU.mult)
                    kb.dma("sp", xdst_d[tok, :], o[:], reads=[o], writes=[r_xdst], owner=o)

            pre(0)
            for b in range(nblk):
                if b + 1 < nblk:
                    pre(b + 1)
                main(b)
            kb.barrier()
            kb.stack = old

    cur_d, cur_r = x_in, r_xin
    for l in range(depth):
        phase1(l, cur_d, cur_r)
        for g in "ABCD":
            attention(l, g)
        with ExitStack() as stw:
            oldw = kb.stack
            kb.stack = stw
            wu = kb.sb("wu", [128, 8, D_FF], BF16)
            wd = kb.sb("wd", [128, 32, 1024], BF16)
            wstg = [kb.sb("p4stg%d" % i, [128, 2048], F32) for i in range(2)]
            phase3a(l, cur_d, cur_r, mlp_weight_chunks(l, wu, wd, wstg))
            last = (l == depth - 1)
            if last:
                phase3b(l, out_d, r_out, True, wu, wd)
            else:
                phase3b(l, xs[l % 2], r_xs[l % 2], False, wu, wd)
                cur_d, cur_r = xs[l % 2], r_xs[l % 2]
            kb.stack = oldw
    kb.wait_all("sp", [r_out])
    kb.wait_all("pool", [r_out])
    kb.barrier()
    print("KB: nsem=%d instr=%s" % (kb.nsem, {n: len(E.prog) for n, E in kb.engs.items()}), flush=True)
    kb.replay()
    stack.close()
    return nc


def host_consts(S):
    pos = np.arange(S, dtype=np.float32)

    def tables(d):
        half = d // 2
        inv = (1.0 / (10000.0 ** (np.arange(0, half, dtype=np.float32) * 2.0 / d))).astype(np.float32)
        ang = pos[:, None] * inv[None, :]
        return np.cos(ang).astype(np.float32), np.sin(ang).astype(np.float32)

    c64, s64 = tables(64)
    c32, s32 = tables(32)
    cm = np.zeros((128, 5 * 512), np.float32)
    eye = np.eye(128, dtype=np.float32)
    kk = np.arange(128)[:, None]
    qq = np.arange(128)[None, :]
    mc = np.where(kk > qq, -BIGM, 0.0).astype(np.float32)
    mp = np.where(kk <= qq, -BIGM, 0.0).astype(np.float32)
    for h in range(4):
        cm[:, h * 128:(h + 1) * 128] = eye
        cm[:, 512 + h * 128:512 + (h + 1) * 128] = mc
        cm[:, 1024 + h * 128:1024 + (h + 1) * 128] = mp
    cm[:, 1536:1664] = np.where(qq > kk, NEG_S, 0.0)
    cm[:, 1664:1792] = (kk <= qq).astype(np.float32)
    cm[:, 1792:1920] = (kk == 127).astype(np.float32) * np.ones((1, 128), np.float32)
    cm[:, 2048:2176] = eye
    return c64, s64, c32, s32, cm


_CACHE = {}


def kernel(x, norm1, w_in, mla_q_norm, mla_kv_norm, mla_w_uq, mla_w_ukv, swa_sinks, fox_b_f, w_out, norm2, w_up,
           w_down, final_norm, _depth=None, _ncores=None, _debug=False):
    x = np.asarray(x, dtype=np.float32)
    B, S, _ = x.shape
    depth = int(_depth) if _depth is not None else int(np.asarray(w_in).shape[0])
    ncores = int(_ncores) if _ncores is not None else B
    f = lambda a: np.ascontiguousarray(np.asarray(a, dtype=np.float32))
    key = (S, depth, _debug)
    if key not in _CACHE:
        _CACHE[key] = build_program(S=S, depth=depth, debug=_debug)
    nc = _CACHE[key]
    c64, s64, c32, s32, cm = host_consts(S)
    shared = {
        "w_in": f(np.asarray(w_in)[:depth][:, :, PERM]),
        "w_uq": f(np.asarray(mla_w_uq)[:depth]),
        "w_ukv": f(np.asarray(mla_w_ukv)[:depth]),
        "w_out": f(np.asarray(w_out)[:depth]),
        "w_up": f(np.asarray(w_up)[:depth]),
        "w_down": f(np.asarray(w_down)[:depth]),
        "norm1": f(np.asarray(norm1)[:depth]),
        "norm2": f(np.asarray(norm2)[:depth]),
        "gq": f(np.asarray(mla_q_norm)[:depth]),
        "gkv": f(np.asarray(mla_kv_norm)[:depth]),
        "sinks": f(np.asarray(swa_sinks)[:depth]),
        "foxb": f(np.asarray(fox_b_f)[:depth]),
        "fnorm": f(np.asarray(final_norm).reshape(1, -1)),
        "ropet": np.ascontiguousarray(np.concatenate([c64, s64, c32, s32], axis=1)), "cmat": cm,
    }
    in_maps = []
    for b in range(ncores):
        m = dict(shared)
        m["x"] = f(x[b])
        in_maps.append(m)
    res = run_bass_kernel_spmd(nc, in_maps, core_ids=list(range(ncores)))
    out = np.stack([np.asarray(r["out"], dtype=np.float32) for r in res.results], axis=0)
    if _debug:
        return out, res.results
    return out
```

```python
import numpy as np
from contextlib import ExitStack
import concourse.bass as bass
import concourse.mybir as mybir
from concourse.bass_utils import run_bass_kernel_spmd

F32 = mybir.dt.float32
BF16 = mybir.dt.bfloat16
ALU = mybir.AluOpType
AF = mybir.ActivationFunctionType
AX = mybir.AxisListType

D_MODEL = 1024
DEPTH = 4
SEQ = 4096
IN_WIDTH = 2764
D_FF = 4096
EPS = 1e-6
BIGM = 262144.0
NEG_S = -1.0e30
N_BISECT = 15

_ORIG = dict(a_cq=(0, 256), a_ckv=(256, 384), a_kr=(384, 416), b_q=(416, 672), b_k=(672, 800), b_v=(800, 928),
             c_q=(928, 1184), c_k=(1184, 1440), c_v=(1440, 1696), c_qi=(1696, 1952), c_ki=(1952, 1984),
             c_w=(1984, 1992), d_q=(1992, 2248), d_k=(2248, 2504), d_v=(2504, 2760), d_f=(2760, 2764))
_ORDER = ["b_q", "b_k", "c_q", "c_k", "c_qi", "c_ki", "a_kr", "b_v", "c_v", "d_q", "d_k", "d_v",
          "a_cq", "a_ckv", "c_w", "d_f"]
COL = {}
_perm = []
_o = 0
for _n in _ORDER:
    _a, _b = _ORIG[_n]
    COL[_n] = (_o, _o + (_b - _a))
    _perm.extend(range(_a, _b))
    _o += _b - _a
PERM = np.array(_perm, dtype=np.int64)
assert _o == IN_WIDTH


class Res:
    __slots__ = ("name", "w", "r", "sem", "cnt", "psum")

    def __init__(self, name):
        self.name = name
        self.psum = False
        self.w = {}
        self.r = {}
        self.sem = None
        self.cnt = 0


class Tile:
    def __init__(self, t, res):
        self.t = t
        self.res = res

    def __getitem__(self, k):
        return self.t[k]


class Eng:
    def __init__(self, name):
        self.name = name
        self.sem = None
        self.cnt = 0
        self.seen = {}
        self.prog = []


class KB:
    def __init__(self, nc, stack):
        self.nc = nc
        self.stack = stack
        self.gstack = stack
        self.engs = {n: Eng(n) for n in ("pe", "act", "dve", "pool", "sp")}
        self.nsem = 0
        self.sems = []
        for e in self.engs.values():
            e.sem = self.new_sem(e.name)
        self.all_res = []
        self.named = {}
        self.uid = 0

    def new_sem(self, name):
        self.nsem += 1
        h = self.gstack.enter_context(self.nc.semaphore("s%d_%s" % (self.nsem, name)))
        self.sems.append(h)
        return h

    def res(self, name):
        r = Res(name)
        self.all_res.append(r)
        return r

    def sb(self, name, shape, dt):
        self.uid += 1
        t = self.stack.enter_context(self.nc.sbuf_tensor("%s_u%d" % (name, self.uid), list(shape), dt))
        return Tile(t, self.res(name))

    def ps(self, name, shape, dt):
        t = self.gstack.enter_context(self.nc.psum_tensor(name, list(shape), dt))
        r = self.res(name)
        r.psum = True
        return Tile(t, r)

    def barrier(self):
        toks = [(E.sem, E.cnt) for E in self.engs.values() if E.cnt > 0]
        toks += [(sem, cnt) for (sem, cnt) in self.named.values()]
        for E in self.engs.values():
            waits = []
            for (sem, val) in toks:
                k = id(sem)
                if sem is E.sem or E.seen.get(k, 0) >= val:
                    continue
                waits.append((sem, val))
                E.seen[k] = val

            def emit(eng, waits=waits):
                for (s_, v) in waits:
                    eng.wait_ge(s_, v)

            E.prog.append(emit)

    @staticmethod
    def _r(x):
        return x.res if isinstance(x, Tile) else x

    def _deps(self, E, reads, writes, is_dma):
        deps = {}

        def add(d, skip_dma=False):
            for k, (sem, val, isd) in d.items():
                if skip_dma and isd:
                    continue
                if k not in deps or deps[k][1] < val:
                    deps[k] = (sem, val)

        for r in reads:
            add(self._r(r).w)
            if self._r(r).psum:
                add(self._r(r).r)
        for w in writes:
            add(self._r(w).w, skip_dma=is_dma)
            add(self._r(w).r)
        waits = []
        for k, (sem, val) in deps.items():
            if E.seen.get(k, 0) >= val:
                continue
            waits.append((sem, val))
            E.seen[k] = val
        return waits

    def op(self, en, fname, reads=(), writes=(), **kw):
        E = self.engs[en]
        reads = [self._r(x) for x in reads]
        writes = [self._r(x) for x in writes]
        own = id(E.sem)
        waits = self._deps(E, reads, writes, False)
        if en == "pe":
            waits = [(s, v) for (s, v) in waits if id(s) != own]
        if E.cnt >= 60000:
            E.sem = self.new_sem(E.name)
            E.cnt = 0
        E.cnt += 1
        sem, cnt = E.sem, E.cnt
        key = id(sem)
        tok = (sem, cnt, False)

        def emit(eng, waits=waits, fname=fname, kw=kw, sem=sem):
            for (s, v) in waits:
                eng.wait_ge(s, v)
            getattr(eng, fname)(**kw).then_inc(sem, 1)

        E.prog.append(emit)
        for r in reads:
            if key not in r.r or r.r[key][1] < cnt:
                r.r[key] = tok
        for w in writes:
            w.w = {key: tok}
            w.r = {}

    def dma(self, en, out, in_, reads=(), writes=(), owner=None, **kw):
        E = self.engs[en]
        reads = [self._r(x) for x in reads]
        writes = [self._r(x) for x in writes]
        owner = self._r(owner)
        waits = self._deps(E, reads, writes, True)
        okey = owner.name + ("@sw" if en == "pool" else "")
        if okey in self.named:
            sem, cnt = self.named[okey]
        else:
            sem, cnt = self.new_sem("d_" + okey.replace("@", "_")), 0
        cnt += 16
        assert cnt < 65000, okey
        self.named[okey] = (sem, cnt)
        key = id(sem)
        tok = (sem, cnt, True)

        def emit(eng, waits=waits, sem=sem, out=out, in_=in_, kw=kw):
            for (s, v) in waits:
                eng.wait_ge(s, v)
            eng.dma_start(out=out, in_=in_, **kw).then_inc(sem, 16)

        E.prog.append(emit)
        for r in reads:
            r.r[key] = tok
        for w in writes:
            w.w[key] = tok

    def finish(self):
        self.barrier()
        sems = list(self.sems)
        done = self.new_sem("done")
        for n, E in self.engs.items():
            if n == "pool":
                continue
            E.prog.append(lambda eng, done=done: eng.sem_inc(done, 1))

        def emit(eng, sems=sems, done=done):
            eng.wait_ge(done, 4)
            for s_ in sems:
                eng.sem_clear(s_)
            eng.sem_clear(done)

        self.engs["pool"].prog.append(emit)

    def wait_all(self, en, ress):
        E = self.engs[en]
        waits = self._deps(E, [self._r(x) for x in ress], [], False)

        def emit(eng, waits=waits):
            for (s, v) in waits:
                eng.wait_ge(s, v)

        E.prog.append(emit)

    def replay(self):
        nc = self.nc
        with nc.Block() as block:
            @block.sync
            def _(e):
                for f in self.engs["sp"].prog:
                    f(e)

            @block.tensor
            def _(e):
                for f in self.engs["pe"].prog:
                    f(e)

            @block.scalar
            def _(e):
                for f in self.engs["act"].prog:
                    f(e)

            @block.vector
            def _(e):
                for f in self.engs["dve"].prog:
                    f(e)

            @block.gpsimd
            def _(e):
                for f in self.engs["pool"].prog:
                    f(e)


def build_program(S=SEQ, depth=DEPTH, topk=None, debug=False):
    NT = S // 128
    if topk is None:
        topk = min(256, S // 4)
    nc = bass.Bass("TRN2", target_bir_lowering=False)
    stack = ExitStack()
    kb = KB(nc, stack)

    def din(name, shape, dt=F32):
        return nc.dram_tensor(name, list(shape), dt, kind="ExternalInput").ap()

    def dscr(name, shape, dt):
        kind = "ExternalOutput" if debug else "Internal"
        return nc.dram_tensor(name, list(shape), dt, kind=kind).ap()

    L = depth
    x_in = din("x", [S, D_MODEL])
    w_in_d = din("w_in", [L, D_MODEL, IN_WIDTH])
    w_uq_d = din("w_uq", [L, 256, 384])
    w_ukv_d = din("w_ukv", [L, 128, 512])
    w_out_d = din("w_out", [L, 1024, 1024])
    w_up_d = din("w_up", [L, 1024, D_FF])
    w_down_d = din("w_down", [L, D_FF, 1024])
    norm1_d = din("norm1", [L, 1024])
    norm2_d = din("norm2", [L, 1024])
    gq_d = din("gq", [L, 256])
    gkv_d = din("gkv", [L, 128])
    sinks_d = din("sinks", [L, 4])
    foxb_d = din("foxb", [L, 4])
    fnorm_d = din("fnorm", [1, 1024])
    ropet_d = din("ropet", [S, 96])
    cmat_d = din("cmat", [128, 5 * 512])
    out_d = nc.dram_tensor("out", [S, D_MODEL], F32, kind="ExternalOutput").ap()

    xs = [dscr("xs0", [S, D_MODEL], F32), dscr("xs1", [S, D_MODEL], F32)]
    xm_d = dscr("xm", [S, D_MODEL], F32)
    mixed_d = dscr("mixed", [S, D_MODEL], BF16)
    DKA = dict(A=96, B=64, C=64, D=68)
    HK = dict(A=4, B=2, C=4, D=4)
    qT_d = {g: dscr("qT_" + g, [DKA[g], 4, S], BF16) for g in "ABCD"}
    kT_d = {g: dscr("kT_" + g, [DKA[g], HK[g], S], BF16) for g in "ABCD"}
    v_d = {g: dscr("v_" + g, [S, HK[g], 65], BF16) for g in "ABCD"}
    qiT_d = dscr("qiT", [32, 8, S], BF16)
    kiT_d = dscr("kiT", [32, S], BF16)
    wi_d = dscr("wi", [S, 8], F32)

    R = kb.res
    r_xin = R("x_in")
    r_xs = [R("xs0"), R("xs1")]
    r_xm = R("xm")
    r_mixed = R("mixed")
    r_qT = {g: R("qT" + g) for g in "ABCD"}
    r_kT = {g: R("kT" + g) for g in "ABCD"}
    r_v = {g: R("v" + g) for g in "ABCD"}
    r_qiT, r_kiT, r_wi = R("qiT"), R("kiT"), R("wi")
    r_out = R("out")
    r_const = R("constin")

    banks = [kb.ps("bank%d" % i, [128, 512], F32) for i in range(8)]

    cm_f = kb.sb("cm_f", [128, 5 * 512], F32)
    kb.dma("sp", cm_f[:], cmat_d[:, :], reads=[r_const], writes=[cm_f], owner=cm_f)
    ident4 = kb.sb("ident4", [128, 512], BF16)
    maskc4 = kb.sb("maskc4", [128, 512], BF16)
    maskp4 = kb.sb("maskp4", [128, 512], BF16)
    kb.op("dve", "tensor_copy", [cm_f], [ident4], out=ident4[:], in_=cm_f[:, 0:512])
    kb.op("dve", "tensor_copy", [cm_f], [maskc4], out=maskc4[:], in_=cm_f[:, 512:1024])
    kb.op("dve", "tensor_copy", [cm_f], [maskp4], out=maskp4[:], in_=cm_f[:, 1024:1536])
    ident = ident4
    negS = cm_f[:, 1536:1664]
    TRI = cm_f[:, 1664:1792]
    LAST = cm_f[:, 1792:1920]
    identf = cm_f[:, 2048:2176]

    def bcast_load(tile_, src_row_ap, n):
        kb.dma("sp", tile_[:], src_row_ap.to_broadcast([128, n]), reads=[r_const], writes=[tile_], owner=tile_)

    def load_cast_weight(dst_tile, dst_ap_fn, src_ap_fn, nchunks, ncols, stg, engs=("dve", "act")):
        for c in range(nchunks):
            st = stg[c % len(stg)]
            kb.dma("sp", st[:, 0:ncols], src_ap_fn(c), reads=[r_const], writes=[st], owner=st)
            en = engs[c % len(engs)]
            if en == "act":
                kb.op("act", "activation", [st], [dst_tile], out=dst_ap_fn(c), in_=st[:, 0:ncols], func=AF.Copy)
            else:
                kb.op(en, "tensor_copy", [st], [dst_tile], out=dst_ap_fn(c), in_=st[:, 0:ncols])

    def rmsnorm_rstd(src_tile, src_ap, n, ss, rstd, junk, lnexp=False):
        kb.op("act", "activation", [src_tile], [junk, ss], out=junk[:, 0:n], in_=src_ap, func=AF.Square,
              accum_out=ss[:, 0:1])
        kb.op("dve", "tensor_scalar", [ss], [rstd], out=rstd[:, 0:1], in0=ss[:, 0:1], scalar1=1.0 / n,
              scalar2=EPS, op0=ALU.mult, op1=ALU.add)
        if lnexp:
            kb.op("act", "activation", [rstd], [rstd], out=rstd[:, 0:1], in_=rstd[:, 0:1], func=AF.Ln)
            kb.op("act", "activation", [rstd], [rstd], out=rstd[:, 0:1], in_=rstd[:, 0:1], func=AF.Exp, scale=-0.5)
            return
        kb.op("act", "activation", [rstd], [rstd], out=rstd[:, 0:1], in_=rstd[:, 0:1], func=AF.Sqrt)
        kb.op("dve", "reciprocal", [rstd], [rstd], out=rstd[:, 0:1], in_=rstd[:, 0:1])

    def rope(src_tile, src4, dst_tile, dst4, cos_t, sin_t, nh, half, tmp, tmpb):
        (cos_t, co), (sin_t, so) = cos_t, sin_t
        cb = cos_t[:, co:co + half].unsqueeze(1).unsqueeze(1).to_broadcast([128, nh, 2, half])
        sb_ = sin_t[:, so:so + half].unsqueeze(1).unsqueeze(1).to_broadcast([128, nh, 2, half])
        n = nh * 2 * half
        tc = tmp[:, 0:n].rearrange("p (h two d) -> p h two d", h=nh, two=2)
        ts = tmpb[:, 0:n].rearrange("p (h two d) -> p h two d", h=nh, two=2)
        kb.op("dve", "tensor_tensor", [src_tile, cos_t], [tmp], out=tc, in0=src4, in1=cb, op=ALU.mult)
        kb.op("pool", "tensor_tensor", [src_tile, sin_t], [tmpb], out=ts, in0=src4, in1=sb_, op=ALU.mult)
        kb.op("dve", "tensor_tensor", [tmp, tmpb], [dst_tile], out=dst4[:, :, 0, :], in0=tc[:, :, 0, :],
              in1=ts[:, :, 1, :], op=ALU.subtract)
        kb.op("dve", "tensor_tensor", [tmp, tmpb], [dst_tile], out=dst4[:, :, 1, :], in0=tc[:, :, 1, :],
              in1=ts[:, :, 0, :], op=ALU.add)

    def phase1(l, xsrc_d, r_xsrc):
        with ExitStack() as st1:
            old = kb.stack
            kb.stack = st1
            win = kb.sb("win", [128, 8, IN_WIDTH], BF16)
            wuq = kb.sb("wuq", [128, 2, 384], BF16)
            wukv = kb.sb("wukv", [128, 512], BF16)
            stg = [kb.sb("p1stg%d" % i, [128, IN_WIDTH], F32) for i in range(2)]
            load_cast_weight(win, lambda c: win[:, c, :], lambda c: w_in_d[l, c * 128:(c + 1) * 128, :], 8,
                             IN_WIDTH, stg)
            load_cast_weight(wuq, lambda c: wuq[:, c, :], lambda c: w_uq_d[l, c * 128:(c + 1) * 128, :], 2, 384, stg)
            load_cast_weight(wukv, lambda c: wukv[:, :], lambda c: w_ukv_d[l, :, :], 1, 512, stg)
            g1 = kb.sb("g1", [128, 1024], F32)
            gq = kb.sb("gq", [128, 256], F32)
            gkv = kb.sb("gkv", [128, 128], F32)
            fb = kb.sb("fb", [128, 4], F32)
            bcast_load(g1, norm1_d[l:l + 1, :], 1024)
            bcast_load(gq, gq_d[l:l + 1, :], 256)
            bcast_load(gkv, gkv_d[l:l + 1, :], 128)
            bcast_load(fb, foxb_d[l:l + 1, :], 4)
            nfb = kb.sb("nfb", [128, 4], F32)
            kb.op("dve", "tensor_scalar", [fb], [nfb], out=nfb[:], in0=fb[:], scalar1=-1.0, scalar2=None,
                  op0=ALU.mult)

            xt = [kb.sb("xt%d" % i, [128, 1024], F32) for i in range(2)]
            rt = [kb.sb("rt%d" % i, [128, 96], F32) for i in range(2)]
            junk = kb.sb("junk", [128, 1024], F32)
            ss = kb.sb("ss", [128, 4], F32)
            rstd = kb.sb("rstd", [128, 4], F32)
            hb = kb.sb("hb", [128, 1024], BF16)
            hT = kb.sb("hT", [128, 8, 128], BF16)
            projs = [kb.sb("proj%d" % i, [128, IN_WIDTH], F32) for i in range(2)]
            junk2 = kb.sb("junk2", [128, 384], F32)
            ss2 = kb.sb("ss2", [128, 4], F32)
            rstd2 = kb.sb("rstd2", [128, 4], F32)
            r64 = kb.sb("r64", [128, 14, 64], BF16)
            r32 = kb.sb("r32", [128, 10, 32], BF16)
            tmp = kb.sb("tmp", [128, 896], F32)
            tmpb = kb.sb("tmpb", [128, 896], F32)
            tmp2 = kb.sb("tmp2", [128, 320], F32)
            tmp2b = kb.sb("tmp2b", [128, 320], F32)
            tmp3 = kb.sb("tmp3", [128, 128], F32)
            tmp3a = kb.sb("tmp3a", [128, 128], F32)
            tmp3b = kb.sb("tmp3b", [128, 128], F32)
            cqn = kb.sb("cqn", [128, 384], BF16)
            cqnT = kb.sb("cqnT", [128, 3, 128], BF16)
            qa = kb.sb("qa", [128, 4, 96], BF16)
            ka = kb.sb("ka", [128, 4, 96], BF16)
            qd = kb.sb("qd", [128, 4, 68], BF16)
            kd = kb.sb("kd", [128, 4, 68], BF16)
            vst = {g: [kb.sb("vst%s%d" % (g, i), [128, HK[g], 65], BF16) for i in range(2)] for g in "ABCD"}
            for g in "ABCD":
                for i in range(2):
                    kb.op("pool", "memset", [], [vst[g][i]], ap=vst[g][i][:], constant=1.0)
            kb.op("pool", "memset", [], [qd], ap=qd[:], constant=1.0)
            kb.op("pool", "memset", [], [kd], ap=kd[:], constant=1.0)
            logf = kb.sb("logf", [128, 4], F32)
            cum = [kb.sb("cum%d" % i, [128, 4], F32) for i in range(2)]
            kb.op("pool", "memset", [], [cum[1]], ap=cum[1][:], constant=0.0)
            c8 = kb.sb("c8", [128, 4], F32)
            cp = kb.sb("cp", [128, 3, 4], BF16)
            cr = kb.sb("cr", [128, 4], F32)
            wst = [kb.sb("wst%d" % i, [128, 8], F32) for i in range(2)]
            tst = [[kb.sb("tst%d_%d" % (j, i), [128, 4, 128], BF16) for j in range(9)] for i in range(2)]

            def stage1(tt):
                par = tt % 2
                tok = slice(tt * 128, (tt + 1) * 128)
                proj = projs[par]
                x = xt[par]
                kb.dma("pool", x[:], xsrc_d[tok, :], reads=[r_xsrc], writes=[x], owner=x)
                kb.dma("pool", rt[par][:], ropet_d[tok, :], reads=[r_const], writes=[rt[par]], owner=rt[par])
                rmsnorm_rstd(x, x[:], 1024, ss, rstd, junk, lnexp=True)
                kb.op("dve", "scalar_tensor_tensor", [x, rstd, g1], [hb], out=hb[:], in0=x[:], scalar=rstd[:, 0:1],
                      in1=g1[:], op0=ALU.mult, op1=ALU.mult)
                bT = banks[6]
                bTv = bT[:].bitcast(BF16)
                for kc in range(8):
                    kb.op("pe", "transpose", [hb, ident], [bT], out=bTv[:, kc * 128:(kc + 1) * 128],
                          in_=hb[:, kc * 128:(kc + 1) * 128], identity=ident[:, 0:128])
                kb.op("act", "activation", [bT], [hT], out=hT[:].rearrange("p k t -> p (k t)"), in_=bTv,
                      func=AF.Copy)
                for nb in range(6):
                    c0, c1 = nb * 512, min((nb + 1) * 512, IN_WIDTH)
                    for kc in range(8):
                        kb.op("pe", "matmul", [hT, win], [banks[nb]], out=banks[nb][:, 0:c1 - c0], lhsT=hT[:, kc, :],
                              rhs=win[:, kc, c0:c1], start=(kc == 0), stop=(kc == 7))
                    if nb % 2 == 0:
                        kb.op("act", "activation", [banks[nb]], [proj], out=proj[:, c0:c1],
                              in_=banks[nb][:, 0:c1 - c0], func=AF.Copy)
                    else:
                        kb.op("dve", "tensor_copy", [banks[nb]], [proj], out=proj[:, c0:c1],
                              in_=banks[nb][:, 0:c1 - c0])
            def stage2(tt):
                par = tt % 2
                tok = slice(tt * 128, (tt + 1) * 128)
                proj = projs[par]
                c64, s64, c32, s32 = (rt[par], 0), (rt[par], 32), (rt[par], 64), (rt[par], 80)
                bT = banks[7]
                bTv = bT[:].bitcast(BF16)
                rope(proj, proj[:, 0:896].rearrange("p (h two d) -> p h two d", h=14, two=2), r64,
                     r64[:].rearrange("p h (two d) -> p h two d", two=2), c64, s64, 14, 32, tmp, tmpb)
                rope(proj, proj[:, 896:1216].rearrange("p (h two d) -> p h two d", h=10, two=2), r32,
                     r32[:].rearrange("p h (two d) -> p h two d", two=2), c32, s32, 10, 16, tmp2, tmp2b)
                vs = {g: vst[g][par] for g in "ABCD"}
                o = COL["b_v"][0]
                kb.op("pool", "tensor_copy", [proj], [vs["B"]], out=vs["B"][:, :, 0:64],
                      in_=proj[:, o:o + 128].rearrange("p (h d) -> p h d", h=2))
                o = COL["c_v"][0]
                kb.op("act", "activation", [proj], [vs["C"]], out=vs["C"][:, :, 0:64],
                      in_=proj[:, o:o + 256].rearrange("p (h d) -> p h d", h=4), func=AF.Copy)
                o = COL["d_v"][0]
                kb.op("act", "activation", [proj], [vs["D"]], out=vs["D"][:, :, 0:64],
                      in_=proj[:, o:o + 256].rearrange("p (h d) -> p h d", h=4), func=AF.Copy)
                o = COL["a_cq"][0]
                kb.op("act", "activation", [proj], [junk2, ss2], out=junk2[:, 0:256], in_=proj[:, o:o + 256],
                      func=AF.Square, accum_out=ss2[:, 1:2])
                kb.op("act", "activation", [proj], [junk2, ss2], out=junk2[:, 256:384], in_=proj[:, o + 256:o + 384],
                      func=AF.Square, accum_out=ss2[:, 2:3])
                kb.op("dve", "tensor_scalar", [ss2], [rstd2], out=rstd2[:, 1:2], in0=ss2[:, 1:2], scalar1=1.0 / 256,
                      scalar2=EPS, op0=ALU.mult, op1=ALU.add)
                kb.op("dve", "tensor_scalar", [ss2], [rstd2], out=rstd2[:, 2:3], in0=ss2[:, 2:3], scalar1=1.0 / 128,
                      scalar2=EPS, op0=ALU.mult, op1=ALU.add)
                kb.op("act", "activation", [rstd2], [rstd2], out=rstd2[:, 1:3], in_=rstd2[:, 1:3], func=AF.Ln)
                kb.op("act", "activation", [rstd2], [rstd2], out=rstd2[:, 1:3], in_=rstd2[:, 1:3], func=AF.Exp,
                      scale=-0.5)
                kb.op("dve", "scalar_tensor_tensor", [proj, rstd2, gq], [cqn], out=cqn[:, 0:256],
                      in0=proj[:, o:o + 256], scalar=rstd2[:, 1:2], in1=gq[:], op0=ALU.mult, op1=ALU.mult)
                kb.op("dve", "scalar_tensor_tensor", [proj, rstd2, gkv], [cqn], out=cqn[:, 256:384],
                      in0=proj[:, o + 256:o + 384], scalar=rstd2[:, 2:3], in1=gkv[:], op0=ALU.mult, op1=ALU.mult)
                for kc in range(3):
                    kb.op("pe", "transpose", [cqn, ident], [bT], out=bTv[:, kc * 128:(kc + 1) * 128],
                          in_=cqn[:, kc * 128:(kc + 1) * 128], identity=ident[:, 0:128])
                kb.op("act", "activation", [bT], [cqnT], out=cqnT[:].rearrange("p k t -> p (k t)"),
                      in_=bTv[:, 0:384], func=AF.Copy)
                bq, bkv = banks[0], banks[1]
                for kc in range(2):
                    kb.op("pe", "matmul", [cqnT, wuq], [bq], out=bq[:, 0:384], lhsT=cqnT[:, kc, :], rhs=wuq[:, kc, :],
                          start=(kc == 0), stop=(kc == 1))
                kb.op("pe", "matmul", [cqnT, wukv], [bkv], out=bkv[:, 0:512], lhsT=cqnT[:, 2, :], rhs=wukv[:, :],
                      start=True, stop=True)
                bq3 = bq[:, 0:384].rearrange("p (h d) -> p h d", h=4)
                kb.op("act", "activation", [bq], [qa], out=qa[:, :, 0:64], in_=bq3[:, :, 0:64], func=AF.Copy)
                kb.op("act", "activation", [bq], [tmp3], out=tmp3[:, 0:128].rearrange("p (h d) -> p h d", h=4),
                      in_=bq3[:, :, 64:96], func=AF.Copy)
                rope(tmp3, tmp3[:, 0:128].rearrange("p (h two d) -> p h two d", h=4, two=2), qa,
                     qa[:, :, 64:96].rearrange("p h (two d) -> p h two d", two=2), c32, s32, 4, 16, tmp3a, tmp3b)
                bkv3 = bkv[:, 0:512].rearrange("p (h d) -> p h d", h=4)
                kb.op("act", "activation", [bkv], [ka], out=ka[:, :, 0:64], in_=bkv3[:, :, 0:64], func=AF.Copy)
                kb.op("dve", "tensor_copy", [bkv], [vs["A"]], out=vs["A"][:, :, 0:64], in_=bkv3[:, :, 64:128])
                kb.op("pool", "tensor_copy", [r32], [ka], out=ka[:, :, 64:96],
                      in_=r32[:, 9:10, :].to_broadcast([128, 4, 32]))
                o = COL["d_f"][0]
                kb.op("dve", "scalar_tensor_tensor", [proj, nfb], [logf], out=logf[:], in0=proj[:, o:o + 4],
                      scalar=-1.0, in1=nfb[:], op0=ALU.mult, op1=ALU.add)
                kb.op("act", "activation", [logf], [logf], out=logf[:], in_=logf[:], func=AF.Exp)
                kb.op("act", "activation", [logf], [logf], out=logf[:], in_=logf[:], func=AF.Ln, bias=1.0)
                kb.op("dve", "tensor_scalar", [logf], [logf], out=logf[:], in0=logf[:], scalar1=-1.0, scalar2=None,
                      op0=ALU.mult)
                bc = banks[2]
                cprev, ccur = cum[(tt + 1) % 2], cum[tt % 2]
                kb.op("pe", "matmul", [cm_f, logf], [bc], out=bc[:, 0:4], lhsT=TRI, rhs=logf[:], start=True,
                      stop=False)
                kb.op("pe", "matmul", [cm_f, cprev], [bc], out=bc[:, 0:4], lhsT=LAST, rhs=cprev[:], start=False,
                      stop=True)
                kb.op("dve", "tensor_copy", [bc], [ccur], out=ccur[:], in_=bc[:, 0:4])
                kb.op("dve", "tensor_scalar", [ccur], [c8], out=c8[:], in0=ccur[:], scalar1=8.0, scalar2=None,
                      op0=ALU.mult)
                kb.op("dve", "tensor_copy", [c8], [cp], out=cp[:, 0, :], in_=c8[:])
                kb.op("dve", "tensor_tensor", [c8, cp], [cr], out=cr[:], in0=c8[:], in1=cp[:, 0, :], op=ALU.subtract)
                kb.op("dve", "tensor_copy", [cr], [cp], out=cp[:, 1, :], in_=cr[:])
                kb.op("dve", "tensor_tensor", [cr, cp], [cr], out=cr[:], in0=cr[:], in1=cp[:, 1, :], op=ALU.subtract)
                kb.op("dve", "tensor_copy", [cr], [cp], out=cp[:, 2, :], in_=cr[:])
                o = COL["d_q"][0]
                kb.op("pool", "tensor_copy", [proj], [qd], out=qd[:, :, 0:64],
                      in_=proj[:, o:o + 256].rearrange("p (h d) -> p h d", h=4))
                kb.op("pool", "tensor_copy", [cp], [qd], out=qd[:, :, 64:65], in_=cp[:, 0, :].unsqueeze(2))
                o = COL["d_k"][0]
                kb.op("pool", "tensor_copy", [proj], [kd], out=kd[:, :, 0:64],
                      in_=proj[:, o:o + 256].rearrange("p (h d) -> p h d", h=4))
                kb.op("dve", "tensor_scalar", [cp], [kd], out=kd[:, :, 65:68], in0=cp[:].rearrange("p c h -> p h c"),
                      scalar1=-1.0, scalar2=None, op0=ALU.mult)
                o = COL["c_w"][0]
                ws = wst[par]
                kb.op("pool", "tensor_copy", [proj], [ws], out=ws[:], in_=proj[:, o:o + 8])
                kb.dma("sp", wi_d[tok, :], ws[:], reads=[ws], writes=[r_wi], owner=ws)
                for g in "ABCD":
                    kb.dma("sp", v_d[g][tok, :, :], vs[g][:], reads=[vs[g]], writes=[r_v[g]], owner=vs[g])
                items = [
                    (qa, [qa[:, h, :] for h in range(4)], 96, qT_d["A"], r_qT["A"]),
                    (ka, [ka[:, h, :] for h in range(4)], 96, kT_d["A"], r_kT["A"]),
                    (r64, [r64[:, h, :] for h in range(0, 4)], 64, qT_d["B"], r_qT["B"]),
                    (r64, [r64[:, h, :] for h in range(4, 6)], 64, kT_d["B"], r_kT["B"]),
                    (r64, [r64[:, h, :] for h in range(6, 10)], 64, qT_d["C"], r_qT["C"]),
                    (r64, [r64[:, h, :] for h in range(10, 14)], 64, kT_d["C"], r_kT["C"]),
                    (qd, [qd[:, h, :] for h in range(4)], 68, qT_d["D"], r_qT["D"]),
                    (kd, [kd[:, h, :] for h in range(4)], 68, kT_d["D"], r_kT["D"]),
                    (r32, [r32[:, h, :] for h in range(0, 4)], 32, qiT_d[:, 0:4, :], r_qiT),
                    (r32, [r32[:, h, :] for h in range(4, 8)], 32, qiT_d[:, 4:8, :], r_qiT),
                    (r32, [r32[:, 8, :]], 32, None, r_kiT),
                ]
                for ii, (src, aps, n, dst, rdst) in enumerate(items):
                    bk = banks[3 + (ii % 3)]
                    bkv_ = bk[:].bitcast(BF16)
                    nh = len(aps)
                    for h, ap in enumerate(aps):
                        kb.op("pe", "transpose", [src, ident], [bk], out=bkv_[0:n, h * 128:(h + 1) * 128], in_=ap,
                              identity=ident[:, 0:128])
                    sg = tst[par][ii % 9] if ii < 9 else tst[par][ii - 9 + 0]
                    en = "act" if ii % 2 == 0 else "dve"
                    if en == "act":
                        kb.op("act", "activation", [bk], [sg], out=sg[0:n, 0:nh, :].rearrange("p h t -> p (h t)"),
                              in_=bkv_[0:n, 0:nh * 128], func=AF.Copy)
                    else:
                        kb.op("dve", "tensor_copy", [bk], [sg], out=sg[0:n, 0:nh, :].rearrange("p h t -> p (h t)"),
                              in_=bkv_[0:n, 0:nh * 128])
                    if dst is None:
                        kb.dma("sp", kiT_d[:, tok], sg[0:32, 0, :], reads=[sg], writes=[rdst], owner=sg)
                    else:
                        kb.dma("sp", dst[:, :, tok], sg[0:n, 0:nh, :], reads=[sg], writes=[rdst], owner=sg)
            stage1(0)
            for tt in range(NT):
                if tt + 1 < NT:
                    stage1(tt + 1)
                stage2(tt)
            kb.barrier()
            kb.stack = old

    def attention(l, g, hook=None):
        dk = DKA[g]
        hk = HK[g]
        gi = "ABCD".index(g)
        scale = {"A": 96 ** -0.5, "B": 0.125, "C": 0.125, "D": 0.125}[g]
        with ExitStack() as st2:
            old = kb.stack
            kb.stack = st2
            qT = kb.sb("aqT", [dk, 4, S], BF16)
            kT = kb.sb("akT", [dk, hk, S], BF16)
            vv = kb.sb("avv", [128, NT, hk * 65], BF16)
            for h in range(4):
                kb.dma("sp", qT[:, h, :], qT_d[g][:, h, :], reads=[r_qT[g]], writes=[qT], owner=qT)
            for h in range(hk):
                kb.dma("sp", kT[:, h, :], kT_d[g][:, h, :], reads=[r_kT[g]], writes=[kT], owner=kT)
            vsrc = v_d[g].rearrange("(j p) h d -> p j (h d)", p=128)
            for j0 in range(0, NT, 4):
                kb.dma("sp", vv[:, j0:j0 + 4, :], vsrc[:, j0:j0 + 4, :], reads=[r_v[g]], writes=[vv], owner=vv)
            pT = [kb.sb("apT%d" % i, [128, 512], BF16) for i in range(2)]
            osb = [kb.sb("aosb%d" % i, [128, 4, 64], BF16) for i in range(2)]
            den = kb.sb("aden", [128, 4], F32)
            if g == "B":
                esink = kb.sb("esink", [128, 4], F32)
                bcast_load(esink, sinks_d[l:l + 1, :], 4)
                kb.op("act", "activation", [esink], [esink], out=esink[:], in_=esink[:], func=AF.Exp)
            if g == "C":
                qiTs = [kb.sb("cqiT%d" % i, [32, 8, 128], BF16) for i in range(2)]
                kiT = kb.sb("ckiT", [32, S], BF16)
                kb.dma("sp", kiT[:], kiT_d[:, :], reads=[r_kiT], writes=[kiT], owner=kiT)
                wsb = kb.sb("cwsb", [128, NT, 8], F32)
                wsrc = wi_d.rearrange("(j p) h -> p j h", p=128)
                for j0 in range(0, NT, 4):
                    kb.dma("sp", wsb[:, j0:j0 + 4, :], wsrc[:, j0:j0 + 4, :], reads=[r_wi], writes=[wsb], owner=wsb)
                Isbs = [kb.sb("cI%d" % i, [128, S], F32) for i in range(2)]
                Madd = [kb.sb("cMadd%d" % i, [128, S], BF16) for i in range(2)]
                rl = [kb.sb("crl%d" % i, [128, 512], BF16) for i in range(3)]
                dgs = [kb.sb("cdg%d" % i, [128, 8, 128], BF16) for i in range(2)]
                cjunk = kb.sb("cjunk", [128, S], BF16)
                lo = kb.sb("clo", [128, 1], F32)
                stp = kb.sb("cstp", [128, 1], F32)
                cand = kb.sb("ccand", [128, 1], F32)
                cnt = kb.sb("ccnt", [128, 1], F32)
                mm = kb.sb("cmm", [128, 1], F32)
                hi = kb.sb("chi", [128, 1], F32)

            def kts_of(qt):
                if g == "B":
                    return [kt for kt in (qt - 1, qt) if kt >= 0]
                return list(range(qt + 1))

            def c_scores(qt):
                qs = slice(qt * 128, (qt + 1) * 128)
                Lk = 128 * (qt + 1)
                nblk = (Lk + 511) // 512
                Isb = Isbs[qt % 2]
                qiT = qiTs[qt % 2]
                dg = dgs[qt % 2]
                kb.dma("pool", qiT[:], qiT_d[:, :, qs], reads=[r_qiT], writes=[qiT], owner=qiT)
                kb.op("pool", "tensor_tensor", [ident4, wsb], [dg], out=dg[:],
                      in0=ident4[:, 0:128].unsqueeze(1).to_broadcast([128, 8, 128]),
                      in1=wsb[:, qt, :].unsqueeze(2).to_broadcast([128, 8, 128]), op=ALU.mult)
                hc = 0
                for kbk in range(nblk):
                    k0, k1 = kbk * 512, min((kbk + 1) * 512, Lk)
                    n = k1 - k0
                    bacc = banks[6 + (kbk % 2)]
                    pend = None
                    for hh in range(8):
                        bi = banks[4 + (hc % 2)]
                        r_ = rl[hc % 3]
                        kb.op("pe", "matmul", [qiT, kiT], [bi], out=bi[:, 0:n], lhsT=qiT[:, hh, :],
                              rhs=kiT[:, k0:k1], start=True, stop=True)
                        kb.op("act", "activation", [bi], [r_], out=r_[:, 0:n], in_=bi[:, 0:n], func=AF.Relu)
                        if pend is not None:
                            ph, pr = pend
                            kb.op("pe", "matmul", [dg, pr], [bacc], out=bacc[:, 0:n], lhsT=dg[:, ph, :], rhs=pr[:, 0:n],
                                  start=(ph == 0), stop=False)
                        pend = (hh, r_)
                        hc += 1
                    ph, pr = pend
                    kb.op("pe", "matmul", [dg, pr], [bacc], out=bacc[:, 0:n], lhsT=dg[:, ph, :], rhs=pr[:, 0:n],
                          start=False, stop=True)
                    kb.op("act", "activation", [bacc], [Isb], out=Isb[:, k0:k1], in_=bacc[:, 0:n], func=AF.Copy)

            def c_select(qt):
                qs = slice(qt * 128, (qt + 1) * 128)
                Lk = 128 * (qt + 1)
                Isb = Isbs[qt % 2]
                madd = Madd[qt % 2]
                kb.op("dve", "tensor_tensor", [Isb, cm_f], [Isb], out=Isb[:, qs], in0=Isb[:, qs], in1=negS, op=ALU.add)
                if Lk <= topk:
                    kb.op("dve", "tensor_scalar", [Isb], [madd], out=madd[:, 0:Lk], in0=Isb[:, 0:Lk],
                          scalar1=-1.0e29, scalar2=-BIGM, op0=ALU.is_lt, op1=ALU.mult)
                    return
                assert Lk - 128 >= topk
                kb.op("dve", "tensor_reduce", [Isb], [hi], out=hi[:], in_=Isb[:, 0:Lk], axis=AX.X, op=ALU.max)
                kb.op("dve", "tensor_reduce", [Isb], [lo], out=lo[:], in_=Isb[:, 0:Lk - 128], axis=AX.X, op=ALU.min)
                kb.op("dve", "tensor_tensor", [hi, lo], [stp], out=stp[:], in0=hi[:], in1=lo[:], op=ALU.subtract)
                for it in range(N_BISECT):
                    f = 0.5 ** (it + 1)
                    kb.op("dve", "scalar_tensor_tensor", [stp, lo], [cand], out=cand[:], in0=stp[:], scalar=f,
                          in1=lo[:], op0=ALU.mult, op1=ALU.add)
                    kb.op("dve", "tensor_scalar", [Isb, cand], [cjunk, cnt], out=cjunk[:, 0:Lk], in0=Isb[:, 0:Lk],
                          scalar1=cand[:, 0:1], scalar2=None, op0=ALU.is_ge, op1=ALU.add, accum_out=cnt[:, 0:1])
                    kb.op("dve", "scalar_tensor_tensor", [cnt, stp], [mm], out=mm[:], in0=cnt[:],
                          scalar=float(topk) - 0.5, in1=stp[:], op0=ALU.is_ge, op1=ALU.mult)
                    kb.op("dve", "scalar_tensor_tensor", [mm, lo], [lo], out=lo[:], in0=mm[:], scalar=f, in1=lo[:],
                          op0=ALU.mult, op1=ALU.add)
                kb.op("dve", "tensor_scalar", [Isb, lo], [madd], out=madd[:, 0:Lk], in0=Isb[:, 0:Lk],
                      scalar1=lo[:, 0:1], scalar2=-BIGM, op0=ALU.is_lt, op1=ALU.mult)

            def emit_scores(qt, ki, kt):
                qs = slice(qt * 128, (qt + 1) * 128)
                ks = slice(kt * 128, (kt + 1) * 128)
                bs = banks[ki % 2]
                have_mask = False
                if g == "C":
                    madd = Madd[qt % 2]
                    kb.op("pe", "matmul", [madd, ident4], [bs], out=bs[:, 0:512], lhsT=madd[:, ks],
                          rhs=ident4[:, 0:512], start=True, stop=False, skip_group_check=True)
                    have_mask = True
                elif kt == qt:
                    kb.op("pe", "matmul", [ident4, maskc4], [bs], out=bs[:, 0:512], lhsT=ident4[:, 0:128],
                          rhs=maskc4[:, 0:512], start=True, stop=False, skip_group_check=True)
                    have_mask = True
                elif g == "B":
                    kb.op("pe", "matmul", [ident4, maskp4], [bs], out=bs[:, 0:512], lhsT=ident4[:, 0:128],
                          rhs=maskp4[:, 0:512], start=True, stop=False, skip_group_check=True)
                    have_mask = True
                for h in range(4):
                    kvh = h if hk == 4 else h // 2
                    kb.op("pe", "matmul", [kT, qT], [bs], out=bs[:, h * 128:(h + 1) * 128], lhsT=kT[:, kvh, ks],
                          rhs=qT[:, h, qs], start=((not have_mask) and h == 0), stop=True, skip_group_check=True)
                p = pT[ki % 2]
                kb.op("act", "activation", [bs], [p], out=p[:], in_=bs[:, 0:512], func=AF.Exp, scale=scale)

            def emit_pv(qt, ki, kt, nk):
                bo = banks[2 + (qt % 2)]
                p = pT[ki % 2]
                for h in range(4):
                    kvh = h if hk == 4 else h // 2
                    kb.op("pe", "matmul", [p, vv], [bo], out=bo[:, h * 65:(h + 1) * 65],
                          lhsT=p[:, h * 128:(h + 1) * 128], rhs=vv[:, kt, kvh * 65:(kvh + 1) * 65],
                          start=(ki == 0 and h == 0), stop=(ki == nk - 1), skip_group_check=True)

            def emit_attn(qt):
                kts = kts_of(qt)
                prev = None
                for ki, kt in enumerate(kts):
                    emit_scores(qt, ki, kt)
                    if prev is not None:
                        emit_pv(qt, prev[0], prev[1], len(kts))
                    prev = (ki, kt)
                emit_pv(qt, prev[0], prev[1], len(kts))

            def emit_norm(qt):
                qs = slice(qt * 128, (qt + 1) * 128)
                bo = banks[2 + (qt % 2)]
                bo3 = bo[:, 0:260].rearrange("p (h d) -> p h d", h=4)
                if g == "B":
                    kb.op("dve", "tensor_tensor", [bo, esink], [den], out=den[:].unsqueeze(2), in0=bo3[:, :, 64:65],
                          in1=esink[:].unsqueeze(2), op=ALU.add)
                else:
                    kb.op("dve", "tensor_copy", [bo], [den], out=den[:].unsqueeze(2), in_=bo3[:, :, 64:65])
                kb.op("dve", "reciprocal", [den], [den], out=den[:], in_=den[:])
                ob = osb[qt % 2]
                kb.op("dve", "tensor_tensor", [bo, den], [ob], out=ob[:], in0=bo3[:, :, 0:64],
                      in1=den[:].unsqueeze(2).to_broadcast([128, 4, 64]), op=ALU.mult)
                kb.dma("sp", mixed_d[qs, gi * 256:(gi + 1) * 256], ob[:].rearrange("p h d -> p (h d)"), reads=[ob],
                       writes=[r_mixed], owner=ob)

            if g == "C":
                c_scores(0)
                for qt in range(NT):
                    if qt + 1 < NT:
                        c_scores(qt + 1)
                    c_select(qt)
                    if qt > 0:
                        emit_norm(qt - 1)
                    emit_attn(qt)
                emit_norm(NT - 1)
            else:
                for qt in range(NT):
                    emit_attn(qt)
                    emit_norm(qt)
                    if hook:
                        hook.pop(0)()
                while hook:
                    hook.pop(0)()
            kb.barrier()
            kb.stack = old

    def mlp_weight_chunks(l, wu, wd, stg, which):
        ems = []
        engs = ("dve", "pool", "dve") if which == "u" else ("pool", "act", "dve")

        def mk(c, dst_ap, src_ap, ncols):
            def em():
                st = stg[c % len(stg)]
                kb.dma("sp", st[:, 0:ncols], src_ap, reads=[r_const], writes=[st], owner=st)
                en = engs[c % 3]
                dst_tile = wu if c < 16 else wd
                if en == "act":
                    kb.op("act", "activation", [st], [dst_tile], out=dst_ap, in_=st[:, 0:ncols], func=AF.Copy)
                else:
                    kb.op(en, "tensor_copy", [st], [dst_tile], out=dst_ap, in_=st[:, 0:ncols])
            return em

        if which == "u":
            for c in range(16):
                ems.append(mk(c, wu[:, c // 2, (c % 2) * 2048:(c % 2 + 1) * 2048],
                              w_up_d[l, (c // 2) * 128:(c // 2 + 1) * 128, (c % 2) * 2048:(c % 2 + 1) * 2048], 2048))
        else:
            for c in range(32):
                ems.append(mk(16 + c, wd[:, c, :], w_down_d[l, c * 128:(c + 1) * 128, :], 1024))
        return ems

    def phase3a(l, xsrc_d, r_xsrc, wchunks):
        with ExitStack() as st3:
            old = kb.stack
            kb.stack = st3
            wo = kb.sb("wo", [128, 8, 1024], BF16)
            stg = [kb.sb("p3stg%d" % i, [128, 1024], F32) for i in range(2)]
            load_cast_weight(wo, lambda c: wo[:, c, :], lambda c: w_out_d[l, c * 128:(c + 1) * 128, :], 8, 1024, stg)
            mx = [kb.sb("mx%d" % i, [128, 1024], BF16) for i in range(2)]
            mT = [kb.sb("mT%d" % i, [128, 8, 128], BF16) for i in range(2)]
            xt = [kb.sb("x3t%d" % i, [128, 1024], F32) for i in range(2)]
            xo = [kb.sb("x3o%d" % i, [128, 1024], F32) for i in range(2)]
            def sA(tt):
                par = tt % 2
                tok = slice(tt * 128, (tt + 1) * 128)
                kb.dma("pool", mx[par][:], mixed_d[tok, :], reads=[r_mixed], writes=[mx[par]], owner=mx[par])
                kb.dma("pool", xt[par][:], xsrc_d[tok, :], reads=[r_xsrc], writes=[xt[par]], owner=xt[par])
                bT = banks[4 + par]
                bTv = bT[:].bitcast(BF16)
                for kc in range(8):
                    kb.op("pe", "transpose", [mx[par], ident], [bT], out=bTv[:, kc * 128:(kc + 1) * 128],
                          in_=mx[par][:, kc * 128:(kc + 1) * 128], identity=ident[:, 0:128])
                kb.op("act", "activation", [bT], [mT[par]], out=mT[par][:].rearrange("p k t -> p (k t)"), in_=bTv,
                      func=AF.Copy)

            def sB(tt):
                par = tt % 2
                tok = slice(tt * 128, (tt + 1) * 128)
                for nb in range(2):
                    bk = banks[2 * par + nb]
                    for kc in range(8):
                        kb.op("pe", "matmul", [mT[par], wo], [bk], out=bk[:, 0:512], lhsT=mT[par][:, kc, :],
                              rhs=wo[:, kc, nb * 512:(nb + 1) * 512], start=(kc == 0), stop=(kc == 7))
                    kb.op("dve", "tensor_tensor", [bk, xt[par]], [xo[par]], out=xo[par][:, nb * 512:(nb + 1) * 512],
                          in0=bk[:, 0:512], in1=xt[par][:, nb * 512:(nb + 1) * 512], op=ALU.add)
                kb.dma("sp", xm_d[tok, :], xo[par][:], reads=[xo[par]], writes=[r_xm], owner=xo[par])

            sA(0)
            wq = list(wchunks)
            per = (len(wq) + NT - 1) // NT
            for tt in range(NT):
                if tt + 1 < NT:
                    sA(tt + 1)
                sB(tt)
                for _ in range(per):
                    if wq:
                        wq.pop(0)()
            while wq:
                wq.pop(0)()
            kb.barrier()
            kb.stack = old

    def phase3b(l, xdst_d, r_xdst, final, wu, wd):
        with ExitStack() as st4:
            old = kb.stack
            kb.stack = st4
            g2 = kb.sb("g2", [128, 1024], F32)
            bcast_load(g2, norm2_d[l:l + 1, :], 1024)
            if final:
                gf = kb.sb("gf", [128, 1024], F32)
                bcast_load(gf, fnorm_d[0:1, :], 1024)
            TB = 2
            xt = [[kb.sb("x4t%d_%d" % (i, j), [128, 1024], F32) for j in range(TB)] for i in range(2)]
            hb = kb.sb("h4b", [128, 1024], BF16)
            hT = [kb.sb("h4T%d" % i, [128, 8, TB * 128], BF16) for i in range(2)]
            uT = [kb.sb("u4T%d" % i, [128, TB * 128], BF16) for i in range(4)]
            rT = [kb.sb("r4T%d" % i, [128, TB * 128], F32) for i in range(3)]
            junk = kb.sb("junk4", [128, 1024], F32)
            ss = kb.sb("ss4", [128, 2], F32)
            rstd = kb.sb("rstd4", [128, 2], F32)
            nblk = NT // TB
            def pre(b):
                par = b % 2
                for j in range(TB):
                    tok = slice((b * TB + j) * 128, (b * TB + j + 1) * 128)
                    x = xt[par][j]
                    kb.dma("pool", x[:], xm_d[tok, :], reads=[r_xm], writes=[x], owner=x)
                    rmsnorm_rstd(x, x[:], 1024, ss, rstd, junk)
                    kb.op("dve", "scalar_tensor_tensor", [x, rstd, g2], [hb], out=hb[:], in0=x[:],
                          scalar=rstd[:, 0:1], in1=g2[:], op0=ALU.mult, op1=ALU.mult)
                    bT = banks[7]
                    bTv = bT[:].bitcast(BF16)
                    for kc in range(8):
                        kb.op("pe", "transpose", [hb, ident], [bT], out=bTv[:, kc * 128:(kc + 1) * 128],
                              in_=hb[:, kc * 128:(kc + 1) * 128], identity=ident[:, 0:128])
                    kb.op("act", "activation", [bT], [hT[par]], out=hT[par][:, :, j * 128:(j + 1) * 128],
                          in_=bTv.rearrange("p (k t) -> p k t", k=8), func=AF.Copy)
            def main(b):
                par = b % 2
                accs = [banks[0], banks[1], banks[2], banks[3]]
                def emit_up(fc):
                    bu = banks[4 + fc % 3]
                    for kc in range(8):
                        kb.op("pe", "matmul", [wu, hT[par]], [bu], out=bu[:, 0:TB * 128],
                              lhsT=wu[:, kc, fc * 128:(fc + 1) * 128], rhs=hT[par][:, kc, :], start=(kc == 0),
                              stop=(kc == 7))
                    u = uT[fc % 4]
                    rr = rT[fc % 3]
                    kb.op("act", "activation", [bu], [rr], out=rr[:], in_=bu[:, 0:TB * 128], func=AF.Relu)
                    kb.op("dve" if fc % 2 == 0 else "pool", "tensor_tensor", [rr], [u], out=u[:], in0=rr[:],
                          in1=rr[:], op=ALU.mult)

                def emit_down(fc):
                    u = uT[fc % 4]
                    for j in range(TB):
                        for nb in range(2):
                            acc = accs[j * 2 + nb]
                            kb.op("pe", "matmul", [u, wd], [acc], out=acc[:, 0:512], lhsT=u[:, j * 128:(j + 1) * 128],
                                  rhs=wd[:, fc, nb * 512:(nb + 1) * 512], start=(fc == 0), stop=(fc == 31))

                emit_up(0)
                emit_up(1)
                for fc in range(32):
                    if fc + 2 < 32:
                        emit_up(fc + 2)
                    emit_down(fc)
                for j in range(TB):
                    tok = slice((b * TB + j) * 128, (b * TB + j + 1) * 128)
                    o = xt[par][j]
                    for nb in range(2):
                        acc = accs[j * 2 + nb]
                        kb.op("dve", "tensor_tensor", [acc, xt[par][j]], [o], out=o[:, nb * 512:(nb + 1) * 512],
                              in0=acc[:, 0:512], in1=xt[par][j][:, nb * 512:(nb + 1) * 512], op=ALU.add)
                    if final:
                        kb.op("act", "activation", [o], [junk, ss], out=junk[:], in_=o[:], func=AF.Square,
                              accum_out=ss[:, 1:2])
                        kb.op("dve", "tensor_scalar", [ss], [rstd], out=rstd[:, 1:2], in0=ss[:, 1:2],
                              scalar1=1.0 / 1024, scalar2=EPS, op0=ALU.mult, op1=ALU.add)
                        kb.op("act", "activation", [rstd], [rstd], out=rstd[:, 1:2], in_=rstd[:, 1:2], func=AF.Sqrt)
                        kb.op("dve", "reciprocal", [rstd], [rstd], out=rstd[:, 1:2], in_=rstd[:, 1:2])
                        kb.op("dve", "scalar_tensor_tensor", [o, rstd, gf], [o], out=o[:], in0=o[:],
                              scalar=rstd[:, 1:2], in1=gf[:], op0=ALU.mult, op1=ALU.mult)
                    kb.dma("sp", xdst_d[tok, :], o[:], reads=[o], writes=[r_xdst], owner=o)

            pre(0)
            for b in range(nblk):
                if b + 1 < nblk:
                    pre(b + 1)
                main(b)
            kb.barrier()
            kb.stack = old

    cur_d, cur_r = x_in, r_xin
    for l in range(depth):
        phase1(l, cur_d, cur_r)
        for g in "ABC":
            attention(l, g)
        with ExitStack() as stw:
            oldw = kb.stack
            kb.stack = stw
            wu = kb.sb("wu", [128, 8, D_FF], BF16)
            wstg = [kb.sb("p4stg%d" % i, [128, 2048], F32) for i in range(2)]
            attention(l, "D", hook=mlp_weight_chunks(l, wu, None, wstg, "u"))
            wd = kb.sb("wd", [128, 32, 1024], BF16)
            phase3a(l, cur_d, cur_r, mlp_weight_chunks(l, wu, wd, wstg, "d"))
            last = (l == depth - 1)
            if last:
                phase3b(l, out_d, r_out, True, wu, wd)
            else:
                phase3b(l, xs[l % 2], r_xs[l % 2], False, wu, wd)
                cur_d, cur_r = xs[l % 2], r_xs[l % 2]
            kb.stack = oldw
    kb.wait_all("sp", [r_out])
    kb.wait_all("pool", [r_out])
    kb.barrier()
    print("KB: nsem=%d instr=%s" % (kb.nsem, {n: len(E.prog) for n, E in kb.engs.items()}), flush=True)
    kb.replay()
    stack.close()
    return nc


def host_consts(S):
    pos = np.arange(S, dtype=np.float32)

    def tables(d):
        half = d // 2
        inv = (1.0 / (10000.0 ** (np.arange(0, half, dtype=np.float32) * 2.0 / d))).astype(np.float32)
        ang = pos[:, None] * inv[None, :]
        return np.cos(ang).astype(np.float32), np.sin(ang).astype(np.float32)

    c64, s64 = tables(64)
    c32, s32 = tables(32)
    cm = np.zeros((128, 5 * 512), np.float32)
    eye = np.eye(128, dtype=np.float32)
    kk = np.arange(128)[:, None]
    qq = np.arange(128)[None, :]
    mc = np.where(kk > qq, -BIGM, 0.0).astype(np.float32)
    mp = np.where(kk <= qq, -BIGM, 0.0).astype(np.float32)
    for h in range(4):
        cm[:, h * 128:(h + 1) * 128] = eye
        cm[:, 512 + h * 128:512 + (h + 1) * 128] = mc
        cm[:, 1024 + h * 128:1024 + (h + 1) * 128] = mp
    cm[:, 1536:1664] = np.where(qq > kk, NEG_S, 0.0)
    cm[:, 1664:1792] = (kk <= qq).astype(np.float32)
    cm[:, 1792:1920] = (kk == 127).astype(np.float32) * np.ones((1, 128), np.float32)
    cm[:, 2048:2176] = eye
    return c64, s64, c32, s32, cm


_CACHE = {}


def kernel(x, norm1, w_in, mla_q_norm, mla_kv_norm, mla_w_uq, mla_w_ukv, swa_sinks, fox_b_f, w_out, norm2, w_up,
           w_down, final_norm, _depth=None, _ncores=None, _debug=False):
    x = np.asarray(x, dtype=np.float32)
    B, S, _ = x.shape
    depth = int(_depth) if _depth is not None else int(np.asarray(w_in).shape[0])
    ncores = int(_ncores) if _ncores is not None else B
    f = lambda a: np.ascontiguousarray(np.asarray(a, dtype=np.float32))
    key = (S, depth, _debug)
    if key not in _CACHE:
        _CACHE[key] = build_program(S=S, depth=depth, debug=_debug)
    nc = _CACHE[key]
    c64, s64, c32, s32, cm = host_consts(S)
    shared = {
        "w_in": f(np.asarray(w_in)[:depth][:, :, PERM]),
        "w_uq": f(np.asarray(mla_w_uq)[:depth]),
        "w_ukv": f(np.asarray(mla_w_ukv)[:depth]),
        "w_out": f(np.asarray(w_out)[:depth]),
        "w_up": f(np.asarray(w_up)[:depth]),
        "w_down": f(np.asarray(w_down)[:depth]),
        "norm1": f(np.asarray(norm1)[:depth]),
        "norm2": f(np.asarray(norm2)[:depth]),
        "gq": f(np.asarray(mla_q_norm)[:depth]),
        "gkv": f(np.asarray(mla_kv_norm)[:depth]),
        "sinks": f(np.asarray(swa_sinks)[:depth]),
        "foxb": f(np.asarray(fox_b_f)[:depth]),
        "fnorm": f(np.asarray(final_norm).reshape(1, -1)),
        "ropet": np.ascontiguousarray(np.concatenate([c64, s64, c32, s32], axis=1)), "cmat": cm,
    }
    in_maps = []
    for b in range(ncores):
        m = dict(shared)
        m["x"] = f(x[b])
        in_maps.append(m)
    res = run_bass_kernel_spmd(nc, in_maps, core_ids=list(range(ncores)))
    out = np.stack([np.asarray(r["out"], dtype=np.float32) for r in res.results], axis=0)
    if _debug:
        return out, res.results
    return out
```

```python
import numpy as np
from contextlib import ExitStack
import concourse.bass as bass
import concourse.mybir as mybir
from concourse.bass_utils import run_bass_kernel_spmd

F32 = mybir.dt.float32
BF16 = mybir.dt.bfloat16
ALU = mybir.AluOpType
AF = mybir.ActivationFunctionType
AX = mybir.AxisListType

D_MODEL = 1024
DEPTH = 4
SEQ = 4096
IN_WIDTH = 2764
D_FF = 4096
EPS = 1e-6
BIGM = 262144.0
NEG_S = -1.0e30
N_BISECT = 15

_ORIG = dict(a_cq=(0, 256), a_ckv=(256, 384), a_kr=(384, 416), b_q=(416, 672), b_k=(672, 800), b_v=(800, 928),
             c_q=(928, 1184), c_k=(1184, 1440), c_v=(1440, 1696), c_qi=(1696, 1952), c_ki=(1952, 1984),
             c_w=(1984, 1992), d_q=(1992, 2248), d_k=(2248, 2504), d_v=(2504, 2760), d_f=(2760, 2764))
_ORDER = ["b_q", "b_k", "c_q", "c_k", "c_qi", "c_ki", "a_kr", "b_v", "c_v", "d_q", "d_k", "d_v",
          "a_cq", "a_ckv", "c_w", "d_f"]
COL = {}
_perm = []
_o = 0
for _n in _ORDER:
    _a, _b = _ORIG[_n]
    COL[_n] = (_o, _o + (_b - _a))
    _perm.extend(range(_a, _b))
    _o += _b - _a
PERM = np.array(_perm, dtype=np.int64)
assert _o == IN_WIDTH


class Res:
    __slots__ = ("name", "w", "r", "sem", "cnt", "psum")

    def __init__(self, name):
        self.name = name
        self.psum = False
        self.w = {}
        self.r = {}
        self.sem = None
        self.cnt = 0


class Tile:
    def __init__(self, t, res):
        self.t = t
        self.res = res

    def __getitem__(self, k):
        return self.t[k]


class Eng:
    def __init__(self, name):
        self.name = name
        self.sem = None
        self.cnt = 0
        self.seen = {}
        self.prog = []


class KB:
    def __init__(self, nc, stack):
        self.nc = nc
        self.stack = stack
        self.gstack = stack
        self.engs = {n: Eng(n) for n in ("pe", "act", "dve", "pool", "sp")}
        self.nsem = 0
        self.sems = []
        for e in self.engs.values():
            e.sem = self.new_sem(e.name)
        self.all_res = []
        self.named = {}
        self.uid = 0

    def new_sem(self, name):
        self.nsem += 1
        h = self.gstack.enter_context(self.nc.semaphore("s%d_%s" % (self.nsem, name)))
        self.sems.append(h)
        return h

    def res(self, name):
        r = Res(name)
        self.all_res.append(r)
        return r

    def sb(self, name, shape, dt):
        self.uid += 1
        t = self.stack.enter_context(self.nc.sbuf_tensor("%s_u%d" % (name, self.uid), list(shape), dt))
        return Tile(t, self.res(name))

    def ps(self, name, shape, dt):
        t = self.gstack.enter_context(self.nc.psum_tensor(name, list(shape), dt))
        r = self.res(name)
        r.psum = True
        return Tile(t, r)

    def barrier(self):
        toks = [(E.sem, E.cnt) for E in self.engs.values() if E.cnt > 0]
        toks += [(sem, cnt) for (sem, cnt) in self.named.values()]
        for E in self.engs.values():
            waits = []
            for (sem, val) in toks:
                k = id(sem)
                if sem is E.sem or E.seen.get(k, 0) >= val:
                    continue
                waits.append((sem, val))
                E.seen[k] = val

            def emit(eng, waits=waits):
                for (s_, v) in waits:
                    eng.wait_ge(s_, v)

            E.prog.append(emit)

    @staticmethod
    def _r(x):
        return x.res if isinstance(x, Tile) else x

    def _deps(self, E, reads, writes, is_dma):
        deps = {}

        def add(d, skip_dma=False):
            for k, (sem, val, isd) in d.items():
                if skip_dma and isd:
                    continue
                if k not in deps or deps[k][1] < val:
                    deps[k] = (sem, val)

        for r in reads:
            add(self._r(r).w)
            if self._r(r).psum:
                add(self._r(r).r)
        for w in writes:
            add(self._r(w).w, skip_dma=is_dma)
            add(self._r(w).r)
        waits = []
        for k, (sem, val) in deps.items():
            if E.seen.get(k, 0) >= val:
                continue
            waits.append((sem, val))
            E.seen[k] = val
        return waits

    def op(self, en, fname, reads=(), writes=(), **kw):
        E = self.engs[en]
        reads = [self._r(x) for x in reads]
        writes = [self._r(x) for x in writes]
        own = id(E.sem)
        waits = self._deps(E, reads, writes, False)
        if en == "pe":
            waits = [(s, v) for (s, v) in waits if id(s) != own]
        if E.cnt >= 60000:
            E.sem = self.new_sem(E.name)
            E.cnt = 0
        E.cnt += 1
        sem, cnt = E.sem, E.cnt
        key = id(sem)
        tok = (sem, cnt, False)

        def emit(eng, waits=waits, fname=fname, kw=kw, sem=sem):
            for (s, v) in waits:
                eng.wait_ge(s, v)
            getattr(eng, fname)(**kw).then_inc(sem, 1)

        E.prog.append(emit)
        for r in reads:
            if key not in r.r or r.r[key][1] < cnt:
                r.r[key] = tok
        for w in writes:
            w.w = {key: tok}
            w.r = {}

    def dma(self, en, out, in_, reads=(), writes=(), owner=None, **kw):
        E = self.engs[en]
        reads = [self._r(x) for x in reads]
        writes = [self._r(x) for x in writes]
        owner = self._r(owner)
        waits = self._deps(E, reads, writes, True)
        okey = owner.name + ("@sw" if en == "pool" else "")
        if okey in self.named:
            sem, cnt = self.named[okey]
        else:
            sem, cnt = self.new_sem("d_" + okey.replace("@", "_")), 0
        cnt += 16
        assert cnt < 65000, okey
        self.named[okey] = (sem, cnt)
        key = id(sem)
        tok = (sem, cnt, True)

        def emit(eng, waits=waits, sem=sem, out=out, in_=in_, kw=kw):
            for (s, v) in waits:
                eng.wait_ge(s, v)
            eng.dma_start(out=out, in_=in_, **kw).then_inc(sem, 16)

        E.prog.append(emit)
        for r in reads:
            r.r[key] = tok
        for w in writes:
            w.w[key] = tok

    def finish(self):
        self.barrier()
        sems = list(self.sems)
        done = self.new_sem("done")
        for n, E in self.engs.items():
            if n == "pool":
                continue
            E.prog.append(lambda eng, done=done: eng.sem_inc(done, 1))

        def emit(eng, sems=sems, done=done):
            eng.wait_ge(done, 4)
            for s_ in sems:
                eng.sem_clear(s_)
            eng.sem_clear(done)

        self.engs["pool"].prog.append(emit)

    def wait_all(self, en, ress):
        E = self.engs[en]
        waits = self._deps(E, [self._r(x) for x in ress], [], False)

        def emit(eng, waits=waits):
            for (s, v) in waits:
                eng.wait_ge(s, v)

        E.prog.append(emit)

    def replay(self):
        nc = self.nc
        with nc.Block() as block:
            @block.sync
            def _(e):
                for f in self.engs["sp"].prog:
                    f(e)

            @block.tensor
            def _(e):
                for f in self.engs["pe"].prog:
                    f(e)

            @block.scalar
            def _(e):
                for f in self.engs["act"].prog:
                    f(e)

            @block.vector
            def _(e):
                for f in self.engs["dve"].prog:
                    f(e)

            @block.gpsimd
            def _(e):
                for f in self.engs["pool"].prog:
                    f(e)


def build_program(S=SEQ, depth=DEPTH, topk=None, debug=False):
    NT = S // 128
    if topk is None:
        topk = min(256, S // 4)
    nc = bass.Bass("TRN2", target_bir_lowering=False)
    stack = ExitStack()
    kb = KB(nc, stack)

    def din(name, shape, dt=F32):
        return nc.dram_tensor(name, list(shape), dt, kind="ExternalInput").ap()

    def dscr(name, shape, dt):
        kind = "ExternalOutput" if debug else "Internal"
        return nc.dram_tensor(name, list(shape), dt, kind=kind).ap()

    L = depth
    x_in = din("x", [S, D_MODEL])
    w_in_d = din("w_in", [L, D_MODEL, IN_WIDTH])
    w_uq_d = din("w_uq", [L, 256, 384])
    w_ukv_d = din("w_ukv", [L, 128, 512])
    w_out_d = din("w_out", [L, 1024, 1024])
    w_up_d = din("w_up", [L, 1024, D_FF])
    w_down_d = din("w_down", [L, D_FF, 1024])
    norm1_d = din("norm1", [L, 1024])
    norm2_d = din("norm2", [L, 1024])
    gq_d = din("gq", [L, 256])
    gkv_d = din("gkv", [L, 128])
    sinks_d = din("sinks", [L, 4])
    foxb_d = din("foxb", [L, 4])
    fnorm_d = din("fnorm", [1, 1024])
    ropet_d = din("ropet", [S, 96])
    cmat_d = din("cmat", [128, 5 * 512])
    out_d = nc.dram_tensor("out", [S, D_MODEL], F32, kind="ExternalOutput").ap()

    xs = [dscr("xs0", [S, D_MODEL], F32), dscr("xs1", [S, D_MODEL], F32)]
    xm_d = dscr("xm", [S, D_MODEL], F32)
    mixed_d = dscr("mixed", [S, D_MODEL], BF16)
    DKA = dict(A=96, B=64, C=64, D=68)
    HK = dict(A=4, B=2, C=4, D=4)
    qT_d = {g: dscr("qT_" + g, [DKA[g], 4, S], BF16) for g in "ABCD"}
    kT_d = {g: dscr("kT_" + g, [DKA[g], HK[g], S], BF16) for g in "ABCD"}
    v_d = {g: dscr("v_" + g, [S, HK[g], 65], BF16) for g in "ABCD"}
    qiT_d = dscr("qiT", [32, 8, S], BF16)
    kiT_d = dscr("kiT", [32, S], BF16)
    wi_d = dscr("wi", [S, 8], F32)

    R = kb.res
    r_xin = R("x_in")
    r_xs = [R("xs0"), R("xs1")]
    r_xm = R("xm")
    r_mixed = R("mixed")
    r_qT = {g: R("qT" + g) for g in "ABCD"}
    r_kT = {g: R("kT" + g) for g in "ABCD"}
    r_v = {g: R("v" + g) for g in "ABCD"}
    r_qiT, r_kiT, r_wi = R("qiT"), R("kiT"), R("wi")
    r_out = R("out")
    r_const = R("constin")

    banks = [kb.ps("bank%d" % i, [128, 512], F32) for i in range(8)]

    cm_f = kb.sb("cm_f", [128, 5 * 512], F32)
    kb.dma("sp", cm_f[:], cmat_d[:, :], reads=[r_const], writes=[cm_f], owner=cm_f)
    ident4 = kb.sb("ident4", [128, 512], BF16)
    maskc4 = kb.sb("maskc4", [128, 512], BF16)
    maskp4 = kb.sb("maskp4", [128, 512], BF16)
    kb.op("dve", "tensor_copy", [cm_f], [ident4], out=ident4[:], in_=cm_f[:, 0:512])
    kb.op("dve", "tensor_copy", [cm_f], [maskc4], out=maskc4[:], in_=cm_f[:, 512:1024])
    kb.op("dve", "tensor_copy", [cm_f], [maskp4], out=maskp4[:], in_=cm_f[:, 1024:1536])
    ident = ident4
    negS = cm_f[:, 1536:1664]
    TRI = cm_f[:, 1664:1792]
    LAST = cm_f[:, 1792:1920]
    identf = cm_f[:, 2048:2176]

    def bcast_load(tile_, src_row_ap, n):
        kb.dma("sp", tile_[:], src_row_ap.to_broadcast([128, n]), reads=[r_const], writes=[tile_], owner=tile_)

    def load_cast_weight(dst_tile, dst_ap_fn, src_ap_fn, nchunks, ncols, stg, engs=("dve", "act")):
        for c in range(nchunks):
            st = stg[c % len(stg)]
            kb.dma("sp", st[:, 0:ncols], src_ap_fn(c), reads=[r_const], writes=[st], owner=st)
            en = engs[c % len(engs)]
            if en == "act":
                kb.op("act", "activation", [st], [dst_tile], out=dst_ap_fn(c), in_=st[:, 0:ncols], func=AF.Copy)
            else:
                kb.op(en, "tensor_copy", [st], [dst_tile], out=dst_ap_fn(c), in_=st[:, 0:ncols])

    def rmsnorm_rstd(src_tile, src_ap, n, ss, rstd, junk, lnexp=False):
        kb.op("act", "activation", [src_tile], [junk, ss], out=junk[:, 0:n], in_=src_ap, func=AF.Square,
              accum_out=ss[:, 0:1])
        kb.op("dve", "tensor_scalar", [ss], [rstd], out=rstd[:, 0:1], in0=ss[:, 0:1], scalar1=1.0 / n,
              scalar2=EPS, op0=ALU.mult, op1=ALU.add)
        if lnexp:
            kb.op("act", "activation", [rstd], [rstd], out=rstd[:, 0:1], in_=rstd[:, 0:1], func=AF.Ln)
            kb.op("act", "activation", [rstd], [rstd], out=rstd[:, 0:1], in_=rstd[:, 0:1], func=AF.Exp, scale=-0.5)
            return
        kb.op("act", "activation", [rstd], [rstd], out=rstd[:, 0:1], in_=rstd[:, 0:1], func=AF.Sqrt)
        kb.op("dve", "reciprocal", [rstd], [rstd], out=rstd[:, 0:1], in_=rstd[:, 0:1])

    def rope(src_tile, src4, dst_tile, dst4, cos_t, sin_t, nh, half, tmp, tmpb):
        (cos_t, co), (sin_t, so) = cos_t, sin_t
        cb = cos_t[:, co:co + half].unsqueeze(1).unsqueeze(1).to_broadcast([128, nh, 2, half])
        sb_ = sin_t[:, so:so + half].unsqueeze(1).unsqueeze(1).to_broadcast([128, nh, 2, half])
        n = nh * 2 * half
        tc = tmp[:, 0:n].rearrange("p (h two d) -> p h two d", h=nh, two=2)
        ts = tmpb[:, 0:n].rearrange("p (h two d) -> p h two d", h=nh, two=2)
        kb.op("dve", "tensor_tensor", [src_tile, cos_t], [tmp], out=tc, in0=src4, in1=cb, op=ALU.mult)
        kb.op("pool", "tensor_tensor", [src_tile, sin_t], [tmpb], out=ts, in0=src4, in1=sb_, op=ALU.mult)
        kb.op("dve", "tensor_tensor", [tmp, tmpb], [dst_tile], out=dst4[:, :, 0, :], in0=tc[:, :, 0, :],
              in1=ts[:, :, 1, :], op=ALU.subtract)
        kb.op("dve", "tensor_tensor", [tmp, tmpb], [dst_tile], out=dst4[:, :, 1, :], in0=tc[:, :, 1, :],
              in1=ts[:, :, 0, :], op=ALU.add)

    def phase1(l, xsrc_d, r_xsrc):
        with ExitStack() as st1:
            old = kb.stack
            kb.stack = st1
            win = kb.sb("win", [128, 8, IN_WIDTH], BF16)
            wuq = kb.sb("wuq", [128, 2, 384], BF16)
            wukv = kb.sb("wukv", [128, 512], BF16)
            stg = [kb.sb("p1stg%d" % i, [128, IN_WIDTH], F32) for i in range(2)]
            load_cast_weight(win, lambda c: win[:, c, :], lambda c: w_in_d[l, c * 128:(c + 1) * 128, :], 8,
                             IN_WIDTH, stg)
            load_cast_weight(wuq, lambda c: wuq[:, c, :], lambda c: w_uq_d[l, c * 128:(c + 1) * 128, :], 2, 384, stg)
            load_cast_weight(wukv, lambda c: wukv[:, :], lambda c: w_ukv_d[l, :, :], 1, 512, stg)
            g1 = kb.sb("g1", [128, 1024], F32)
            gq = kb.sb("gq", [128, 256], F32)
            gkv = kb.sb("gkv", [128, 128], F32)
            fb = kb.sb("fb", [128, 4], F32)
            bcast_load(g1, norm1_d[l:l + 1, :], 1024)
            bcast_load(gq, gq_d[l:l + 1, :], 256)
            bcast_load(gkv, gkv_d[l:l + 1, :], 128)
            bcast_load(fb, foxb_d[l:l + 1, :], 4)
            nfb = kb.sb("nfb", [128, 4], F32)
            kb.op("dve", "tensor_scalar", [fb], [nfb], out=nfb[:], in0=fb[:], scalar1=-1.0, scalar2=None,
                  op0=ALU.mult)

            xt = [kb.sb("xt%d" % i, [128, 1024], F32) for i in range(2)]
            rt = [kb.sb("rt%d" % i, [128, 96], F32) for i in range(2)]
            junk = kb.sb("junk", [128, 1024], F32)
            ss = kb.sb("ss", [128, 4], F32)
            rstd = kb.sb("rstd", [128, 4], F32)
            hb = kb.sb("hb", [128, 1024], BF16)
            hT = kb.sb("hT", [128, 8, 128], BF16)
            projs = [kb.sb("proj%d" % i, [128, IN_WIDTH], F32) for i in range(2)]
            junk2 = kb.sb("junk2", [128, 384], F32)
            ss2 = kb.sb("ss2", [128, 4], F32)
            rstd2 = kb.sb("rstd2", [128, 4], F32)
            r64 = kb.sb("r64", [128, 14, 64], BF16)
            r32 = kb.sb("r32", [128, 10, 32], BF16)
            tmp = kb.sb("tmp", [128, 896], F32)
            tmpb = kb.sb("tmpb", [128, 896], F32)
            tmp2 = kb.sb("tmp2", [128, 320], F32)
            tmp2b = kb.sb("tmp2b", [128, 320], F32)
            tmp3 = kb.sb("tmp3", [128, 128], F32)
            tmp3a = kb.sb("tmp3a", [128, 128], F32)
            tmp3b = kb.sb("tmp3b", [128, 128], F32)
            cqn = kb.sb("cqn", [128, 384], BF16)
            cqnT = kb.sb("cqnT", [128, 3, 128], BF16)
            qa = kb.sb("qa", [128, 4, 96], BF16)
            ka = kb.sb("ka", [128, 4, 96], BF16)
            qd = kb.sb("qd", [128, 4, 68], BF16)
            kd = kb.sb("kd", [128, 4, 68], BF16)
            vst = {g: [kb.sb("vst%s%d" % (g, i), [128, HK[g], 65], BF16) for i in range(2)] for g in "ABCD"}
            for g in "ABCD":
                for i in range(2):
                    kb.op("pool", "memset", [], [vst[g][i]], ap=vst[g][i][:], constant=1.0)
            kb.op("pool", "memset", [], [qd], ap=qd[:], constant=1.0)
            kb.op("pool", "memset", [], [kd], ap=kd[:], constant=1.0)
            logf = kb.sb("logf", [128, 4], F32)
            cum = [kb.sb("cum%d" % i, [128, 4], F32) for i in range(2)]
            kb.op("pool", "memset", [], [cum[1]], ap=cum[1][:], constant=0.0)
            c8 = kb.sb("c8", [128, 4], F32)
            cp = kb.sb("cp", [128, 3, 4], BF16)
            cr = kb.sb("cr", [128, 4], F32)
            wst = [kb.sb("wst%d" % i, [128, 8], F32) for i in range(2)]
            tst = [[kb.sb("tst%d_%d" % (j, i), [128, 4, 128], BF16) for j in range(9)] for i in range(2)]

            def stage1(tt, g2=None):
                par = tt % 2
                tok = slice(tt * 128, (tt + 1) * 128)
                proj = projs[par]
                x = xt[par]
                kb.dma("pool", x[:], xsrc_d[tok, :], reads=[r_xsrc], writes=[x], owner=x)
                kb.dma("pool", rt[par][:], ropet_d[tok, :], reads=[r_const], writes=[rt[par]], owner=rt[par])
                rmsnorm_rstd(x, x[:], 1024, ss, rstd, junk, lnexp=True)
                kb.op("dve", "scalar_tensor_tensor", [x, rstd, g1], [hb], out=hb[:], in0=x[:], scalar=rstd[:, 0:1],
                      in1=g1[:], op0=ALU.mult, op1=ALU.mult)
                bT = banks[6]
                bTv = bT[:].bitcast(BF16)
                for kc in range(8):
                    kb.op("pe", "transpose", [hb, ident], [bT], out=bTv[:, kc * 128:(kc + 1) * 128],
                          in_=hb[:, kc * 128:(kc + 1) * 128], identity=ident[:, 0:128])
                kb.op("act", "activation", [bT], [hT], out=hT[:].rearrange("p k t -> p (k t)"), in_=bTv,
                      func=AF.Copy)
                for nb in range(6):
                    c0, c1 = nb * 512, min((nb + 1) * 512, IN_WIDTH)
                    for kc in range(8):
                        kb.op("pe", "matmul", [hT, win], [banks[nb]], out=banks[nb][:, 0:c1 - c0], lhsT=hT[:, kc, :],
                              rhs=win[:, kc, c0:c1], start=(kc == 0), stop=(kc == 7))
                    if nb % 2 == 0:
                        kb.op("act", "activation", [banks[nb]], [proj], out=proj[:, c0:c1],
                              in_=banks[nb][:, 0:c1 - c0], func=AF.Copy)
                    else:
                        kb.op("dve", "tensor_copy", [banks[nb]], [proj], out=proj[:, c0:c1],
                              in_=banks[nb][:, 0:c1 - c0])
                    if g2 is not None:
                        next(g2, None)
                        next(g2, None)
                if g2 is not None:
                    for _ in g2:
                        pass
            def stage2(tt):
                par = tt % 2
                tok = slice(tt * 128, (tt + 1) * 128)
                proj = projs[par]
                c64, s64, c32, s32 = (rt[par], 0), (rt[par], 32), (rt[par], 64), (rt[par], 80)
                bT = banks[7]
                bTv = bT[:].bitcast(BF16)
                rope(proj, proj[:, 0:896].rearrange("p (h two d) -> p h two d", h=14, two=2), r64,
                     r64[:].rearrange("p h (two d) -> p h two d", two=2), c64, s64, 14, 32, tmp, tmpb)
                yield
                rope(proj, proj[:, 896:1216].rearrange("p (h two d) -> p h two d", h=10, two=2), r32,
                     r32[:].rearrange("p h (two d) -> p h two d", two=2), c32, s32, 10, 16, tmp2, tmp2b)
                vs = {g: vst[g][par] for g in "ABCD"}
                o = COL["b_v"][0]
                kb.op("pool", "tensor_copy", [proj], [vs["B"]], out=vs["B"][:, :, 0:64],
                      in_=proj[:, o:o + 128].rearrange("p (h d) -> p h d", h=2))
                o = COL["c_v"][0]
                kb.op("act", "activation", [proj], [vs["C"]], out=vs["C"][:, :, 0:64],
                      in_=proj[:, o:o + 256].rearrange("p (h d) -> p h d", h=4), func=AF.Copy)
                o = COL["d_v"][0]
                kb.op("act", "activation", [proj], [vs["D"]], out=vs["D"][:, :, 0:64],
                      in_=proj[:, o:o + 256].rearrange("p (h d) -> p h d", h=4), func=AF.Copy)
                yield
                o = COL["a_cq"][0]
                kb.op("act", "activation", [proj], [junk2, ss2], out=junk2[:, 0:256], in_=proj[:, o:o + 256],
                      func=AF.Square, accum_out=ss2[:, 1:2])
                kb.op("act", "activation", [proj], [junk2, ss2], out=junk2[:, 256:384], in_=proj[:, o + 256:o + 384],
                      func=AF.Square, accum_out=ss2[:, 2:3])
                kb.op("dve", "tensor_scalar", [ss2], [rstd2], out=rstd2[:, 1:2], in0=ss2[:, 1:2], scalar1=1.0 / 256,
                      scalar2=EPS, op0=ALU.mult, op1=ALU.add)
                kb.op("dve", "tensor_scalar", [ss2], [rstd2], out=rstd2[:, 2:3], in0=ss2[:, 2:3], scalar1=1.0 / 128,
                      scalar2=EPS, op0=ALU.mult, op1=ALU.add)
                kb.op("act", "activation", [rstd2], [rstd2], out=rstd2[:, 1:3], in_=rstd2[:, 1:3], func=AF.Ln)
                kb.op("act", "activation", [rstd2], [rstd2], out=rstd2[:, 1:3], in_=rstd2[:, 1:3], func=AF.Exp,
                      scale=-0.5)
                kb.op("dve", "scalar_tensor_tensor", [proj, rstd2, gq], [cqn], out=cqn[:, 0:256],
                      in0=proj[:, o:o + 256], scalar=rstd2[:, 1:2], in1=gq[:], op0=ALU.mult, op1=ALU.mult)
                kb.op("dve", "scalar_tensor_tensor", [proj, rstd2, gkv], [cqn], out=cqn[:, 256:384],
                      in0=proj[:, o + 256:o + 384], scalar=rstd2[:, 2:3], in1=gkv[:], op0=ALU.mult, op1=ALU.mult)
                for kc in range(3):
                    kb.op("pe", "transpose", [cqn, ident], [bT], out=bTv[:, kc * 128:(kc + 1) * 128],
                          in_=cqn[:, kc * 128:(kc + 1) * 128], identity=ident[:, 0:128])
                kb.op("act", "activation", [bT], [cqnT], out=cqnT[:].rearrange("p k t -> p (k t)"),
                      in_=bTv[:, 0:384], func=AF.Copy)
                yield
                bq, bkv = banks[0], banks[1]
                for kc in range(2):
                    kb.op("pe", "matmul", [cqnT, wuq], [bq], out=bq[:, 0:384], lhsT=cqnT[:, kc, :], rhs=wuq[:, kc, :],
                          start=(kc == 0), stop=(kc == 1))
                kb.op("pe", "matmul", [cqnT, wukv], [bkv], out=bkv[:, 0:512], lhsT=cqnT[:, 2, :], rhs=wukv[:, :],
                      start=True, stop=True)
                bq3 = bq[:, 0:384].rearrange("p (h d) -> p h d", h=4)
                kb.op("act", "activation", [bq], [qa], out=qa[:, :, 0:64], in_=bq3[:, :, 0:64], func=AF.Copy)
                kb.op("act", "activation", [bq], [tmp3], out=tmp3[:, 0:128].rearrange("p (h d) -> p h d", h=4),
                      in_=bq3[:, :, 64:96], func=AF.Copy)
                rope(tmp3, tmp3[:, 0:128].rearrange("p (h two d) -> p h two d", h=4, two=2), qa,
                     qa[:, :, 64:96].rearrange("p h (two d) -> p h two d", two=2), c32, s32, 4, 16, tmp3a, tmp3b)
                bkv3 = bkv[:, 0:512].rearrange("p (h d) -> p h d", h=4)
                kb.op("act", "activation", [bkv], [ka], out=ka[:, :, 0:64], in_=bkv3[:, :, 0:64], func=AF.Copy)
                kb.op("dve", "tensor_copy", [bkv], [vs["A"]], out=vs["A"][:, :, 0:64], in_=bkv3[:, :, 64:128])
                kb.op("pool", "tensor_copy", [r32], [ka], out=ka[:, :, 64:96],
                      in_=r32[:, 9:10, :].to_broadcast([128, 4, 32]))
                yield
                o = COL["d_f"][0]
                kb.op("dve", "scalar_tensor_tensor", [proj, nfb], [logf], out=logf[:], in0=proj[:, o:o + 4],
                      scalar=-1.0, in1=nfb[:], op0=ALU.mult, op1=ALU.add)
                kb.op("act", "activation", [logf], [logf], out=logf[:], in_=logf[:], func=AF.Exp)
                kb.op("act", "activation", [logf], [logf], out=logf[:], in_=logf[:], func=AF.Ln, bias=1.0)
                kb.op("dve", "tensor_scalar", [logf], [logf], out=logf[:], in0=logf[:], scalar1=-1.0, scalar2=None,
                      op0=ALU.mult)
                bc = banks[2]
                cprev, ccur = cum[(tt + 1) % 2], cum[tt % 2]
                kb.op("pe", "matmul", [cm_f, logf], [bc], out=bc[:, 0:4], lhsT=TRI, rhs=logf[:], start=True,
                      stop=False)
                kb.op("pe", "matmul", [cm_f, cprev], [bc], out=bc[:, 0:4], lhsT=LAST, rhs=cprev[:], start=False,
                      stop=True)
                kb.op("dve", "tensor_copy", [bc], [ccur], out=ccur[:], in_=bc[:, 0:4])
                kb.op("dve", "tensor_scalar", [ccur], [c8], out=c8[:], in0=ccur[:], scalar1=8.0, scalar2=None,
                      op0=ALU.mult)
                kb.op("dve", "tensor_copy", [c8], [cp], out=cp[:, 0, :], in_=c8[:])
                kb.op("dve", "tensor_tensor", [c8, cp], [cr], out=cr[:], in0=c8[:], in1=cp[:, 0, :], op=ALU.subtract)
                kb.op("dve", "tensor_copy", [cr], [cp], out=cp[:, 1, :], in_=cr[:])
                kb.op("dve", "tensor_tensor", [cr, cp], [cr], out=cr[:], in0=cr[:], in1=cp[:, 1, :], op=ALU.subtract)
                kb.op("dve", "tensor_copy", [cr], [cp], out=cp[:, 2, :], in_=cr[:])
                o = COL["d_q"][0]
                kb.op("pool", "tensor_copy", [proj], [qd], out=qd[:, :, 0:64],
                      in_=proj[:, o:o + 256].rearrange("p (h d) -> p h d", h=4))
                kb.op("pool", "tensor_copy", [cp], [qd], out=qd[:, :, 64:65], in_=cp[:, 0, :].unsqueeze(2))
                o = COL["d_k"][0]
                kb.op("pool", "tensor_copy", [proj], [kd], out=kd[:, :, 0:64],
                      in_=proj[:, o:o + 256].rearrange("p (h d) -> p h d", h=4))
                kb.op("dve", "tensor_scalar", [cp], [kd], out=kd[:, :, 65:68], in0=cp[:].rearrange("p c h -> p h c"),
                      scalar1=-1.0, scalar2=None, op0=ALU.mult)
                yield
                o = COL["c_w"][0]
                ws = wst[par]
                kb.op("pool", "tensor_copy", [proj], [ws], out=ws[:], in_=proj[:, o:o + 8])
                kb.dma("sp", wi_d[tok, :], ws[:], reads=[ws], writes=[r_wi], owner=ws)
                for g in "ABCD":
                    kb.dma("sp", v_d[g][tok, :, :], vs[g][:], reads=[vs[g]], writes=[r_v[g]], owner=vs[g])
                yield
                items = [
                    (qa, [qa[:, h, :] for h in range(4)], 96, qT_d["A"], r_qT["A"]),
                    (ka, [ka[:, h, :] for h in range(4)], 96, kT_d["A"], r_kT["A"]),
                    (r64, [r64[:, h, :] for h in range(0, 4)], 64, qT_d["B"], r_qT["B"]),
                    (r64, [r64[:, h, :] for h in range(4, 6)], 64, kT_d["B"], r_kT["B"]),
                    (r64, [r64[:, h, :] for h in range(6, 10)], 64, qT_d["C"], r_qT["C"]),
                    (r64, [r64[:, h, :] for h in range(10, 14)], 64, kT_d["C"], r_kT["C"]),
                    (qd, [qd[:, h, :] for h in range(4)], 68, qT_d["D"], r_qT["D"]),
                    (kd, [kd[:, h, :] for h in range(4)], 68, kT_d["D"], r_kT["D"]),
                    (r32, [r32[:, h, :] for h in range(0, 4)], 32, qiT_d[:, 0:4, :], r_qiT),
                    (r32, [r32[:, h, :] for h in range(4, 8)], 32, qiT_d[:, 4:8, :], r_qiT),
                    (r32, [r32[:, 8, :]], 32, None, r_kiT),
                ]
                for ii, (src, aps, n, dst, rdst) in enumerate(items):
                    bk = banks[3 + (ii % 3)]
                    bkv_ = bk[:].bitcast(BF16)
                    nh = len(aps)
                    for h, ap in enumerate(aps):
                        kb.op("pe", "transpose", [src, ident], [bk], out=bkv_[0:n, h * 128:(h + 1) * 128], in_=ap,
                              identity=ident[:, 0:128])
                    sg = tst[par][ii % 9] if ii < 9 else tst[par][ii - 9 + 0]
                    en = "act" if ii % 2 == 0 else "dve"
                    if en == "act":
                        kb.op("act", "activation", [bk], [sg], out=sg[0:n, 0:nh, :].rearrange("p h t -> p (h t)"),
                              in_=bkv_[0:n, 0:nh * 128], func=AF.Copy)
                    else:
                        kb.op("dve", "tensor_copy", [bk], [sg], out=sg[0:n, 0:nh, :].rearrange("p h t -> p (h t)"),
                              in_=bkv_[0:n, 0:nh * 128])
                    if dst is None:
                        kb.dma("sp", kiT_d[:, tok], sg[0:32, 0, :], reads=[sg], writes=[rdst], owner=sg)
                    else:
                        kb.dma("sp", dst[:, :, tok], sg[0:n, 0:nh, :], reads=[sg], writes=[rdst], owner=sg)
                    if ii % 3 == 2:
                        yield
            stage1(0)
            for tt in range(NT):
                g2 = stage2(tt)
                if tt + 1 < NT:
                    stage1(tt + 1, g2)
                else:
                    for _ in g2:
                        pass
            kb.barrier()
            kb.stack = old

    def attention(l, g, hook=None):
        dk = DKA[g]
        hk = HK[g]
        gi = "ABCD".index(g)
        scale = {"A": 96 ** -0.5, "B": 0.125, "C": 0.125, "D": 0.125}[g]
        with ExitStack() as st2:
            old = kb.stack
            kb.stack = st2
            qT = kb.sb("aqT", [dk, 4, S], BF16)
            kT = kb.sb("akT", [dk, hk, S], BF16)
            vv = kb.sb("avv", [128, NT, hk * 65], BF16)
            for h in range(4):
                kb.dma("sp", qT[:, h, :], qT_d[g][:, h, :], reads=[r_qT[g]], writes=[qT], owner=qT)
            for h in range(hk):
                kb.dma("sp", kT[:, h, :], kT_d[g][:, h, :], reads=[r_kT[g]], writes=[kT], owner=kT)
            vsrc = v_d[g].rearrange("(j p) h d -> p j (h d)", p=128)
            for j0 in range(0, NT, 4):
                kb.dma("sp", vv[:, j0:j0 + 4, :], vsrc[:, j0:j0 + 4, :], reads=[r_v[g]], writes=[vv], owner=vv)
            pT = [kb.sb("apT%d" % i, [128, 512], BF16) for i in range(2)]
            osb = [kb.sb("aosb%d" % i, [128, 4, 64], BF16) for i in range(2)]
            den = kb.sb("aden", [128, 4], F32)
            if g == "B":
                esink = kb.sb("esink", [128, 4], F32)
                bcast_load(esink, sinks_d[l:l + 1, :], 4)
                kb.op("act", "activation", [esink], [esink], out=esink[:], in_=esink[:], func=AF.Exp)
            if g == "C":
                qiTs = [kb.sb("cqiT%d" % i, [32, 8, 128], BF16) for i in range(2)]
                kiT = kb.sb("ckiT", [32, S], BF16)
                kb.dma("sp", kiT[:], kiT_d[:, :], reads=[r_kiT], writes=[kiT], owner=kiT)
                wsb = kb.sb("cwsb", [128, NT, 8], F32)
                wsrc = wi_d.rearrange("(j p) h -> p j h", p=128)
                for j0 in range(0, NT, 4):
                    kb.dma("sp", wsb[:, j0:j0 + 4, :], wsrc[:, j0:j0 + 4, :], reads=[r_wi], writes=[wsb], owner=wsb)
                Isbs = [kb.sb("cI%d" % i, [128, S], F32) for i in range(2)]
                Madd = [kb.sb("cMadd%d" % i, [128, S], BF16) for i in range(2)]
                rl = [kb.sb("crl%d" % i, [128, 512], BF16) for i in range(3)]
                dgs = [kb.sb("cdg%d" % i, [128, 8, 128], BF16) for i in range(2)]
                cjunk = kb.sb("cjunk", [128, S], BF16)
                lo = kb.sb("clo", [128, 1], F32)
                stp = kb.sb("cstp", [128, 1], F32)
                cand = kb.sb("ccand", [128, 1], F32)
                cnt = kb.sb("ccnt", [128, 1], F32)
                mm = kb.sb("cmm", [128, 1], F32)
                hi = kb.sb("chi", [128, 1], F32)

            def kts_of(qt):
                if g == "B":
                    return [kt for kt in (qt - 1, qt) if kt >= 0]
                return list(range(qt + 1))

            def c_scores(qt):
                qs = slice(qt * 128, (qt + 1) * 128)
                Lk = 128 * (qt + 1)
                nblk = (Lk + 511) // 512
                Isb = Isbs[qt % 2]
                qiT = qiTs[qt % 2]
                dg = dgs[qt % 2]
                kb.dma("pool", qiT[:], qiT_d[:, :, qs], reads=[r_qiT], writes=[qiT], owner=qiT)
                kb.op("pool", "tensor_tensor", [ident4, wsb], [dg], out=dg[:],
                      in0=ident4[:, 0:128].unsqueeze(1).to_broadcast([128, 8, 128]),
                      in1=wsb[:, qt, :].unsqueeze(2).to_broadcast([128, 8, 128]), op=ALU.mult)
                hc = 0
                for kbk in range(nblk):
                    k0, k1 = kbk * 512, min((kbk + 1) * 512, Lk)
                    n = k1 - k0
                    bacc = banks[6 + (kbk % 2)]
                    pend = None
                    for hh in range(8):
                        bi = banks[4 + (hc % 2)]
                        r_ = rl[hc % 3]
                        kb.op("pe", "matmul", [qiT, kiT], [bi], out=bi[:, 0:n], lhsT=qiT[:, hh, :],
                              rhs=kiT[:, k0:k1], start=True, stop=True)
                        kb.op("act", "activation", [bi], [r_], out=r_[:, 0:n], in_=bi[:, 0:n], func=AF.Relu)
                        if pend is not None:
                            ph, pr = pend
                            kb.op("pe", "matmul", [dg, pr], [bacc], out=bacc[:, 0:n], lhsT=dg[:, ph, :], rhs=pr[:, 0:n],
                                  start=(ph == 0), stop=False)
                        pend = (hh, r_)
                        hc += 1
                    ph, pr = pend
                    kb.op("pe", "matmul", [dg, pr], [bacc], out=bacc[:, 0:n], lhsT=dg[:, ph, :], rhs=pr[:, 0:n],
                          start=False, stop=True)
                    kb.op("act", "activation", [bacc], [Isb], out=Isb[:, k0:k1], in_=bacc[:, 0:n], func=AF.Copy)

            def c_select(qt):
                qs = slice(qt * 128, (qt + 1) * 128)
                Lk = 128 * (qt + 1)
                Isb = Isbs[qt % 2]
                madd = Madd[qt % 2]
                kb.op("dve", "tensor_tensor", [Isb, cm_f], [Isb], out=Isb[:, qs], in0=Isb[:, qs], in1=negS, op=ALU.add)
                if Lk <= topk:
                    kb.op("dve", "tensor_scalar", [Isb], [madd], out=madd[:, 0:Lk], in0=Isb[:, 0:Lk],
                          scalar1=-1.0e29, scalar2=-BIGM, op0=ALU.is_lt, op1=ALU.mult)
                    return
                assert Lk - 128 >= topk
                kb.op("dve", "tensor_reduce", [Isb], [hi], out=hi[:], in_=Isb[:, 0:Lk], axis=AX.X, op=ALU.max)
                kb.op("dve", "tensor_reduce", [Isb], [lo], out=lo[:], in_=Isb[:, 0:Lk - 128], axis=AX.X, op=ALU.min)
                kb.op("dve", "tensor_tensor", [hi, lo], [stp], out=stp[:], in0=hi[:], in1=lo[:], op=ALU.subtract)
                for it in range(N_BISECT):
                    f = 0.5 ** (it + 1)
                    kb.op("dve", "scalar_tensor_tensor", [stp, lo], [cand], out=cand[:], in0=stp[:], scalar=f,
                          in1=lo[:], op0=ALU.mult, op1=ALU.add)
                    kb.op("dve", "tensor_scalar", [Isb, cand], [cjunk, cnt], out=cjunk[:, 0:Lk], in0=Isb[:, 0:Lk],
                          scalar1=cand[:, 0:1], scalar2=None, op0=ALU.is_ge, op1=ALU.add, accum_out=cnt[:, 0:1])
                    kb.op("dve", "scalar_tensor_tensor", [cnt, stp], [mm], out=mm[:], in0=cnt[:],
                          scalar=float(topk) - 0.5, in1=stp[:], op0=ALU.is_ge, op1=ALU.mult)
                    kb.op("dve", "scalar_tensor_tensor", [mm, lo], [lo], out=lo[:], in0=mm[:], scalar=f, in1=lo[:],
                          op0=ALU.mult, op1=ALU.add)
                kb.op("dve", "tensor_scalar", [Isb, lo], [madd], out=madd[:, 0:Lk], in0=Isb[:, 0:Lk],
                      scalar1=lo[:, 0:1], scalar2=-BIGM, op0=ALU.is_lt, op1=ALU.mult)

            def emit_scores(qt, ki, kt):
                qs = slice(qt * 128, (qt + 1) * 128)
                ks = slice(kt * 128, (kt + 1) * 128)
                bs = banks[ki % 2]
                have_mask = False
                if g == "C":
                    madd = Madd[qt % 2]
                    kb.op("pe", "matmul", [madd, ident4], [bs], out=bs[:, 0:512], lhsT=madd[:, ks],
                          rhs=ident4[:, 0:512], start=True, stop=False, skip_group_check=True)
                    have_mask = True
                elif kt == qt:
                    kb.op("pe", "matmul", [ident4, maskc4], [bs], out=bs[:, 0:512], lhsT=ident4[:, 0:128],
                          rhs=maskc4[:, 0:512], start=True, stop=False, skip_group_check=True)
                    have_mask = True
                elif g == "B":
                    kb.op("pe", "matmul", [ident4, maskp4], [bs], out=bs[:, 0:512], lhsT=ident4[:, 0:128],
                          rhs=maskp4[:, 0:512], start=True, stop=False, skip_group_check=True)
                    have_mask = True
                for h in range(4):
                    kvh = h if hk == 4 else h // 2
                    kb.op("pe", "matmul", [kT, qT], [bs], out=bs[:, h * 128:(h + 1) * 128], lhsT=kT[:, kvh, ks],
                          rhs=qT[:, h, qs], start=((not have_mask) and h == 0), stop=True, skip_group_check=True)
                p = pT[ki % 2]
                kb.op("act", "activation", [bs], [p], out=p[:], in_=bs[:, 0:512], func=AF.Exp, scale=scale)

            def emit_pv(qt, ki, kt, nk):
                bo = banks[2 + (qt % 2)]
                p = pT[ki % 2]
                for h in range(4):
                    kvh = h if hk == 4 else h // 2
                    kb.op("pe", "matmul", [p, vv], [bo], out=bo[:, h * 65:(h + 1) * 65],
                          lhsT=p[:, h * 128:(h + 1) * 128], rhs=vv[:, kt, kvh * 65:(kvh + 1) * 65],
                          start=(ki == 0 and h == 0), stop=(ki == nk - 1), skip_group_check=True)

            def emit_attn(qt):
                kts = kts_of(qt)
                prev = None
                for ki, kt in enumerate(kts):
                    emit_scores(qt, ki, kt)
                    if prev is not None:
                        emit_pv(qt, prev[0], prev[1], len(kts))
                    prev = (ki, kt)
                emit_pv(qt, prev[0], prev[1], len(kts))

            def emit_norm(qt):
                qs = slice(qt * 128, (qt + 1) * 128)
                bo = banks[2 + (qt % 2)]
                bo3 = bo[:, 0:260].rearrange("p (h d) -> p h d", h=4)
                if g == "B":
                    kb.op("dve", "tensor_tensor", [bo, esink], [den], out=den[:].unsqueeze(2), in0=bo3[:, :, 64:65],
                          in1=esink[:].unsqueeze(2), op=ALU.add)
                else:
                    kb.op("dve", "tensor_copy", [bo], [den], out=den[:].unsqueeze(2), in_=bo3[:, :, 64:65])
                kb.op("dve", "reciprocal", [den], [den], out=den[:], in_=den[:])
                ob = osb[qt % 2]
                kb.op("dve", "tensor_tensor", [bo, den], [ob], out=ob[:], in0=bo3[:, :, 0:64],
                      in1=den[:].unsqueeze(2).to_broadcast([128, 4, 64]), op=ALU.mult)
                kb.dma("sp", mixed_d[qs, gi * 256:(gi + 1) * 256], ob[:].rearrange("p h d -> p (h d)"), reads=[ob],
                       writes=[r_mixed], owner=ob)

            if g == "C":
                c_scores(0)
                for qt in range(NT):
                    if qt + 1 < NT:
                        c_scores(qt + 1)
                    c_select(qt)
                    if qt > 0:
                        emit_norm(qt - 1)
                    emit_attn(qt)
                emit_norm(NT - 1)
            else:
                for qt in range(NT):
                    emit_attn(qt)
                    emit_norm(qt)
                    if hook:
                        hook.pop(0)()
                while hook:
                    hook.pop(0)()
            kb.barrier()
            kb.stack = old

    def mlp_weight_chunks(l, wu, wd, stg, which):
        ems = []
        engs = ("dve", "pool", "dve") if which == "u" else ("pool", "act", "dve")

        def mk(c, dst_ap, src_ap, ncols):
            def em():
                st = stg[c % len(stg)]
                kb.dma("sp", st[:, 0:ncols], src_ap, reads=[r_const], writes=[st], owner=st)
                en = engs[c % 3]
                dst_tile = wu if c < 16 else wd
                if en == "act":
                    kb.op("act", "activation", [st], [dst_tile], out=dst_ap, in_=st[:, 0:ncols], func=AF.Copy)
                else:
                    kb.op(en, "tensor_copy", [st], [dst_tile], out=dst_ap, in_=st[:, 0:ncols])
            return em

        if which == "u":
            for c in range(16):
                ems.append(mk(c, wu[:, c // 2, (c % 2) * 2048:(c % 2 + 1) * 2048],
                              w_up_d[l, (c // 2) * 128:(c // 2 + 1) * 128, (c % 2) * 2048:(c % 2 + 1) * 2048], 2048))
        else:
            for c in range(32):
                ems.append(mk(16 + c, wd[:, c, :], w_down_d[l, c * 128:(c + 1) * 128, :], 1024))
        return ems

    def phase3a(l, xsrc_d, r_xsrc, wchunks):
        with ExitStack() as st3:
            old = kb.stack
            kb.stack = st3
            wo = kb.sb("wo", [128, 8, 1024], BF16)
            stg = [kb.sb("p3stg%d" % i, [128, 1024], F32) for i in range(2)]
            load_cast_weight(wo, lambda c: wo[:, c, :], lambda c: w_out_d[l, c * 128:(c + 1) * 128, :], 8, 1024, stg)
            mx = [kb.sb("mx%d" % i, [128, 1024], BF16) for i in range(2)]
            mT = [kb.sb("mT%d" % i, [128, 8, 128], BF16) for i in range(2)]
            xt = [kb.sb("x3t%d" % i, [128, 1024], F32) for i in range(2)]
            xo = [kb.sb("x3o%d" % i, [128, 1024], F32) for i in range(2)]
            def sA(tt):
                par = tt % 2
                tok = slice(tt * 128, (tt + 1) * 128)
                kb.dma("pool", mx[par][:], mixed_d[tok, :], reads=[r_mixed], writes=[mx[par]], owner=mx[par])
                kb.dma("pool", xt[par][:], xsrc_d[tok, :], reads=[r_xsrc], writes=[xt[par]], owner=xt[par])
                bT = banks[4 + par]
                bTv = bT[:].bitcast(BF16)
                for kc in range(8):
                    kb.op("pe", "transpose", [mx[par], ident], [bT], out=bTv[:, kc * 128:(kc + 1) * 128],
                          in_=mx[par][:, kc * 128:(kc + 1) * 128], identity=ident[:, 0:128])
                kb.op("act", "activation", [bT], [mT[par]], out=mT[par][:].rearrange("p k t -> p (k t)"), in_=bTv,
                      func=AF.Copy)

            def sB(tt):
                par = tt % 2
                tok = slice(tt * 128, (tt + 1) * 128)
                for nb in range(2):
                    bk = banks[2 * par + nb]
                    for kc in range(8):
                        kb.op("pe", "matmul", [mT[par], wo], [bk], out=bk[:, 0:512], lhsT=mT[par][:, kc, :],
                              rhs=wo[:, kc, nb * 512:(nb + 1) * 512], start=(kc == 0), stop=(kc == 7))
                    kb.op("dve", "tensor_tensor", [bk, xt[par]], [xo[par]], out=xo[par][:, nb * 512:(nb + 1) * 512],
                          in0=bk[:, 0:512], in1=xt[par][:, nb * 512:(nb + 1) * 512], op=ALU.add)
                kb.dma("sp", xm_d[tok, :], xo[par][:], reads=[xo[par]], writes=[r_xm], owner=xo[par])

            sA(0)
            wq = list(wchunks)
            per = (len(wq) + NT - 1) // NT
            for tt in range(NT):
                if tt + 1 < NT:
                    sA(tt + 1)
                sB(tt)
                for _ in range(per):
                    if wq:
                        wq.pop(0)()
            while wq:
                wq.pop(0)()
            kb.barrier()
            kb.stack = old

    def phase3b(l, xdst_d, r_xdst, final, wu, wd):
        with ExitStack() as st4:
            old = kb.stack
            kb.stack = st4
            g2 = kb.sb("g2", [128, 1024], F32)
            bcast_load(g2, norm2_d[l:l + 1, :], 1024)
            if final:
                gf = kb.sb("gf", [128, 1024], F32)
                bcast_load(gf, fnorm_d[0:1, :], 1024)
            TB = 2
            xt = [[kb.sb("x4t%d_%d" % (i, j), [128, 1024], F32) for j in range(TB)] for i in range(2)]
            hb = kb.sb("h4b", [128, 1024], BF16)
            hT = [kb.sb("h4T%d" % i, [128, 8, TB * 128], BF16) for i in range(2)]
            uT = [kb.sb("u4T%d" % i, [128, TB * 128], BF16) for i in range(4)]
            rT = [kb.sb("r4T%d" % i, [128, TB * 128], F32) for i in range(3)]
            junk = kb.sb("junk4", [128, 1024], F32)
            ss = kb.sb("ss4", [128, 2], F32)
            rstd = kb.sb("rstd4", [128, 2], F32)
            nblk = NT // TB
            def pre(b):
                par = b % 2
                for j in range(TB):
                    tok = slice((b * TB + j) * 128, (b * TB + j + 1) * 128)
                    x = xt[par][j]
                    kb.dma("pool", x[:], xm_d[tok, :], reads=[r_xm], writes=[x], owner=x)
                    rmsnorm_rstd(x, x[:], 1024, ss, rstd, junk)
                    kb.op("dve", "scalar_tensor_tensor", [x, rstd, g2], [hb], out=hb[:], in0=x[:],
                          scalar=rstd[:, 0:1], in1=g2[:], op0=ALU.mult, op1=ALU.mult)
                    bT = banks[7]
                    bTv = bT[:].bitcast(BF16)
                    for kc in range(8):
                        kb.op("pe", "transpose", [hb, ident], [bT], out=bTv[:, kc * 128:(kc + 1) * 128],
                              in_=hb[:, kc * 128:(kc + 1) * 128], identity=ident[:, 0:128])
                    kb.op("act", "activation", [bT], [hT[par]], out=hT[par][:, :, j * 128:(j + 1) * 128],
                          in_=bTv.rearrange("p (k t) -> p k t", k=8), func=AF.Copy)
            def main(b):
                par = b % 2
                accs = [banks[0], banks[1], banks[2], banks[3]]
                def emit_up(fc):
                    bu = banks[4 + fc % 3]
                    for kc in range(8):
                        kb.op("pe", "matmul", [wu, hT[par]], [bu], out=bu[:, 0:TB * 128],
                              lhsT=wu[:, kc, fc * 128:(fc + 1) * 128], rhs=hT[par][:, kc, :], start=(kc == 0),
                              stop=(kc == 7))
                    u = uT[fc % 4]
                    rr = rT[fc % 3]
                    kb.op("act", "activation", [bu], [rr], out=rr[:], in_=bu[:, 0:TB * 128], func=AF.Relu)
                    kb.op("dve" if fc % 2 == 0 else "pool", "tensor_tensor", [rr], [u], out=u[:], in0=rr[:],
                          in1=rr[:], op=ALU.mult)

                def emit_down(fc):
                    u = uT[fc % 4]
                    for j in range(TB):
                        for nb in range(2):
                            acc = accs[j * 2 + nb]
                            kb.op("pe", "matmul", [u, wd], [acc], out=acc[:, 0:512], lhsT=u[:, j * 128:(j + 1) * 128],
                                  rhs=wd[:, fc, nb * 512:(nb + 1) * 512], start=(fc == 0), stop=(fc == 31))

                emit_up(0)
                emit_up(1)
                for fc in range(32):
                    if fc + 2 < 32:
                        emit_up(fc + 2)
                    emit_down(fc)
                for j in range(TB):
                    tok = slice((b * TB + j) * 128, (b * TB + j + 1) * 128)
                    o = xt[par][j]
                    for nb in range(2):
                        acc = accs[j * 2 + nb]
                        kb.op("dve", "tensor_tensor", [acc, xt[par][j]], [o], out=o[:, nb * 512:(nb + 1) * 512],
                              in0=acc[:, 0:512], in1=xt[par][j][:, nb * 512:(nb + 1) * 512], op=ALU.add)
                    if final:
                        kb.op("act", "activation", [o], [junk, ss], out=junk[:], in_=o[:], func=AF.Square,
                              accum_out=ss[:, 1:2])
                        kb.op("dve", "tensor_scalar", [ss], [rstd], out=rstd[:, 1:2], in0=ss[:, 1:2],
                              scalar1=1.0 / 1024, scalar2=EPS, op0=ALU.mult, op1=ALU.add)
                        kb.op("act", "activation", [rstd], [rstd], out=rstd[:, 1:2], in_=rstd[:, 1:2], func=AF.Sqrt)
                        kb.op("dve", "reciprocal", [rstd], [rstd], out=rstd[:, 1:2], in_=rstd[:, 1:2])
                        kb.op("dve", "scalar_tensor_tensor", [o, rstd, gf], [o], out=o[:], in0=o[:],
                              scalar=rstd[:, 1:2], in1=gf[:], op0=ALU.mult, op1=ALU.mult)
                    kb.dma("sp", xdst_d[tok, :], o[:], reads=[o], writes=[r_xdst], owner=o)

            pre(0)
            for b in range(nblk):
                if b + 1 < nblk:
                    pre(b + 1)
                main(b)
            kb.barrier()
            kb.stack = old

    cur_d, cur_r = x_in, r_xin
    for l in range(depth):
        phase1(l, cur_d, cur_r)
        for g in "ABC":
            attention(l, g)
        with ExitStack() as stw:
            oldw = kb.stack
            kb.stack = stw
            wu = kb.sb("wu", [128, 8, D_FF], BF16)
            wstg = [kb.sb("p4stg%d" % i, [128, 2048], F32) for i in range(2)]
            attention(l, "D", hook=mlp_weight_chunks(l, wu, None, wstg, "u"))
            wd = kb.sb("wd", [128, 32, 1024], BF16)
            phase3a(l, cur_d, cur_r, mlp_weight_chunks(l, wu, wd, wstg, "d"))
            last = (l == depth - 1)
            if last:
                phase3b(l, out_d, r_out, True, wu, wd)
            else:
                phase3b(l, xs[l % 2], r_xs[l % 2], False, wu, wd)
                cur_d, cur_r = xs[l % 2], r_xs[l % 2]
            kb.stack = oldw
    kb.wait_all("sp", [r_out])
    kb.wait_all("pool", [r_out])
    kb.barrier()
    print("KB: nsem=%d instr=%s" % (kb.nsem, {n: len(E.prog) for n, E in kb.engs.items()}), flush=True)
    kb.replay()
    stack.close()
    return nc


def host_consts(S):
    pos = np.arange(S, dtype=np.float32)

    def tables(d):
        half = d // 2
        inv = (1.0 / (10000.0 ** (np.arange(0, half, dtype=np.float32) * 2.0 / d))).astype(np.float32)
        ang = pos[:, None] * inv[None, :]
        return np.cos(ang).astype(np.float32), np.sin(ang).astype(np.float32)

    c64, s64 = tables(64)
    c32, s32 = tables(32)
    cm = np.zeros((128, 5 * 512), np.float32)
    eye = np.eye(128, dtype=np.float32)
    kk = np.arange(128)[:, None]
    qq = np.arange(128)[None, :]
    mc = np.where(kk > qq, -BIGM, 0.0).astype(np.float32)
    mp = np.where(kk <= qq, -BIGM, 0.0).astype(np.float32)
    for h in range(4):
        cm[:, h * 128:(h + 1) * 128] = eye
        cm[:, 512 + h * 128:512 + (h + 1) * 128] = mc
        cm[:, 1024 + h * 128:1024 + (h + 1) * 128] = mp
    cm[:, 1536:1664] = np.where(qq > kk, NEG_S, 0.0)
    cm[:, 1664:1792] = (kk <= qq).astype(np.float32)
    cm[:, 1792:1920] = (kk == 127).astype(np.float32) * np.ones((1, 128), np.float32)
    cm[:, 2048:2176] = eye
    return c64, s64, c32, s32, cm


_CACHE = {}


def kernel(x, norm1, w_in, mla_q_norm, mla_kv_norm, mla_w_uq, mla_w_ukv, swa_sinks, fox_b_f, w_out, norm2, w_up,
           w_down, final_norm, _depth=None, _ncores=None, _debug=False):
    x = np.asarray(x, dtype=np.float32)
    B, S, _ = x.shape
    depth = int(_depth) if _depth is not None else int(np.asarray(w_in).shape[0])
    ncores = int(_ncores) if _ncores is not None else B
    f = lambda a: np.ascontiguousarray(np.asarray(a, dtype=np.float32))
    key = (S, depth, _debug)
    if key not in _CACHE:
        _CACHE[key] = build_program(S=S, depth=depth, debug=_debug)
    nc = _CACHE[key]
    c64, s64, c32, s32, cm = host_consts(S)
    shared = {
        "w_in": f(np.asarray(w_in)[:depth][:, :, PERM]),
        "w_uq": f(np.asarray(mla_w_uq)[:depth]),
        "w_ukv": f(np.asarray(mla_w_ukv)[:depth]),
        "w_out": f(np.asarray(w_out)[:depth]),
        "w_up": f(np.asarray(w_up)[:depth]),
        "w_down": f(np.asarray(w_down)[:depth]),
        "norm1": f(np.asarray(norm1)[:depth]),
        "norm2": f(np.asarray(norm2)[:depth]),
        "gq": f(np.asarray(mla_q_norm)[:depth]),
        "gkv": f(np.asarray(mla_kv_norm)[:depth]),
        "sinks": f(np.asarray(swa_sinks)[:depth]),
        "foxb": f(np.asarray(fox_b_f)[:depth]),
        "fnorm": f(np.asarray(final_norm).reshape(1, -1)),
        "ropet": np.ascontiguousarray(np.concatenate([c64, s64, c32, s32], axis=1)), "cmat": cm,
    }
    in_maps = []
    for b in range(ncores):
        m = dict(shared)
        m["x"] = f(x[b])
        in_maps.append(m)
    res = run_bass_kernel_spmd(nc, in_maps, core_ids=list(range(ncores)))
    out = np.stack([np.asarray(r["out"], dtype=np.float32) for r in res.results], axis=0)
    if _debug:
        return out, res.results
    return out
```

```python
import numpy as np
from contextlib import ExitStack
import concourse.bass as bass
import concourse.mybir as mybir
from concourse.bass_utils import run_bass_kernel_spmd

F32 = mybir.dt.float32
BF16 = mybir.dt.bfloat16
ALU = mybir.AluOpType
AF = mybir.ActivationFunctionType
AX = mybir.AxisListType

D_MODEL = 1024
DEPTH = 4
SEQ = 4096
IN_WIDTH = 2764
D_FF = 4096
EPS = 1e-6
BIGM = 262144.0
NEG_S = -1.0e30
N_BISECT = 15

_ORIG = dict(a_cq=(0, 256), a_ckv=(256, 384), a_kr=(384, 416), b_q=(416, 672), b_k=(672, 800), b_v=(800, 928),
             c_q=(928, 1184), c_k=(1184, 1440), c_v=(1440, 1696), c_qi=(1696, 1952), c_ki=(1952, 1984),
             c_w=(1984, 1992), d_q=(1992, 2248), d_k=(2248, 2504), d_v=(2504, 2760), d_f=(2760, 2764))
_ORDER = ["b_q", "b_k", "c_q", "c_k", "c_qi", "c_ki", "a_kr", "b_v", "c_v", "d_q", "d_k", "d_v",
          "a_cq", "a_ckv", "c_w", "d_f"]
COL = {}
_perm = []
_o = 0
for _n in _ORDER:
    _a, _b = _ORIG[_n]
    COL[_n] = (_o, _o + (_b - _a))
    _perm.extend(range(_a, _b))
    _o += _b - _a
PERM = np.array(_perm, dtype=np.int64)
assert _o == IN_WIDTH


class Res:
    __slots__ = ("name", "w", "r", "sem", "cnt", "psum")

    def __init__(self, name):
        self.name = name
        self.psum = False
        self.w = {}
        self.r = {}
        self.sem = None
        self.cnt = 0


class Tile:
    def __init__(self, t, res):
        self.t = t
        self.res = res

    def __getitem__(self, k):
        return self.t[k]


class Eng:
    def __init__(self, name):
        self.name = name
        self.sem = None
        self.cnt = 0
        self.seen = {}
        self.prog = []


class KB:
    def __init__(self, nc, stack):
        self.nc = nc
        self.stack = stack
        self.gstack = stack
        self.engs = {n: Eng(n) for n in ("pe", "act", "dve", "pool", "sp")}
        self.nsem = 0
        self.sems = []
        for e in self.engs.values():
            e.sem = self.new_sem(e.name)
        self.all_res = []
        self.named = {}
        self.uid = 0

    def new_sem(self, name):
        self.nsem += 1
        h = self.gstack.enter_context(self.nc.semaphore("s%d_%s" % (self.nsem, name)))
        self.sems.append(h)
        return h

    def res(self, name):
        r = Res(name)
        self.all_res.append(r)
        return r

    def sb(self, name, shape, dt):
        self.uid += 1
        t = self.stack.enter_context(self.nc.sbuf_tensor("%s_u%d" % (name, self.uid), list(shape), dt))
        return Tile(t, self.res(name))

    def ps(self, name, shape, dt):
        t = self.gstack.enter_context(self.nc.psum_tensor(name, list(shape), dt))
        r = self.res(name)
        r.psum = True
        return Tile(t, r)

    def barrier(self):
        toks = [(E.sem, E.cnt) for E in self.engs.values() if E.cnt > 0]
        toks += [(sem, cnt) for (sem, cnt) in self.named.values()]
        for E in self.engs.values():
            waits = []
            for (sem, val) in toks:
                k = id(sem)
                if sem is E.sem or E.seen.get(k, 0) >= val:
                    continue
                waits.append((sem, val))
                E.seen[k] = val

            def emit(eng, waits=waits):
                for (s_, v) in waits:
                    eng.wait_ge(s_, v)

            E.prog.append(emit)

    @staticmethod
    def _r(x):
        return x.res if isinstance(x, Tile) else x

    def _deps(self, E, reads, writes, is_dma):
        deps = {}

        def add(d, skip_dma=False):
            for k, (sem, val, isd) in d.items():
                if skip_dma and isd:
                    continue
                if k not in deps or deps[k][1] < val:
                    deps[k] = (sem, val)

        for r in reads:
            add(self._r(r).w)
            if self._r(r).psum:
                add(self._r(r).r)
        for w in writes:
            add(self._r(w).w, skip_dma=is_dma)
            add(self._r(w).r)
        waits = []
        for k, (sem, val) in deps.items():
            if E.seen.get(k, 0) >= val:
                continue
            waits.append((sem, val))
            E.seen[k] = val
        return waits

    def op(self, en, fname, reads=(), writes=(), **kw):
        E = self.engs[en]
        reads = [self._r(x) for x in reads]
        writes = [self._r(x) for x in writes]
        own = id(E.sem)
        waits = self._deps(E, reads, writes, False)
        if en == "pe":
            waits = [(s, v) for (s, v) in waits if id(s) != own]
        if E.cnt >= 60000:
            E.sem = self.new_sem(E.name)
            E.cnt = 0
        E.cnt += 1
        sem, cnt = E.sem, E.cnt
        key = id(sem)
        tok = (sem, cnt, False)

        def emit(eng, waits=waits, fname=fname, kw=kw, sem=sem):
            for (s, v) in waits:
                eng.wait_ge(s, v)
            getattr(eng, fname)(**kw).then_inc(sem, 1)

        E.prog.append(emit)
        for r in reads:
            if key not in r.r or r.r[key][1] < cnt:
                r.r[key] = tok
        for w in writes:
            w.w = {key: tok}
            w.r = {}

    def dma(self, en, out, in_, reads=(), writes=(), owner=None, **kw):
        E = self.engs[en]
        reads = [self._r(x) for x in reads]
        writes = [self._r(x) for x in writes]
        owner = self._r(owner)
        waits = self._deps(E, reads, writes, True)
        okey = owner.name + ("@sw" if en == "pool" else "")
        if okey in self.named:
            sem, cnt = self.named[okey]
        else:
            sem, cnt = self.new_sem("d_" + okey.replace("@", "_")), 0
        cnt += 16
        assert cnt < 65000, okey
        self.named[okey] = (sem, cnt)
        key = id(sem)
        tok = (sem, cnt, True)

        def emit(eng, waits=waits, sem=sem, out=out, in_=in_, kw=kw):
            for (s, v) in waits:
                eng.wait_ge(s, v)
            eng.dma_start(out=out, in_=in_, **kw).then_inc(sem, 16)

        E.prog.append(emit)
        for r in reads:
            r.r[key] = tok
        for w in writes:
            w.w[key] = tok

    def finish(self):
        self.barrier()
        sems = list(self.sems)
        done = self.new_sem("done")
        for n, E in self.engs.items():
            if n == "pool":
                continue
            E.prog.append(lambda eng, done=done: eng.sem_inc(done, 1))

        def emit(eng, sems=sems, done=done):
            eng.wait_ge(done, 4)
            for s_ in sems:
                eng.sem_clear(s_)
            eng.sem_clear(done)

        self.engs["pool"].prog.append(emit)

    def wait_all(self, en, ress):
        E = self.engs[en]
        waits = self._deps(E, [self._r(x) for x in ress], [], False)

        def emit(eng, waits=waits):
            for (s, v) in waits:
                eng.wait_ge(s, v)

        E.prog.append(emit)

    def replay(self):
        nc = self.nc
        with nc.Block() as block:
            @block.sync
            def _(e):
                for f in self.engs["sp"].prog:
                    f(e)

            @block.tensor
            def _(e):
                for f in self.engs["pe"].prog:
                    f(e)

            @block.scalar
            def _(e):
                for f in self.engs["act"].prog:
                    f(e)

            @block.vector
            def _(e):
                for f in self.engs["dve"].prog:
                    f(e)

            @block.gpsimd
            def _(e):
                for f in self.engs["pool"].prog:
                    f(e)


def build_program(S=SEQ, depth=DEPTH, topk=None, debug=False):
    NT = S // 128
    if topk is None:
        topk = min(256, S // 4)
    nc = bass.Bass("TRN2", target_bir_lowering=False)
    stack = ExitStack()
    kb = KB(nc, stack)

    def din(name, shape, dt=F32):
        return nc.dram_tensor(name, list(shape), dt, kind="ExternalInput").ap()

    def dscr(name, shape, dt):
        kind = "ExternalOutput" if debug else "Internal"
        return nc.dram_tensor(name, list(shape), dt, kind=kind).ap()

    L = depth
    x_in = din("x", [S, D_MODEL])
    w_in_d = din("w_in", [L, D_MODEL, IN_WIDTH])
    w_uq_d = din("w_uq", [L, 256, 384])
    w_ukv_d = din("w_ukv", [L, 128, 512])
    w_out_d = din("w_out", [L, 1024, 1024])
    w_up_d = din("w_up", [L, 1024, D_FF])
    w_down_d = din("w_down", [L, D_FF, 1024])
    norm1_d = din("norm1", [L, 1024])
    norm2_d = din("norm2", [L, 1024])
    gq_d = din("gq", [L, 256])
    gkv_d = din("gkv", [L, 128])
    sinks_d = din("sinks", [L, 4])
    foxb_d = din("foxb", [L, 4])
    fnorm_d = din("fnorm", [1, 1024])
    ropet_d = din("ropet", [S, 96])
    cmat_d = din("cmat", [128, 5 * 512])
    out_d = nc.dram_tensor("out", [S, D_MODEL], F32, kind="ExternalOutput").ap()

    xs = [dscr("xs0", [S, D_MODEL], F32), dscr("xs1", [S, D_MODEL], F32)]
    xm_d = dscr("xm", [S, D_MODEL], F32)
    mixed_d = dscr("mixed", [S, D_MODEL], BF16)
    DKA = dict(A=96, B=64, C=64, D=68)
    HK = dict(A=4, B=2, C=4, D=4)
    qT_d = {g: dscr("qT_" + g, [DKA[g], 4, S], BF16) for g in "ABCD"}
    kT_d = {g: dscr("kT_" + g, [DKA[g], HK[g], S], BF16) for g in "ABCD"}
    v_d = {g: dscr("v_" + g, [S, HK[g], 65], BF16) for g in "ABCD"}
    qiT_d = dscr("qiT", [32, 8, S], BF16)
    kiT_d = dscr("kiT", [32, S], BF16)
    wi_d = dscr("wi", [S, 8], F32)

    R = kb.res
    r_xin = R("x_in")
    r_xs = [R("xs0"), R("xs1")]
    r_xm = R("xm")
    r_mixed = R("mixed")
    r_qT = {g: R("qT" + g) for g in "ABCD"}
    r_kT = {g: R("kT" + g) for g in "ABCD"}
    r_v = {g: R("v" + g) for g in "ABCD"}
    r_qiT, r_kiT, r_wi = R("qiT"), R("kiT"), R("wi")
    r_out = R("out")
    r_const = R("constin")

    banks = [kb.ps("bank%d" % i, [128, 512], F32) for i in range(8)]

    cm_f = kb.sb("cm_f", [128, 5 * 512], F32)
    kb.dma("sp", cm_f[:], cmat_d[:, :], reads=[r_const], writes=[cm_f], owner=cm_f)
    ident4 = kb.sb("ident4", [128, 512], BF16)
    maskc4 = kb.sb("maskc4", [128, 512], BF16)
    maskp4 = kb.sb("maskp4", [128, 512], BF16)
    kb.op("dve", "tensor_copy", [cm_f], [ident4], out=ident4[:], in_=cm_f[:, 0:512])
    kb.op("dve", "tensor_copy", [cm_f], [maskc4], out=maskc4[:], in_=cm_f[:, 512:1024])
    kb.op("dve", "tensor_copy", [cm_f], [maskp4], out=maskp4[:], in_=cm_f[:, 1024:1536])
    ident = ident4
    negS = cm_f[:, 1536:1664]
    TRI = cm_f[:, 1664:1792]
    LAST = cm_f[:, 1792:1920]
    identf = cm_f[:, 2048:2176]

    def bcast_load(tile_, src_row_ap, n):
        kb.dma("sp", tile_[:], src_row_ap.to_broadcast([128, n]), reads=[r_const], writes=[tile_], owner=tile_)

    def load_cast_weight(dst_tile, dst_ap_fn, src_ap_fn, nchunks, ncols, stg, engs=("dve", "act")):
        for c in range(nchunks):
            st = stg[c % len(stg)]
            kb.dma("sp", st[:, 0:ncols], src_ap_fn(c), reads=[r_const], writes=[st], owner=st)
            en = engs[c % len(engs)]
            if en == "act":
                kb.op("act", "activation", [st], [dst_tile], out=dst_ap_fn(c), in_=st[:, 0:ncols], func=AF.Copy)
            else:
                kb.op(en, "tensor_copy", [st], [dst_tile], out=dst_ap_fn(c), in_=st[:, 0:ncols])

    def rmsnorm_rstd(src_tile, src_ap, n, ss, rstd, junk, lnexp=False):
        kb.op("act", "activation", [src_tile], [junk, ss], out=junk[:, 0:n], in_=src_ap, func=AF.Square,
              accum_out=ss[:, 0:1])
        kb.op("dve", "tensor_scalar", [ss], [rstd], out=rstd[:, 0:1], in0=ss[:, 0:1], scalar1=1.0 / n,
              scalar2=EPS, op0=ALU.mult, op1=ALU.add)
        if lnexp:
            kb.op("act", "activation", [rstd], [rstd], out=rstd[:, 0:1], in_=rstd[:, 0:1], func=AF.Ln)
            kb.op("act", "activation", [rstd], [rstd], out=rstd[:, 0:1], in_=rstd[:, 0:1], func=AF.Exp, scale=-0.5)
            return
        kb.op("act", "activation", [rstd], [rstd], out=rstd[:, 0:1], in_=rstd[:, 0:1], func=AF.Sqrt)
        kb.op("dve", "reciprocal", [rstd], [rstd], out=rstd[:, 0:1], in_=rstd[:, 0:1])

    def rope(src_tile, src4, dst_tile, dst4, cos_t, sin_t, nh, half, tmp, tmpb):
        (cos_t, co), (sin_t, so) = cos_t, sin_t
        cb = cos_t[:, co:co + half].unsqueeze(1).unsqueeze(1).to_broadcast([128, nh, 2, half])
        sb_ = sin_t[:, so:so + half].unsqueeze(1).unsqueeze(1).to_broadcast([128, nh, 2, half])
        n = nh * 2 * half
        tc = tmp[:, 0:n].rearrange("p (h two d) -> p h two d", h=nh, two=2)
        ts = tmpb[:, 0:n].rearrange("p (h two d) -> p h two d", h=nh, two=2)
        kb.op("dve", "tensor_tensor", [src_tile, cos_t], [tmp], out=tc, in0=src4, in1=cb, op=ALU.mult)
        kb.op("pool", "tensor_tensor", [src_tile, sin_t], [tmpb], out=ts, in0=src4, in1=sb_, op=ALU.mult)
        kb.op("dve", "tensor_tensor", [tmp, tmpb], [dst_tile], out=dst4[:, :, 0, :], in0=tc[:, :, 0, :],
              in1=ts[:, :, 1, :], op=ALU.subtract)
        kb.op("dve", "tensor_tensor", [tmp, tmpb], [dst_tile], out=dst4[:, :, 1, :], in0=tc[:, :, 1, :],
              in1=ts[:, :, 0, :], op=ALU.add)

    def phase1(l, xsrc_d, r_xsrc):
        with ExitStack() as st1:
            old = kb.stack
            kb.stack = st1
            win = kb.sb("win", [128, 8, IN_WIDTH], BF16)
            wuq = kb.sb("wuq", [128, 2, 384], BF16)
            wukv = kb.sb("wukv", [128, 512], BF16)
            stg = [kb.sb("p1stg%d" % i, [128, IN_WIDTH], F32) for i in range(2)]
            load_cast_weight(win, lambda c: win[:, c, :], lambda c: w_in_d[l, c * 128:(c + 1) * 128, :], 8,
                             IN_WIDTH, stg)
            load_cast_weight(wuq, lambda c: wuq[:, c, :], lambda c: w_uq_d[l, c * 128:(c + 1) * 128, :], 2, 384, stg)
            load_cast_weight(wukv, lambda c: wukv[:, :], lambda c: w_ukv_d[l, :, :], 1, 512, stg)
            g1 = kb.sb("g1", [128, 1024], F32)
            gq = kb.sb("gq", [128, 256], F32)
            gkv = kb.sb("gkv", [128, 128], F32)
            fb = kb.sb("fb", [128, 4], F32)
            bcast_load(g1, norm1_d[l:l + 1, :], 1024)
            bcast_load(gq, gq_d[l:l + 1, :], 256)
            bcast_load(gkv, gkv_d[l:l + 1, :], 128)
            bcast_load(fb, foxb_d[l:l + 1, :], 4)
            nfb = kb.sb("nfb", [128, 4], F32)
            kb.op("dve", "tensor_scalar", [fb], [nfb], out=nfb[:], in0=fb[:], scalar1=-1.0, scalar2=None,
                  op0=ALU.mult)

            xt = [kb.sb("xt%d" % i, [128, 1024], F32) for i in range(2)]
            rt = [kb.sb("rt%d" % i, [128, 96], F32) for i in range(2)]
            junk = kb.sb("junk", [128, 1024], F32)
            ss = kb.sb("ss", [128, 4], F32)
            rstd = kb.sb("rstd", [128, 4], F32)
            hb = kb.sb("hb", [128, 1024], BF16)
            hT = kb.sb("hT", [128, 8, 128], BF16)
            projs = [kb.sb("proj%d" % i, [128, IN_WIDTH], F32) for i in range(2)]
            junk2 = kb.sb("junk2", [128, 384], F32)
            ss2 = kb.sb("ss2", [128, 4], F32)
            rstd2 = kb.sb("rstd2", [128, 4], F32)
            r64 = kb.sb("r64", [128, 14, 64], BF16)
            r32 = kb.sb("r32", [128, 10, 32], BF16)
            tmp = kb.sb("tmp", [128, 896], F32)
            tmpb = kb.sb("tmpb", [128, 896], F32)
            tmp2 = kb.sb("tmp2", [128, 320], F32)
            tmp2b = kb.sb("tmp2b", [128, 320], F32)
            tmp3 = kb.sb("tmp3", [128, 128], F32)
            tmp3a = kb.sb("tmp3a", [128, 128], F32)
            tmp3b = kb.sb("tmp3b", [128, 128], F32)
            cqn = kb.sb("cqn", [128, 384], BF16)
            cqnT = kb.sb("cqnT", [128, 3, 128], BF16)
            qa = kb.sb("qa", [128, 4, 96], BF16)
            ka = kb.sb("ka", [128, 4, 96], BF16)
            qd = kb.sb("qd", [128, 4, 68], BF16)
            kd = kb.sb("kd", [128, 4, 68], BF16)
            vst = {g: [kb.sb("vst%s%d" % (g, i), [128, HK[g], 65], BF16) for i in range(2)] for g in "ABCD"}
            for g in "ABCD":
                for i in range(2):
                    kb.op("pool", "memset", [], [vst[g][i]], ap=vst[g][i][:], constant=1.0)
            kb.op("pool", "memset", [], [qd], ap=qd[:], constant=1.0)
            kb.op("pool", "memset", [], [kd], ap=kd[:], constant=1.0)
            logf = kb.sb("logf", [128, 4], F32)
            cum = [kb.sb("cum%d" % i, [128, 4], F32) for i in range(2)]
            kb.op("pool", "memset", [], [cum[1]], ap=cum[1][:], constant=0.0)
            c8 = kb.sb("c8", [128, 4], F32)
            cp = kb.sb("cp", [128, 3, 4], BF16)
            cr = kb.sb("cr", [128, 4], F32)
            wst = [kb.sb("wst%d" % i, [128, 8], F32) for i in range(2)]
            tst = [[kb.sb("tst%d_%d" % (j, i), [128, 4, 128], BF16) for j in range(9)] for i in range(2)]

            def stage1(tt, g2=None):
                par = tt % 2
                tok = slice(tt * 128, (tt + 1) * 128)
                proj = projs[par]
                x = xt[par]
                kb.dma("pool", x[:], xsrc_d[tok, :], reads=[r_xsrc], writes=[x], owner=x)
                kb.dma("pool", rt[par][:], ropet_d[tok, :], reads=[r_const], writes=[rt[par]], owner=rt[par])
                rmsnorm_rstd(x, x[:], 1024, ss, rstd, junk, lnexp=True)
                kb.op("dve", "scalar_tensor_tensor", [x, rstd, g1], [hb], out=hb[:], in0=x[:], scalar=rstd[:, 0:1],
                      in1=g1[:], op0=ALU.mult, op1=ALU.mult)
                bT = banks[6]
                bTv = bT[:].bitcast(BF16)
                for kc in range(8):
                    kb.op("pe", "transpose", [hb, ident], [bT], out=bTv[:, kc * 128:(kc + 1) * 128],
                          in_=hb[:, kc * 128:(kc + 1) * 128], identity=ident[:, 0:128])
                kb.op("act", "activation", [bT], [hT], out=hT[:].rearrange("p k t -> p (k t)"), in_=bTv,
                      func=AF.Copy)
                for nb in range(6):
                    c0, c1 = nb * 512, min((nb + 1) * 512, IN_WIDTH)
                    for kc in range(8):
                        kb.op("pe", "matmul", [hT, win], [banks[nb]], out=banks[nb][:, 0:c1 - c0], lhsT=hT[:, kc, :],
                              rhs=win[:, kc, c0:c1], start=(kc == 0), stop=(kc == 7))
                    if nb % 2 == 0:
                        kb.op("act", "activation", [banks[nb]], [proj], out=proj[:, c0:c1],
                              in_=banks[nb][:, 0:c1 - c0], func=AF.Copy)
                    else:
                        kb.op("dve", "tensor_copy", [banks[nb]], [proj], out=proj[:, c0:c1],
                              in_=banks[nb][:, 0:c1 - c0])
                    if g2 is not None:
                        next(g2, None)
                        next(g2, None)
                if g2 is not None:
                    for _ in g2:
                        pass
            def stage2(tt):
                par = tt % 2
                tok = slice(tt * 128, (tt + 1) * 128)
                proj = projs[par]
                c64, s64, c32, s32 = (rt[par], 0), (rt[par], 32), (rt[par], 64), (rt[par], 80)
                bT = banks[7]
                bTv = bT[:].bitcast(BF16)
                rope(proj, proj[:, 0:896].rearrange("p (h two d) -> p h two d", h=14, two=2), r64,
                     r64[:].rearrange("p h (two d) -> p h two d", two=2), c64, s64, 14, 32, tmp, tmpb)
                yield
                rope(proj, proj[:, 896:1216].rearrange("p (h two d) -> p h two d", h=10, two=2), r32,
                     r32[:].rearrange("p h (two d) -> p h two d", two=2), c32, s32, 10, 16, tmp2, tmp2b)
                vs = {g: vst[g][par] for g in "ABCD"}
                o = COL["b_v"][0]
                kb.op("pool", "tensor_copy", [proj], [vs["B"]], out=vs["B"][:, :, 0:64],
                      in_=proj[:, o:o + 128].rearrange("p (h d) -> p h d", h=2))
                o = COL["c_v"][0]
                kb.op("act", "activation", [proj], [vs["C"]], out=vs["C"][:, :, 0:64],
                      in_=proj[:, o:o + 256].rearrange("p (h d) -> p h d", h=4), func=AF.Copy)
                o = COL["d_v"][0]
                kb.op("act", "activation", [proj], [vs["D"]], out=vs["D"][:, :, 0:64],
                      in_=proj[:, o:o + 256].rearrange("p (h d) -> p h d", h=4), func=AF.Copy)
                yield
                o = COL["a_cq"][0]
                kb.op("act", "activation", [proj], [junk2, ss2], out=junk2[:, 0:256], in_=proj[:, o:o + 256],
                      func=AF.Square, accum_out=ss2[:, 1:2])
                kb.op("act", "activation", [proj], [junk2, ss2], out=junk2[:, 256:384], in_=proj[:, o + 256:o + 384],
                      func=AF.Square, accum_out=ss2[:, 2:3])
                kb.op("dve", "tensor_scalar", [ss2], [rstd2], out=rstd2[:, 1:2], in0=ss2[:, 1:2], scalar1=1.0 / 256,
                      scalar2=EPS, op0=ALU.mult, op1=ALU.add)
                kb.op("dve", "tensor_scalar", [ss2], [rstd2], out=rstd2[:, 2:3], in0=ss2[:, 2:3], scalar1=1.0 / 128,
                      scalar2=EPS, op0=ALU.mult, op1=ALU.add)
                kb.op("act", "activation", [rstd2], [rstd2], out=rstd2[:, 1:3], in_=rstd2[:, 1:3], func=AF.Ln)
                kb.op("act", "activation", [rstd2], [rstd2], out=rstd2[:, 1:3], in_=rstd2[:, 1:3], func=AF.Exp,
                      scale=-0.5)
                kb.op("dve", "scalar_tensor_tensor", [proj, rstd2, gq], [cqn], out=cqn[:, 0:256],
                      in0=proj[:, o:o + 256], scalar=rstd2[:, 1:2], in1=gq[:], op0=ALU.mult, op1=ALU.mult)
                kb.op("dve", "scalar_tensor_tensor", [proj, rstd2, gkv], [cqn], out=cqn[:, 256:384],
                      in0=proj[:, o + 256:o + 384], scalar=rstd2[:, 2:3], in1=gkv[:], op0=ALU.mult, op1=ALU.mult)
                for kc in range(3):
                    kb.op("pe", "transpose", [cqn, ident], [bT], out=bTv[:, kc * 128:(kc + 1) * 128],
                          in_=cqn[:, kc * 128:(kc + 1) * 128], identity=ident[:, 0:128])
                kb.op("act", "activation", [bT], [cqnT], out=cqnT[:].rearrange("p k t -> p (k t)"),
                      in_=bTv[:, 0:384], func=AF.Copy)
                yield
                bq, bkv = banks[0], banks[1]
                for kc in range(2):
                    kb.op("pe", "matmul", [cqnT, wuq], [bq], out=bq[:, 0:384], lhsT=cqnT[:, kc, :], rhs=wuq[:, kc, :],
                          start=(kc == 0), stop=(kc == 1))
                kb.op("pe", "matmul", [cqnT, wukv], [bkv], out=bkv[:, 0:512], lhsT=cqnT[:, 2, :], rhs=wukv[:, :],
                      start=True, stop=True)
                bq3 = bq[:, 0:384].rearrange("p (h d) -> p h d", h=4)
                kb.op("act", "activation", [bq], [qa], out=qa[:, :, 0:64], in_=bq3[:, :, 0:64], func=AF.Copy)
                kb.op("act", "activation", [bq], [tmp3], out=tmp3[:, 0:128].rearrange("p (h d) -> p h d", h=4),
                      in_=bq3[:, :, 64:96], func=AF.Copy)
                rope(tmp3, tmp3[:, 0:128].rearrange("p (h two d) -> p h two d", h=4, two=2), qa,
                     qa[:, :, 64:96].rearrange("p h (two d) -> p h two d", two=2), c32, s32, 4, 16, tmp3a, tmp3b)
                bkv3 = bkv[:, 0:512].rearrange("p (h d) -> p h d", h=4)
                kb.op("act", "activation", [bkv], [ka], out=ka[:, :, 0:64], in_=bkv3[:, :, 0:64], func=AF.Copy)
                kb.op("dve", "tensor_copy", [bkv], [vs["A"]], out=vs["A"][:, :, 0:64], in_=bkv3[:, :, 64:128])
                kb.op("pool", "tensor_copy", [r32], [ka], out=ka[:, :, 64:96],
                      in_=r32[:, 9:10, :].to_broadcast([128, 4, 32]))
                yield
                o = COL["d_f"][0]
                kb.op("dve", "scalar_tensor_tensor", [proj, nfb], [logf], out=logf[:], in0=proj[:, o:o + 4],
                      scalar=-1.0, in1=nfb[:], op0=ALU.mult, op1=ALU.add)
                kb.op("act", "activation", [logf], [logf], out=logf[:], in_=logf[:], func=AF.Exp)
                kb.op("act", "activation", [logf], [logf], out=logf[:], in_=logf[:], func=AF.Ln, bias=1.0)
                kb.op("dve", "tensor_scalar", [logf], [logf], out=logf[:], in0=logf[:], scalar1=-1.0, scalar2=None,
                      op0=ALU.mult)
                bc = banks[2]
                cprev, ccur = cum[(tt + 1) % 2], cum[tt % 2]
                kb.op("pe", "matmul", [cm_f, logf], [bc], out=bc[:, 0:4], lhsT=TRI, rhs=logf[:], start=True,
                      stop=False)
                kb.op("pe", "matmul", [cm_f, cprev], [bc], out=bc[:, 0:4], lhsT=LAST, rhs=cprev[:], start=False,
                      stop=True)
                kb.op("dve", "tensor_copy", [bc], [ccur], out=ccur[:], in_=bc[:, 0:4])
                kb.op("dve", "tensor_scalar", [ccur], [c8], out=c8[:], in0=ccur[:], scalar1=8.0, scalar2=None,
                      op0=ALU.mult)
                kb.op("dve", "tensor_copy", [c8], [cp], out=cp[:, 0, :], in_=c8[:])
                kb.op("dve", "tensor_tensor", [c8, cp], [cr], out=cr[:], in0=c8[:], in1=cp[:, 0, :], op=ALU.subtract)
                kb.op("dve", "tensor_copy", [cr], [cp], out=cp[:, 1, :], in_=cr[:])
                kb.op("dve", "tensor_tensor", [cr, cp], [cr], out=cr[:], in0=cr[:], in1=cp[:, 1, :], op=ALU.subtract)
                kb.op("dve", "tensor_copy", [cr], [cp], out=cp[:, 2, :], in_=cr[:])
                o = COL["d_q"][0]
                kb.op("pool", "tensor_copy", [proj], [qd], out=qd[:, :, 0:64],
                      in_=proj[:, o:o + 256].rearrange("p (h d) -> p h d", h=4))
                kb.op("pool", "tensor_copy", [cp], [qd], out=qd[:, :, 64:65], in_=cp[:, 0, :].unsqueeze(2))
                o = COL["d_k"][0]
                kb.op("pool", "tensor_copy", [proj], [kd], out=kd[:, :, 0:64],
                      in_=proj[:, o:o + 256].rearrange("p (h d) -> p h d", h=4))
                kb.op("dve", "tensor_scalar", [cp], [kd], out=kd[:, :, 65:68], in0=cp[:].rearrange("p c h -> p h c"),
                      scalar1=-1.0, scalar2=None, op0=ALU.mult)
                yield
                o = COL["c_w"][0]
                ws = wst[par]
                kb.op("pool", "tensor_copy", [proj], [ws], out=ws[:], in_=proj[:, o:o + 8])
                kb.dma("sp", wi_d[tok, :], ws[:], reads=[ws], writes=[r_wi], owner=ws)
                for g in "ABCD":
                    kb.dma("sp", v_d[g][tok, :, :], vs[g][:], reads=[vs[g]], writes=[r_v[g]], owner=vs[g])
                yield
                items = [
                    (qa, [qa[:, h, :] for h in range(4)], 96, qT_d["A"], r_qT["A"]),
                    (ka, [ka[:, h, :] for h in range(4)], 96, kT_d["A"], r_kT["A"]),
                    (r64, [r64[:, h, :] for h in range(0, 4)], 64, qT_d["B"], r_qT["B"]),
                    (r64, [r64[:, h, :] for h in range(4, 6)], 64, kT_d["B"], r_kT["B"]),
                    (r64, [r64[:, h, :] for h in range(6, 10)], 64, qT_d["C"], r_qT["C"]),
                    (r64, [r64[:, h, :] for h in range(10, 14)], 64, kT_d["C"], r_kT["C"]),
                    (qd, [qd[:, h, :] for h in range(4)], 68, qT_d["D"], r_qT["D"]),
                    (kd, [kd[:, h, :] for h in range(4)], 68, kT_d["D"], r_kT["D"]),
                    (r32, [r32[:, h, :] for h in range(0, 4)], 32, qiT_d[:, 0:4, :], r_qiT),
                    (r32, [r32[:, h, :] for h in range(4, 8)], 32, qiT_d[:, 4:8, :], r_qiT),
                    (r32, [r32[:, 8, :]], 32, None, r_kiT),
                ]
                for ii, (src, aps, n, dst, rdst) in enumerate(items):
                    bk = banks[3 + (ii % 3)]
                    bkv_ = bk[:].bitcast(BF16)
                    nh = len(aps)
                    for h, ap in enumerate(aps):
                        kb.op("pe", "transpose", [src, ident], [bk], out=bkv_[0:n, h * 128:(h + 1) * 128], in_=ap,
                              identity=ident[:, 0:128])
                    sg = tst[par][ii % 9] if ii < 9 else tst[par][ii - 9 + 0]
                    en = "act" if ii % 2 == 0 else "dve"
                    if en == "act":
                        kb.op("act", "activation", [bk], [sg], out=sg[0:n, 0:nh, :].rearrange("p h t -> p (h t)"),
                              in_=bkv_[0:n, 0:nh * 128], func=AF.Copy)
                    else:
                        kb.op("dve", "tensor_copy", [bk], [sg], out=sg[0:n, 0:nh, :].rearrange("p h t -> p (h t)"),
                              in_=bkv_[0:n, 0:nh * 128])
                    if dst is None:
                        kb.dma("sp", kiT_d[:, tok], sg[0:32, 0, :], reads=[sg], writes=[rdst], owner=sg)
                    else:
                        kb.dma("sp", dst[:, :, tok], sg[0:n, 0:nh, :], reads=[sg], writes=[rdst], owner=sg)
                    if ii % 3 == 2:
                        yield
            stage1(0)
            for tt in range(NT):
                g2 = stage2(tt)
                if tt + 1 < NT:
                    stage1(tt + 1, g2)
                else:
                    for _ in g2:
                        pass
            kb.barrier()
            kb.stack = old

    def attention(l, g, hook=None):
        dk = DKA[g]
        hk = HK[g]
        gi = "ABCD".index(g)
        scale = {"A": 96 ** -0.5, "B": 0.125, "C": 0.125, "D": 0.125}[g]
        with ExitStack() as st2:
            old = kb.stack
            kb.stack = st2
            qT = kb.sb("aqT", [dk, 4, S], BF16)
            kT = kb.sb("akT", [dk, hk, S], BF16)
            vv = kb.sb("avv", [128, NT, hk * 65], BF16)
            for h in range(4):
                kb.dma("sp", qT[:, h, :], qT_d[g][:, h, :], reads=[r_qT[g]], writes=[qT], owner=qT)
            for h in range(hk):
                kb.dma("sp", kT[:, h, :], kT_d[g][:, h, :], reads=[r_kT[g]], writes=[kT], owner=kT)
            vsrc = v_d[g].rearrange("(j p) h d -> p j (h d)", p=128)
            for j0 in range(0, NT, 4):
                kb.dma("sp", vv[:, j0:j0 + 4, :], vsrc[:, j0:j0 + 4, :], reads=[r_v[g]], writes=[vv], owner=vv)
            nbuf = 2 if g == "C" else 3
            sbanks = [banks[0], banks[1], banks[4]]
            pT = [kb.sb("apT%d" % i, [128, 512], BF16) for i in range(nbuf)]
            osb = [kb.sb("aosb%d" % i, [128, 4, 64], BF16) for i in range(2)]
            den = kb.sb("aden", [128, 4], F32)
            if g == "B":
                esink = kb.sb("esink", [128, 4], F32)
                bcast_load(esink, sinks_d[l:l + 1, :], 4)
                kb.op("act", "activation", [esink], [esink], out=esink[:], in_=esink[:], func=AF.Exp)
            if g == "C":
                qiTs = [kb.sb("cqiT%d" % i, [32, 8, 128], BF16) for i in range(2)]
                kiT = kb.sb("ckiT", [32, S], BF16)
                kb.dma("sp", kiT[:], kiT_d[:, :], reads=[r_kiT], writes=[kiT], owner=kiT)
                wsb = kb.sb("cwsb", [128, NT, 8], F32)
                wsrc = wi_d.rearrange("(j p) h -> p j h", p=128)
                for j0 in range(0, NT, 4):
                    kb.dma("sp", wsb[:, j0:j0 + 4, :], wsrc[:, j0:j0 + 4, :], reads=[r_wi], writes=[wsb], owner=wsb)
                Isbs = [kb.sb("cI%d" % i, [128, S], F32) for i in range(2)]
                Madd = [kb.sb("cMadd%d" % i, [128, S], BF16) for i in range(2)]
                rl = [kb.sb("crl%d" % i, [128, 512], BF16) for i in range(3)]
                dgs = [kb.sb("cdg%d" % i, [128, 8, 128], BF16) for i in range(2)]
                cjunk = kb.sb("cjunk", [128, S], BF16)
                lo = kb.sb("clo", [128, 1], F32)
                stp = kb.sb("cstp", [128, 1], F32)
                cand = kb.sb("ccand", [128, 1], F32)
                cnt = kb.sb("ccnt", [128, 1], F32)
                mm = kb.sb("cmm", [128, 1], F32)
                hi = kb.sb("chi", [128, 1], F32)

            def kts_of(qt):
                if g == "B":
                    return [kt for kt in (qt - 1, qt) if kt >= 0]
                return list(range(qt + 1))

            def c_scores(qt):
                qs = slice(qt * 128, (qt + 1) * 128)
                Lk = 128 * (qt + 1)
                nblk = (Lk + 511) // 512
                Isb = Isbs[qt % 2]
                qiT = qiTs[qt % 2]
                dg = dgs[qt % 2]
                kb.dma("pool", qiT[:], qiT_d[:, :, qs], reads=[r_qiT], writes=[qiT], owner=qiT)
                kb.op("pool", "tensor_tensor", [ident4, wsb], [dg], out=dg[:],
                      in0=ident4[:, 0:128].unsqueeze(1).to_broadcast([128, 8, 128]),
                      in1=wsb[:, qt, :].unsqueeze(2).to_broadcast([128, 8, 128]), op=ALU.mult)
                hc = 0
                for kbk in range(nblk):
                    k0, k1 = kbk * 512, min((kbk + 1) * 512, Lk)
                    n = k1 - k0
                    bacc = banks[6 + (kbk % 2)]
                    pend = None
                    for hh in range(8):
                        bi = banks[4 + (hc % 2)]
                        r_ = rl[hc % 3]
                        kb.op("pe", "matmul", [qiT, kiT], [bi], out=bi[:, 0:n], lhsT=qiT[:, hh, :],
                              rhs=kiT[:, k0:k1], start=True, stop=True)
                        kb.op("act", "activation", [bi], [r_], out=r_[:, 0:n], in_=bi[:, 0:n], func=AF.Relu)
                        if pend is not None:
                            ph, pr = pend
                            kb.op("pe", "matmul", [dg, pr], [bacc], out=bacc[:, 0:n], lhsT=dg[:, ph, :], rhs=pr[:, 0:n],
                                  start=(ph == 0), stop=False)
                        pend = (hh, r_)
                        hc += 1
                    ph, pr = pend
                    kb.op("pe", "matmul", [dg, pr], [bacc], out=bacc[:, 0:n], lhsT=dg[:, ph, :], rhs=pr[:, 0:n],
                          start=False, stop=True)
                    kb.op("act", "activation", [bacc], [Isb], out=Isb[:, k0:k1], in_=bacc[:, 0:n], func=AF.Copy)

            def c_select(qt):
                qs = slice(qt * 128, (qt + 1) * 128)
                Lk = 128 * (qt + 1)
                Isb = Isbs[qt % 2]
                madd = Madd[qt % 2]
                kb.op("dve", "tensor_tensor", [Isb, cm_f], [Isb], out=Isb[:, qs], in0=Isb[:, qs], in1=negS, op=ALU.add)
                if Lk <= topk:
                    kb.op("dve", "tensor_scalar", [Isb], [madd], out=madd[:, 0:Lk], in0=Isb[:, 0:Lk],
                          scalar1=-1.0e29, scalar2=-BIGM, op0=ALU.is_lt, op1=ALU.mult)
                    return
                assert Lk - 128 >= topk
                kb.op("dve", "tensor_reduce", [Isb], [hi], out=hi[:], in_=Isb[:, 0:Lk], axis=AX.X, op=ALU.max)
                kb.op("dve", "tensor_reduce", [Isb], [lo], out=lo[:], in_=Isb[:, 0:Lk - 128], axis=AX.X, op=ALU.min)
                kb.op("dve", "tensor_tensor", [hi, lo], [stp], out=stp[:], in0=hi[:], in1=lo[:], op=ALU.subtract)
                for it in range(N_BISECT):
                    f = 0.5 ** (it + 1)
                    kb.op("dve", "scalar_tensor_tensor", [stp, lo], [cand], out=cand[:], in0=stp[:], scalar=f,
                          in1=lo[:], op0=ALU.mult, op1=ALU.add)
                    kb.op("dve", "tensor_scalar", [Isb, cand], [cjunk, cnt], out=cjunk[:, 0:Lk], in0=Isb[:, 0:Lk],
                          scalar1=cand[:, 0:1], scalar2=None, op0=ALU.is_ge, op1=ALU.add, accum_out=cnt[:, 0:1])
                    kb.op("dve", "scalar_tensor_tensor", [cnt, stp], [mm], out=mm[:], in0=cnt[:],
                          scalar=float(topk) - 0.5, in1=stp[:], op0=ALU.is_ge, op1=ALU.mult)
                    kb.op("dve", "scalar_tensor_tensor", [mm, lo], [lo], out=lo[:], in0=mm[:], scalar=f, in1=lo[:],
                          op0=ALU.mult, op1=ALU.add)
                kb.op("dve", "tensor_scalar", [Isb, lo], [madd], out=madd[:, 0:Lk], in0=Isb[:, 0:Lk],
                      scalar1=lo[:, 0:1], scalar2=-BIGM, op0=ALU.is_lt, op1=ALU.mult)

            def emit_scores(qt, ki, kt):
                qs = slice(qt * 128, (qt + 1) * 128)
                ks = slice(kt * 128, (kt + 1) * 128)
                bs = sbanks[ki % nbuf]
                have_mask = False
                if g == "C":
                    madd = Madd[qt % 2]
                    kb.op("pe", "matmul", [madd, ident4], [bs], out=bs[:, 0:512], lhsT=madd[:, ks],
                          rhs=ident4[:, 0:512], start=True, stop=False, skip_group_check=True)
                    have_mask = True
                elif kt == qt:
                    kb.op("pe", "matmul", [ident4, maskc4], [bs], out=bs[:, 0:512], lhsT=ident4[:, 0:128],
                          rhs=maskc4[:, 0:512], start=True, stop=False, skip_group_check=True)
                    have_mask = True
                elif g == "B":
                    kb.op("pe", "matmul", [ident4, maskp4], [bs], out=bs[:, 0:512], lhsT=ident4[:, 0:128],
                          rhs=maskp4[:, 0:512], start=True, stop=False, skip_group_check=True)
                    have_mask = True
                for h in range(4):
                    kvh = h if hk == 4 else h // 2
                    kb.op("pe", "matmul", [kT, qT], [bs], out=bs[:, h * 128:(h + 1) * 128], lhsT=kT[:, kvh, ks],
                          rhs=qT[:, h, qs], start=((not have_mask) and h == 0), stop=True, skip_group_check=True)
                p = pT[ki % nbuf]
                kb.op("act", "activation", [bs], [p], out=p[:], in_=bs[:, 0:512], func=AF.Exp, scale=scale)

            def emit_pv(qt, ki, kt, nk):
                bo = banks[2 + (qt % 2)]
                p = pT[ki % nbuf]
                for h in range(4):
                    kvh = h if hk == 4 else h // 2
                    kb.op("pe", "matmul", [p, vv], [bo], out=bo[:, h * 65:(h + 1) * 65],
                          lhsT=p[:, h * 128:(h + 1) * 128], rhs=vv[:, kt, kvh * 65:(kvh + 1) * 65],
                          start=(ki == 0 and h == 0), stop=(ki == nk - 1), skip_group_check=True)

            def emit_attn(qt):
                kts = kts_of(qt)
                pend = []
                for ki, kt in enumerate(kts):
                    emit_scores(qt, ki, kt)
                    pend.append((ki, kt))
                    if len(pend) > nbuf - 1:
                        a = pend.pop(0)
                        emit_pv(qt, a[0], a[1], len(kts))
                for a in pend:
                    emit_pv(qt, a[0], a[1], len(kts))

            def emit_norm(qt):
                qs = slice(qt * 128, (qt + 1) * 128)
                bo = banks[2 + (qt % 2)]
                bo3 = bo[:, 0:260].rearrange("p (h d) -> p h d", h=4)
                if g == "B":
                    kb.op("dve", "tensor_tensor", [bo, esink], [den], out=den[:].unsqueeze(2), in0=bo3[:, :, 64:65],
                          in1=esink[:].unsqueeze(2), op=ALU.add)
                else:
                    kb.op("dve", "tensor_copy", [bo], [den], out=den[:].unsqueeze(2), in_=bo3[:, :, 64:65])
                kb.op("dve", "reciprocal", [den], [den], out=den[:], in_=den[:])
                ob = osb[qt % 2]
                kb.op("dve", "tensor_tensor", [bo, den], [ob], out=ob[:], in0=bo3[:, :, 0:64],
                      in1=den[:].unsqueeze(2).to_broadcast([128, 4, 64]), op=ALU.mult)
                kb.dma("sp", mixed_d[qs, gi * 256:(gi + 1) * 256], ob[:].rearrange("p h d -> p (h d)"), reads=[ob],
                       writes=[r_mixed], owner=ob)

            if g == "C":
                c_scores(0)
                for qt in range(NT):
                    if qt + 1 < NT:
                        c_scores(qt + 1)
                    c_select(qt)
                    if qt > 0:
                        emit_norm(qt - 1)
                    emit_attn(qt)
                emit_norm(NT - 1)
            else:
                for qt in range(NT):
                    emit_attn(qt)
                    emit_norm(qt)
                    if hook:
                        hook.pop(0)()
                while hook:
                    hook.pop(0)()
            kb.barrier()
            kb.stack = old

    def mlp_weight_chunks(l, wu, wd, stg, which):
        ems = []
        engs = ("dve", "pool", "dve") if which == "u" else ("pool", "act", "dve")

        def mk(c, dst_ap, src_ap, ncols):
            def em():
                st = stg[c % len(stg)]
                kb.dma("sp", st[:, 0:ncols], src_ap, reads=[r_const], writes=[st], owner=st)
                en = engs[c % 3]
                dst_tile = wu if c < 16 else wd
                if en == "act":
                    kb.op("act", "activation", [st], [dst_tile], out=dst_ap, in_=st[:, 0:ncols], func=AF.Copy)
                else:
                    kb.op(en, "tensor_copy", [st], [dst_tile], out=dst_ap, in_=st[:, 0:ncols])
            return em

        if which == "u":
            for c in range(16):
                ems.append(mk(c, wu[:, c // 2, (c % 2) * 2048:(c % 2 + 1) * 2048],
                              w_up_d[l, (c // 2) * 128:(c // 2 + 1) * 128, (c % 2) * 2048:(c % 2 + 1) * 2048], 2048))
        else:
            for c in range(32):
                ems.append(mk(16 + c, wd[:, c, :], w_down_d[l, c * 128:(c + 1) * 128, :], 1024))
        return ems

    def phase3a(l, xsrc_d, r_xsrc, wchunks):
        with ExitStack() as st3:
            old = kb.stack
            kb.stack = st3
            wo = kb.sb("wo", [128, 8, 1024], BF16)
            stg = [kb.sb("p3stg%d" % i, [128, 1024], F32) for i in range(2)]
            load_cast_weight(wo, lambda c: wo[:, c, :], lambda c: w_out_d[l, c * 128:(c + 1) * 128, :], 8, 1024, stg)
            mx = [kb.sb("mx%d" % i, [128, 1024], BF16) for i in range(2)]
            mT = [kb.sb("mT%d" % i, [128, 8, 128], BF16) for i in range(2)]
            xt = [kb.sb("x3t%d" % i, [128, 1024], F32) for i in range(2)]
            xo = [kb.sb("x3o%d" % i, [128, 1024], F32) for i in range(2)]
            def sA(tt):
                par = tt % 2
                tok = slice(tt * 128, (tt + 1) * 128)
                kb.dma("pool", mx[par][:], mixed_d[tok, :], reads=[r_mixed], writes=[mx[par]], owner=mx[par])
                kb.dma("pool", xt[par][:], xsrc_d[tok, :], reads=[r_xsrc], writes=[xt[par]], owner=xt[par])
                bT = banks[4 + par]
                bTv = bT[:].bitcast(BF16)
                for kc in range(8):
                    kb.op("pe", "transpose", [mx[par], ident], [bT], out=bTv[:, kc * 128:(kc + 1) * 128],
                          in_=mx[par][:, kc * 128:(kc + 1) * 128], identity=ident[:, 0:128])
                kb.op("act", "activation", [bT], [mT[par]], out=mT[par][:].rearrange("p k t -> p (k t)"), in_=bTv,
                      func=AF.Copy)

            def sB(tt):
                par = tt % 2
                tok = slice(tt * 128, (tt + 1) * 128)
                for nb in range(2):
                    bk = banks[2 * par + nb]
                    for kc in range(8):
                        kb.op("pe", "matmul", [mT[par], wo], [bk], out=bk[:, 0:512], lhsT=mT[par][:, kc, :],
                              rhs=wo[:, kc, nb * 512:(nb + 1) * 512], start=(kc == 0), stop=(kc == 7))
                    kb.op("dve", "tensor_tensor", [bk, xt[par]], [xo[par]], out=xo[par][:, nb * 512:(nb + 1) * 512],
                          in0=bk[:, 0:512], in1=xt[par][:, nb * 512:(nb + 1) * 512], op=ALU.add)
                kb.dma("sp", xm_d[tok, :], xo[par][:], reads=[xo[par]], writes=[r_xm], owner=xo[par])

            sA(0)
            wq = list(wchunks)
            per = (len(wq) + NT - 1) // NT
            for tt in range(NT):
                if tt + 1 < NT:
                    sA(tt + 1)
                sB(tt)
                for _ in range(per):
                    if wq:
                        wq.pop(0)()
            while wq:
                wq.pop(0)()
            kb.barrier()
            kb.stack = old

    def phase3b(l, xdst_d, r_xdst, final, wu, wd):
        with ExitStack() as st4:
            old = kb.stack
            kb.stack = st4
            g2 = kb.sb("g2", [128, 1024], F32)
            bcast_load(g2, norm2_d[l:l + 1, :], 1024)
            if final:
                gf = kb.sb("gf", [128, 1024], F32)
                bcast_load(gf, fnorm_d[0:1, :], 1024)
            TB = 2
            xt = [[kb.sb("x4t%d_%d" % (i, j), [128, 1024], F32) for j in range(TB)] for i in range(2)]
            hb = kb.sb("h4b", [128, 1024], BF16)
            hT = [kb.sb("h4T%d" % i, [128, 8, TB * 128], BF16) for i in range(2)]
            uT = [kb.sb("u4T%d" % i, [128, TB * 128], BF16) for i in range(4)]
            rT = [kb.sb("r4T%d" % i, [128, TB * 128], F32) for i in range(3)]
            junk = kb.sb("junk4", [128, 1024], F32)
            ss = kb.sb("ss4", [128, 2], F32)
            rstd = kb.sb("rstd4", [128, 2], F32)
            nblk = NT // TB
            def pre(b):
                par = b % 2
                for j in range(TB):
                    tok = slice((b * TB + j) * 128, (b * TB + j + 1) * 128)
                    x = xt[par][j]
                    kb.dma("pool", x[:], xm_d[tok, :], reads=[r_xm], writes=[x], owner=x)
                    rmsnorm_rstd(x, x[:], 1024, ss, rstd, junk)
                    kb.op("dve", "scalar_tensor_tensor", [x, rstd, g2], [hb], out=hb[:], in0=x[:],
                          scalar=rstd[:, 0:1], in1=g2[:], op0=ALU.mult, op1=ALU.mult)
                    bT = banks[7]
                    bTv = bT[:].bitcast(BF16)
                    for kc in range(8):
                        kb.op("pe", "transpose", [hb, ident], [bT], out=bTv[:, kc * 128:(kc + 1) * 128],
                              in_=hb[:, kc * 128:(kc + 1) * 128], identity=ident[:, 0:128])
                    kb.op("act", "activation", [bT], [hT[par]], out=hT[par][:, :, j * 128:(j + 1) * 128],
                          in_=bTv.rearrange("p (k t) -> p k t", k=8), func=AF.Copy)
            def main(b):
                par = b % 2
                accs = [banks[0], banks[1], banks[2], banks[3]]
                def emit_up(fc):
                    bu = banks[4 + fc % 3]
                    for kc in range(8):
                        kb.op("pe", "matmul", [wu, hT[par]], [bu], out=bu[:, 0:TB * 128],
                              lhsT=wu[:, kc, fc * 128:(fc + 1) * 128], rhs=hT[par][:, kc, :], start=(kc == 0),
                              stop=(kc == 7))
                    u = uT[fc % 4]
                    rr = rT[fc % 3]
                    kb.op("act", "activation", [bu], [rr], out=rr[:], in_=bu[:, 0:TB * 128], func=AF.Relu)
                    kb.op("dve" if fc % 2 == 0 else "pool", "tensor_tensor", [rr], [u], out=u[:], in0=rr[:],
                          in1=rr[:], op=ALU.mult)

                def emit_down(fc):
                    u = uT[fc % 4]
                    for j in range(TB):
                        for nb in range(2):
                            acc = accs[j * 2 + nb]
                            kb.op("pe", "matmul", [u, wd], [acc], out=acc[:, 0:512], lhsT=u[:, j * 128:(j + 1) * 128],
                                  rhs=wd[:, fc, nb * 512:(nb + 1) * 512], start=(fc == 0), stop=(fc == 31))

                emit_up(0)
                emit_up(1)
                for fc in range(32):
                    if fc + 2 < 32:
                        emit_up(fc + 2)
                    emit_down(fc)
                for j in range(TB):
                    tok = slice((b * TB + j) * 128, (b * TB + j + 1) * 128)
                    o = xt[par][j]
                    for nb in range(2):
                        acc = accs[j * 2 + nb]
                        kb.op("dve", "tensor_tensor", [acc, xt[par][j]], [o], out=o[:, nb * 512:(nb + 1) * 512],
                              in0=acc[:, 0:512], in1=xt[par][j][:, nb * 512:(nb + 1) * 512], op=ALU.add)
                    if final:
                        kb.op("act", "activation", [o], [junk, ss], out=junk[:], in_=o[:], func=AF.Square,
                              accum_out=ss[:, 1:2])
                        kb.op("dve", "tensor_scalar", [ss], [rstd], out=rstd[:, 1:2], in0=ss[:, 1:2],
                              scalar1=1.0 / 1024, scalar2=EPS, op0=ALU.mult, op1=ALU.add)
                        kb.op("act", "activation", [rstd], [rstd], out=rstd[:, 1:2], in_=rstd[:, 1:2], func=AF.Sqrt)
                        kb.op("dve", "reciprocal", [rstd], [rstd], out=rstd[:, 1:2], in_=rstd[:, 1:2])
                        kb.op("dve", "scalar_tensor_tensor", [o, rstd, gf], [o], out=o[:], in0=o[:],
                              scalar=rstd[:, 1:2], in1=gf[:], op0=ALU.mult, op1=ALU.mult)
                    kb.dma("sp", xdst_d[tok, :], o[:], reads=[o], writes=[r_xdst], owner=o)

            pre(0)
            for b in range(nblk):
                if b + 1 < nblk:
                    pre(b + 1)
                main(b)
            kb.barrier()
            kb.stack = old

    cur_d, cur_r = x_in, r_xin
    for l in range(depth):
        phase1(l, cur_d, cur_r)
        for g in "ABC":
            attention(l, g)
        with ExitStack() as stw:
            oldw = kb.stack
            kb.stack = stw
            wu = kb.sb("wu", [128, 8, D_FF], BF16)
            wstg = [kb.sb("p4stg%d" % i, [128, 2048], F32) for i in range(2)]
            attention(l, "D", hook=mlp_weight_chunks(l, wu, None, wstg, "u"))
            wd = kb.sb("wd", [128, 32, 1024], BF16)
            phase3a(l, cur_d, cur_r, mlp_weight_chunks(l, wu, wd, wstg, "d"))
            last = (l == depth - 1)
            if last:
                phase3b(l, out_d, r_out, True, wu, wd)
            else:
                phase3b(l, xs[l % 2], r_xs[l % 2], False, wu, wd)
                cur_d, cur_r = xs[l % 2], r_xs[l % 2]
            kb.stack = oldw
    kb.wait_all("sp", [r_out])
    kb.wait_all("pool", [r_out])
    kb.barrier()
    print("KB: nsem=%d instr=%s" % (kb.nsem, {n: len(E.prog) for n, E in kb.engs.items()}), flush=True)
    kb.replay()
    stack.close()
    return nc


def host_consts(S):
    pos = np.arange(S, dtype=np.float32)

    def tables(d):
        half = d // 2
        inv = (1.0 / (10000.0 ** (np.arange(0, half, dtype=np.float32) * 2.0 / d))).astype(np.float32)
        ang = pos[:, None] * inv[None, :]
        return np.cos(ang).astype(np.float32), np.sin(ang).astype(np.float32)

    c64, s64 = tables(64)
    c32, s32 = tables(32)
    cm = np.zeros((128, 5 * 512), np.float32)
    eye = np.eye(128, dtype=np.float32)
    kk = np.arange(128)[:, None]
    qq = np.arange(128)[None, :]
    mc = np.where(kk > qq, -BIGM, 0.0).astype(np.float32)
    mp = np.where(kk <= qq, -BIGM, 0.0).astype(np.float32)
    for h in range(4):
        cm[:, h * 128:(h + 1) * 128] = eye
        cm[:, 512 + h * 128:512 + (h + 1) * 128] = mc
        cm[:, 1024 + h * 128:1024 + (h + 1) * 128] = mp
    cm[:, 1536:1664] = np.where(qq > kk, NEG_S, 0.0)
    cm[:, 1664:1792] = (kk <= qq).astype(np.float32)
    cm[:, 1792:1920] = (kk == 127).astype(np.float32) * np.ones((1, 128), np.float32)
    cm[:, 2048:2176] = eye
    return c64, s64, c32, s32, cm


_CACHE = {}


def kernel(x, norm1, w_in, mla_q_norm, mla_kv_norm, mla_w_uq, mla_w_ukv, swa_sinks, fox_b_f, w_out, norm2, w_up,
           w_down, final_norm, _depth=None, _ncores=None, _debug=False):
    x = np.asarray(x, dtype=np.float32)
    B, S, _ = x.shape
    depth = int(_depth) if _depth is not None else int(np.asarray(w_in).shape[0])
    ncores = int(_ncores) if _ncores is not None else B
    f = lambda a: np.ascontiguousarray(np.asarray(a, dtype=np.float32))
    key = (S, depth, _debug)
    if key not in _CACHE:
        _CACHE[key] = build_program(S=S, depth=depth, debug=_debug)
    nc = _CACHE[key]
    c64, s64, c32, s32, cm = host_consts(S)
    shared = {
        "w_in": f(np.asarray(w_in)[:depth][:, :, PERM]),
        "w_uq": f(np.asarray(mla_w_uq)[:depth]),
        "w_ukv": f(np.asarray(mla_w_ukv)[:depth]),
        "w_out": f(np.asarray(w_out)[:depth]),
        "w_up": f(np.asarray(w_up)[:depth]),
        "w_down": f(np.asarray(w_down)[:depth]),
        "norm1": f(np.asarray(norm1)[:depth]),
        "norm2": f(np.asarray(norm2)[:depth]),
        "gq": f(np.asarray(mla_q_norm)[:depth]),
        "gkv": f(np.asarray(mla_kv_norm)[:depth]),
        "sinks": f(np.asarray(swa_sinks)[:depth]),
        "foxb": f(np.asarray(fox_b_f)[:depth]),
        "fnorm": f(np.asarray(final_norm).reshape(1, -1)),
        "ropet": np.ascontiguousarray(np.concatenate([c64, s64, c32, s32], axis=1)), "cmat": cm,
    }
    in_maps = []
    for b in range(ncores):
        m = dict(shared)
        m["x"] = f(x[b])
        in_maps.append(m)
    res = run_bass_kernel_spmd(nc, in_maps, core_ids=list(range(ncores)))
    out = np.stack([np.asarray(r["out"], dtype=np.float32) for r in res.results], axis=0)
    if _debug:
        return out, res.results
    return out
```
